# Optimizing a Trainium2 kernel written in Bass

```python
import jax, jax.numpy as jnp
from jax import lax
import numpy as np

D_MODEL = 2048
BATCH = 2
SEQ = 8192
DEPTH = 1

HEAD_DIM = 128
N_ATTN_HEADS = 8
ATTN_WIDTH = N_ATTN_HEADS * HEAD_DIM
CONV_CHANNELS = D_MODEL - ATTN_WIDTH
CONV_GROUPS = 8
MIX_WIDTH = ATTN_WIDTH + CONV_CHANNELS
IN_COLS = 3 * ATTN_WIDTH + 2 * CONV_CHANNELS
MOBA_BLOCK = 256
MOBA_TOPK = 3
Q_CHUNK = 64
CONV_WIDTH = 31
D_FF = 4 * D_MODEL
RMS_EPS = 1e-6
LN_EPS = 1e-5

kernel_name = 'hybrid_moba_conformer_layer'


def rms_norm(x, g):
    xf = x.astype(jnp.float32)
    y = xf * lax.rsqrt(jnp.mean(xf * xf, axis=-1, keepdims=True) + RMS_EPS)
    return (y * g.astype(jnp.float32)).astype(x.dtype)


def layer_norm(x, g, b):
    xf = x.astype(jnp.float32)
    mu = jnp.mean(xf, axis=-1, keepdims=True)
    var = jnp.mean(jnp.square(xf - mu), axis=-1, keepdims=True)
    y = (xf - mu) * lax.rsqrt(var + LN_EPS)
    return (y * g.astype(jnp.float32) + b.astype(jnp.float32)).astype(x.dtype)


def alibi_slopes(n_heads):
    return jnp.asarray(2.0 ** (-8.0 * np.arange(1, n_heads + 1) / n_heads), dtype=jnp.float32)


def moba_attention(q, k, v):
    B, H, S, Dh = q.shape
    L = MOBA_BLOCK
    n_blocks = -(-S // L)
    pad = n_blocks * L - S
    kp = jnp.pad(k, ((0, 0), (0, 0), (0, pad), (0, 0)))
    vp = jnp.pad(v, ((0, 0), (0, 0), (0, pad), (0, 0)))
    k_blk = kp.reshape(B, H, n_blocks, L, Dh)
    v_blk = vp.reshape(B, H, n_blocks, L, Dh)
    k_mean = jnp.mean(k_blk.astype(jnp.float32), axis=3)
    topk = min(MOBA_TOPK, n_blocks)
    scale = Dh ** -0.5
    slopes = alibi_slopes(H)
    n_chunks = S // Q_CHUNK
    q_chunks = q.reshape(B, H, n_chunks, Q_CHUNK, Dh).transpose(2, 0, 1, 3, 4)
    gather = jax.vmap(jax.vmap(lambda blocks, idx: blocks[idx]))
    neg_inf = -jnp.inf

    def chunk_fn(args):
        c, qc = args
        q0 = c * Q_CHUNK
        blk = q0 // L
        q_pos = q0 + jnp.arange(Q_CHUNK)
        gate = jnp.einsum('bhqd,bhnd->bhqn', qc.astype(jnp.float32), k_mean)
        gate = jnp.where(jnp.arange(n_blocks) < blk, gate, neg_inf)
        _, sel = lax.top_k(gate, topk)
        sel_valid = jnp.arange(topk) < blk
        k_sel = gather(k_blk, sel)
        v_sel = gather(v_blk, sel)
        s_sel = jnp.einsum('bhqd,bhqrld->bhqrl', qc, k_sel,
                           preferred_element_type=jnp.float32) * scale
        k_pos_sel = sel[..., None] * L + jnp.arange(L)
        dist_sel = jnp.abs(q_pos[None, None, :, None, None] - k_pos_sel).astype(jnp.float32)
        s_sel = jnp.where(sel_valid[:, None],
                          s_sel - slopes[None, :, None, None, None] * dist_sel, neg_inf)
        k_own = lax.dynamic_slice_in_dim(kp, blk * L, L, axis=2)
        v_own = lax.dynamic_slice_in_dim(vp, blk * L, L, axis=2)
        s_own = jnp.einsum('bhqd,bhld->bhql', qc, k_own,
                           preferred_element_type=jnp.float32) * scale
        k_pos_own = blk * L + jnp.arange(L)
        dist_own = jnp.abs(q_pos[:, None] - k_pos_own[None, :]).astype(jnp.float32)
        causal = k_pos_own[None, :] <= q_pos[:, None]
        s_own = jnp.where(causal, s_own - slopes[:, None, None] * dist_own, neg_inf)
        s = jnp.concatenate([s_sel.reshape(B, H, Q_CHUNK, topk * L), s_own], axis=-1)
        p = jax.nn.softmax(s, axis=-1)
        p_sel = p[..., :topk * L].reshape(B, H, Q_CHUNK, topk, L).astype(v.dtype)
        p_own = p[..., topk * L:].astype(v.dtype)
        out = (jnp.einsum('bhqrl,bhqrld->bhqd', p_sel, v_sel, preferred_element_type=jnp.float32)
               + jnp.einsum('bhql,bhld->bhqd', p_own, v_own, preferred_element_type=jnp.float32))
        return out.astype(qc.dtype)

    out = lax.map(chunk_fn, (jnp.arange(n_chunks), q_chunks))
    return out.transpose(1, 0, 3, 2, 4).reshape(B, S, H * Dh)


def conformer_conv(u, b_glu, w_dw, b_dw, ln_g, ln_b):
    u = u + b_glu
    val, gt = jnp.split(u, 2, axis=-1)
    h = val * jax.nn.sigmoid(gt)
    C = h.shape[-1]
    rhs = w_dw.reshape(CONV_WIDTH, 1, C)
    y = lax.conv_general_dilated(h, rhs, window_strides=(1,),
                                 padding=[(CONV_WIDTH - 1, 0)],
                                 dimension_numbers=('NWC', 'WIO', 'NWC'),
                                 feature_group_count=C)
    y = layer_norm(y + b_dw, ln_g, ln_b)
    return jax.nn.silu(y)


def setup_inputs(seed: int = 0) -> dict:
    key = jax.random.key(seed)
    ks = jax.random.split(key, 14)
    f32 = jnp.float32

    def nrm(k, shape, scale):
        return jax.random.normal(k, shape, f32) * scale

    def gain(k, shape):
        return 1.0 + 0.02 * jax.random.normal(k, shape, f32)

    return {
        'x': nrm(ks[0], (BATCH, SEQ, D_MODEL), 1.0),
        'g_mix_pre': gain(ks[1], (DEPTH, D_MODEL)),
        'w_in': nrm(ks[2], (DEPTH, D_MODEL, IN_COLS), D_MODEL ** -0.5),
        'b_glu': nrm(ks[3], (DEPTH, 2 * CONV_CHANNELS), 0.02),
        'w_dw': nrm(ks[4], (DEPTH, CONV_WIDTH, CONV_CHANNELS), CONV_WIDTH ** -0.5),
        'b_dw': nrm(ks[5], (DEPTH, CONV_CHANNELS), 0.02),
        'ln_conv_g': gain(ks[6], (DEPTH, CONV_CHANNELS)),
        'ln_conv_b': nrm(ks[7], (DEPTH, CONV_CHANNELS), 0.02),
        'w_out': nrm(ks[8], (DEPTH, MIX_WIDTH, D_MODEL), MIX_WIDTH ** -0.5),
        'g_mix_post': gain(ks[9], (DEPTH, D_MODEL)),
        'g_ffn_pre': gain(ks[10], (DEPTH, D_MODEL)),
        'w_ff1': nrm(ks[11], (DEPTH, D_MODEL, D_FF), D_MODEL ** -0.5),
        'w_ff2': nrm(ks[12], (DEPTH, D_FF, D_MODEL), D_FF ** -0.5),
        'g_ffn_post': gain(ks[13], (DEPTH, D_MODEL)),
    }


def reference(x, g_mix_pre, w_in, b_glu, w_dw, b_dw, ln_conv_g, ln_conv_b, w_out,
              g_mix_post, g_ffn_pre, w_ff1, w_ff2, g_ffn_post):
    B, S, _ = x.shape
    h = x
    for l in range(DEPTH):
        a = rms_norm(h, g_mix_pre[l])
        proj = a @ w_in[l]
        q = proj[..., :ATTN_WIDTH]
        k = proj[..., ATTN_WIDTH:2 * ATTN_WIDTH]
        v = proj[..., 2 * ATTN_WIDTH:3 * ATTN_WIDTH]
        u = proj[..., 3 * ATTN_WIDTH:]
        to_heads = lambda t: t.reshape(B, S, N_ATTN_HEADS, HEAD_DIM).transpose(0, 2, 1, 3)
        attn = moba_attention(to_heads(q), to_heads(k), to_heads(v))
        conv = conformer_conv(u, b_glu[l], w_dw[l], b_dw[l], ln_conv_g[l], ln_conv_b[l])
        mixed = jnp.concatenate([attn, conv], axis=-1) @ w_out[l]
        h = h + rms_norm(mixed, g_mix_post[l])
        f = rms_norm(h, g_ffn_pre[l])
        f = jnp.square(jax.nn.relu(f @ w_ff1[l])) @ w_ff2[l]
        h = h + rms_norm(f, g_ffn_post[l])
    return h
```

```python
import contextlib
import numpy as np
import ml_dtypes
import concourse.bass as bass
import concourse.mybir as mybir
from concourse.bass_utils import run_bass_kernel_spmd

F32 = mybir.dt.float32
BF16 = mybir.dt.bfloat16
ALU = mybir.AluOpType
AF = mybir.ActivationFunctionType
AX = mybir.AxisListType

P = 128
D = 2048
KC = 16
S = 8192
NB = 32
L = 256
H = 8
DH = 128
C = 1024
CC = 8
DFF = 8192
INC = 5120
NSLOT = 8
HALO = 32
SLOTW = L + HALO
NOWN = NSLOT * L
NOWNH = NSLOT * SLOTW
SCALE = DH ** -0.5
RMS_EPS = 1e-6
LN_EPS = 1e-5
CW = 31

STREAMS = ("pe", "act", "dve", "pool", "sp")


class Buf:
    __slots__ = ("name", "writer", "readers", "inherit", "excl")

    def __init__(self, name, inherit=(), excl=False):
        self.name = name
        self.excl = excl
        self.writer = None
        self.readers = []
        self.inherit = list(inherit)


class Op:
    __slots__ = ("stream", "emit", "deps", "is_dma", "semkey", "signal", "name")

    def __init__(self, stream, emit, is_dma=False, semkey=None, name=""):
        self.stream = stream
        self.emit = emit
        self.deps = []
        self.is_dma = is_dma
        self.semkey = semkey
        self.signal = False
        self.name = name


class Prog:
    def __init__(self, nc):
        self.nc = nc
        self.ops = {s: [] for s in STREAMS}
        self.all_ops = []
        self.final_waits = []

    def _add(self, op, reads, writes):
        deps = []
        for b in reads:
            if b.writer is not None:
                deps.append(b.writer)
            elif b.inherit:
                deps.extend(b.inherit)
            if b.excl:
                deps.extend(r for r in b.readers if r.stream != op.stream)
        for b in writes:
            if b.writer is not None:
                deps.append(b.writer)
            deps.extend(b.readers)
            if b.inherit:
                deps.extend(b.inherit)
                b.inherit = []
        seen = set()
        for d in deps:
            if d is op or id(d) in seen:
                continue
            seen.add(id(d))
            op.deps.append(d)
        for b in reads:
            b.readers.append(op)
        for b in writes:
            b.writer = op
            b.readers = []
        self.ops[op.stream].append(op)
        self.all_ops.append(op)
        return op

    def op(self, stream, emit, reads=(), writes=(), name=""):
        return self._add(Op(stream, emit, name=name), reads, writes)

    def dma(self, stream, out, in_, semkey, reads=(), writes=(), name=""):
        def emit(eng):
            return eng.dma_start(out=out, in_=in_)
        return self._add(Op(stream, emit, is_dma=True, semkey=semkey, name=name), reads, writes)

    def must_finish(self, op):
        self.final_waits.append(op)

    def emit_all(self, stack):
        nc = self.nc
        for op in self.all_ops:
            for d in op.deps:
                d.signal = True
        for op in self.final_waits:
            op.signal = True
        eng_sem = {s: stack.enter_context(nc.semaphore("done_" + s)) for s in STREAMS}
        dma_sems = {}
        dma_cnt = {}
        cnt = {s: 0 for s in STREAMS}
        comp = {}
        for op in self.all_ops:
            s = op.stream
            if op.is_dma:
                k = op.semkey
                if k not in dma_sems:
                    dma_sems[k] = stack.enter_context(nc.semaphore("dq_%d" % len(dma_sems)))
                    dma_cnt[k] = 0
                dma_cnt[k] += 16
                comp[id(op)] = (dma_sems[k], dma_cnt[k])
            elif op.signal:
                cnt[s] += 1
                comp[id(op)] = (eng_sem[s], cnt[s])
        self.n_sems = len(dma_sems) + len(STREAMS)
        self.max_count = dict(cnt)
        block = stack.enter_context(nc.Block())
        prog = self

        def run_stream(s, eng):
            waited = {}
            for op in prog.ops[s]:
                need = {}
                for d in op.deps:
                    sem, val = comp[id(d)]
                    key = id(sem)
                    if waited.get(key, 0) >= val:
                        continue
                    if key not in need or need[key][1] < val:
                        need[key] = (sem, val)
                for key, (sem, val) in need.items():
                    waited[key] = val
                    eng.wait_ge(sem, val)
                ins = op.emit(eng)
                if op.is_dma:
                    ins.then_inc(comp[id(op)][0], 16)
                elif op.signal:
                    ins.then_inc(eng_sem[s], 1)
            if s == "sp":
                for op in prog.final_waits:
                    sem, val = comp[id(op)]
                    eng.wait_ge(sem, val)

        @block.tensor
        def _(e):
            run_stream("pe", e)

        @block.scalar
        def _(e):
            run_stream("act", e)

        @block.vector
        def _(e):
            run_stream("dve", e)

        @block.gpsimd
        def _(e):
            run_stream("pool", e)

        @block.sync
        def _(e):
            run_stream("sp", e)


DT_SIZE = {F32: 4, BF16: 2}


class Arena:
    def __init__(self, nc, stack, kib):
        self.words = kib * 256
        self.t = stack.enter_context(nc.sbuf_tensor("arena", [P, self.words], F32))
        self.top = 0
        self.peak = 0
        self.retired = []
        self.live = []

    def alloc(self, name, shape, dtype):
        n = 1
        for s in shape:
            n *= s
        nbytes = (n * DT_SIZE[dtype] + 31) // 32 * 32
        lo = self.top
        hi = lo + nbytes
        assert hi <= self.words * 4, "SBUF arena overflow at %s: %d > %d" % (name, hi, self.words * 4)
        self.top = hi
        self.peak = max(self.peak, hi)
        inh = []
        keep = []
        for (l, h, ops) in self.retired:
            if l < hi and h > lo:
                inh.extend(ops)
                if l >= lo and h <= hi:
                    continue
            keep.append((l, h, ops))
        self.retired = keep
        buf = Buf(name, inherit=inh)
        v = self.t[:, lo // 4:hi // 4]
        if dtype != F32:
            v = v.bitcast(dtype)
        v = v[:, 0:n]
        if len(shape) == 2:
            v = v.rearrange("p (a b) -> p a b", a=shape[0], b=shape[1])
        elif len(shape) == 3:
            v = v.rearrange("p (a b c) -> p a b c", a=shape[0], b=shape[1], c=shape[2])
        elif len(shape) == 4:
            v = v.rearrange("p (a b c d) -> p a b c d", a=shape[0], b=shape[1], c=shape[2], d=shape[3])
        self.live.append((lo, hi, buf))
        return v, buf

    def mark(self):
        return self.top

    def release(self, mark):
        keep = []
        for (lo, hi, buf) in self.live:
            if lo >= mark:
                ops = list(buf.readers) + list(buf.inherit)
                if buf.writer is not None:
                    ops.append(buf.writer)
                if ops:
                    self.retired.append((lo, hi, ops))
            else:
                keep.append((lo, hi, buf))
        self.live = keep
        self.top = mark


def own_blocks(j):
    return [8 * (s // 2) + (j if s % 2 == 0 else 7 - j) for s in range(NSLOT)]


PAST = [8 * (s // 2) + (3 if s % 2 == 0 else 7) for s in range(NSLOT)]


def build_nc(debug=False, stop_after="all"):
    nc = bass.Bass("TRN2", target_bir_lowering=False)
    skind = "ExternalOutput" if debug else "Internal"

    def din(name, shape, dt=F32):
        return nc.dram_tensor(name, list(shape), dt, kind="ExternalInput").ap()

    def dscr(name, shape, dt):
        return nc.dram_tensor(name, list(shape), dt, kind=skind).ap()

    xall_d = din("xall", [S, D])
    xown_d = din("xown", [NOWNH, D])
    w_in_d = din("w_in", [D, INC])
    w_out_d = din("w_out", [D, D])
    w_ff1_d = din("w_ff1", [D, DFF])
    w_ff2_d = din("w_ff2", [DFF, D])
    grow_d = din("grow", [4, D])
    cols_d = din("cols", [P, 16 + CC * CW + 3 * CC])
    fd_d = din("fd", [NSLOT, P, 2 * H * NB])
    gbias_d = din("gbias", [P, NSLOT * NB])
    hmask_d = din("hmask", [P, NSLOT])
    bkt_d = din("bkt", [P, H * 2])
    fown_d = din("fown", [P, H])
    tri_d = din("tri", [P, P], BF16)
    ident_d = din("ident", [P, P], BF16)

    y_d = nc.dram_tensor("y", [NOWN, D], F32, kind="ExternalOutput").ap()

    w_in_bf = dscr("w_in_bf", [D, INC], BF16)
    w_out_bf = dscr("w_out_bf", [D, D], BF16)
    w_ff1_bf = dscr("w_ff1_bf", [D, DFF], BF16)
    w_ff2_bf = dscr("w_ff2_bf", [DFF, D], BF16)
    KT_d = dscr("KT", [H, DH, S], BF16)
    V_d = dscr("V", [S, H * DH], BF16)
    KTo_d = dscr("KTo", [H, DH, NOWN], BF16)
    Vo_d = dscr("Vo", [NOWN, H * DH], BF16)
    QT_d = dscr("QT", [H, DH, NOWN], BF16)
    F_d = dscr("Fsel", [H, P, 16 * NB], F32)
    yT_d = dscr("yT", [CC, P, NOWN], F32)
    mixT_d = dscr("mixT", [D, NOWN], BF16)
    kmean_dbg = dscr("kmean_dbg", [P, H * NB], F32) if debug else None

    with contextlib.ExitStack() as st:
        pr = Prog(nc)
        ar = Arena(nc, st, 204)
        psum = []
        pbuf = []
        for i in range(8):
            psum.append(st.enter_context(nc.psum_tensor("ps%d" % i, [P, 512], F32)))
            pbuf.append(Buf("ps%d" % i, excl=True))

        def ps_f32(i, n=512):
            return psum[i][:, 0:n]

        def ps_bf16(i):
            return psum[i][:, :].bitcast(BF16)

        cols, B_cols = ar.alloc("cols", [16 + CC * CW + 3 * CC], F32)
        pr.dma("sp", cols, cols_d, "c0", writes=[B_cols])
        bglu = cols[:, 0:16]
        wdw = cols[:, 16:16 + CC * CW].rearrange("p (c j) -> p c j", c=CC, j=CW)
        o0 = 16 + CC * CW
        bdw = cols[:, o0:o0 + CC]
        lng = cols[:, o0 + CC:o0 + 2 * CC]
        lnb = cols[:, o0 + 2 * CC:o0 + 3 * CC]
        ident, B_ident = ar.alloc("ident", [P], BF16)
        pr.dma("sp", ident, ident_d, "c1", writes=[B_ident])
        tri, B_tri = ar.alloc("tri", [P], BF16)
        pr.dma("sp", tri, tri_d, "c2", writes=[B_tri])
        bkt, B_bkt = ar.alloc("bkt", [H, 2], F32)
        pr.dma("sp", bkt, bkt_d.rearrange("p (h t) -> p h t", h=H, t=2), "c3", writes=[B_bkt])
        fown, B_fown = ar.alloc("fown", [H], F32)
        pr.dma("sp", fown, fown_d, "c4", writes=[B_fown])
        gbias, B_gbias = ar.alloc("gbias", [NSLOT, NB], F32)
        pr.dma("sp", gbias, gbias_d.rearrange("p (s n) -> p s n", s=NSLOT, n=NB), "c5", writes=[B_gbias])
        hmask, B_hmask = ar.alloc("hmask", [NSLOT], F32)
        pr.dma("sp", hmask, hmask_d, "c6", writes=[B_hmask])
        kmean, B_kmean = ar.alloc("kmean", [H, NB], F32)
        ones32, B_ones = ar.alloc("ones32", [P], F32)
        pr.op("pool", lambda e: e.memset(ones32, 1.0), writes=[B_ones])
        ss_t, _ = ar.alloc("ss", [8], F32)
        rs_t, _ = ar.alloc("rs", [8], F32)
        B_ss = [Buf("ss%d" % i) for i in range(8)]
        B_rs = [Buf("rs%d" % i) for i in range(8)]
        stat_ctr = [0]

        def cast_group(name, dst, src, pieces):
            bufs = []
            for i, (dsl, ssl) in enumerate(pieces):
                b = Buf("%s_%d" % (name, i))
                pr.dma("pool", dst[dsl], src[ssl], "cast_" + name, writes=[b])
                bufs.append(b)
            return bufs

        def rows4(c0, c1, nrows=D):
            q = nrows // 4
            return [((slice(i * q, (i + 1) * q), slice(c0, c1)),) * 2 for i in range(4)]

        B_wkv = cast_group("wkv", w_in_bf, w_in_d, rows4(1024, 3072))
        B_wq = cast_group("wq", w_in_bf, w_in_d, rows4(0, 1024))
        B_wu = cast_group("wu", w_in_bf, w_in_d, rows4(3072, 5120))
        B_wout = cast_group("wout", w_out_bf, w_out_d, rows4(0, D))
        B_wff1 = [cast_group("wff1_%d" % g, w_ff1_bf, w_ff1_d, rows4(g * 2048, (g + 1) * 2048)) for g in range(4)]
        B_wff2 = []
        for g in range(4):
            pcs = [((slice(g * 2048 + i * 512, g * 2048 + (i + 1) * 512), slice(0, D)),) * 2 for i in range(4)]
            B_wff2.append(cast_group("wff2_%d" % g, w_ff2_bf, w_ff2_d, pcs))

        def in_col_bufs(c0):
            if c0 < 1024:
                return B_wq
            if c0 < 3072:
                return B_wkv
            return B_wu

        def norm_T_tile(x_src, xs, B_xs, xs_key, g_b, B_gb, abf, B_abf, dstT, B_dstT, col0, tbanks):
            i = stat_ctr[0] % 8
            stat_ctr[0] += 1
            ssv = ss_t[:, i:i + 1]
            rsv = rs_t[:, i:i + 1]
            if x_src is not None:
                pr.dma("sp", xs, x_src, xs_key, writes=[B_xs])
            pr.op("act", lambda e: e.activation(out=abf, in_=xs, func=AF.Square, accum_out=ssv),
                  reads=[B_xs], writes=[B_ss[i], B_abf])
            pr.op("act", lambda e: e.activation(out=rsv, in_=ssv, func=AF.Sqrt, bias=RMS_EPS, scale=1.0 / D),
                  reads=[B_ss[i]], writes=[B_rs[i]])
            pr.op("dve", lambda e: e.reciprocal(out=rsv, in_=rsv), reads=[B_rs[i]], writes=[B_rs[i]])
            pr.op("dve", lambda e: e.scalar_tensor_tensor(out=abf, in0=xs, scalar=rsv, in1=g_b,
                                                          op0=ALU.mult, op1=ALU.mult),
                  reads=[B_xs, B_rs[i], B_gb], writes=[B_abf])
            for half in range(2):
                bank = tbanks[half]
                pt = ps_bf16(bank).rearrange("p (k n) -> p k n", k=8, n=P)

                def tr(e, half=half, pt=pt):
                    ins = None
                    for kk in range(8):
                        k = half * 8 + kk
                        ins = e.transpose(out=pt[:, kk, :], in_=abf[:, k * P:(k + 1) * P], identity=ident)
                    return ins
                pr.op("pe", tr, reads=[B_abf, B_ident], writes=[pbuf[bank]])
                dst = dstT[:, half * 8:(half + 1) * 8, col0:col0 + P]
                if half == 0:
                    pr.op("act", lambda e, dst=dst, pt=pt: e.copy(out=dst, in_=pt), reads=[pbuf[bank]], writes=[B_dstT])
                else:
                    pr.op("dve", lambda e, dst=dst, pt=pt: e.tensor_copy(out=dst, in_=pt), reads=[pbuf[bank]],
                          writes=[B_dstT])

        def mm_group(out_ap, pairs, reads, writes, name=""):
            def emit(e):
                ins = None
                n = len(pairs)
                for i, (l, r) in enumerate(pairs):
                    ins = e.matmul(out_ap, lhsT=l, rhs=r, start=(i == 0), stop=(i == n - 1))
                return ins
            return pr.op("pe", emit, reads=reads, writes=writes, name=name)

        mA = ar.mark()
        gpre_b, B_gpre = ar.alloc("gpre_b", [D], F32)
        pr.dma("sp", gpre_b, grow_d[0:1, :].partition_broadcast(P), "c7", writes=[B_gpre])
        wkv, _ = ar.alloc("wkv", [KC, 2048], BF16)
        B_wkvS = [Buf("wkvS%d" % i) for i in range(4)]
        wsrc = w_in_bf[:, 1024:3072].rearrange("(k p) n -> p k n", p=P)
        for i in range(4):
            pr.dma("sp", wkv[:, 4 * i:4 * i + 4, :], wsrc[:, 4 * i:4 * i + 4, :], "wkvS%d" % i, reads=B_wkv,
                   writes=[B_wkvS[i]])
        xsA = [ar.alloc("xsA%d" % i, [D], F32) for i in range(2)]
        abA = [ar.alloc("abA%d" % i, [D], BF16) for i in range(2)]
        aTA = [ar.alloc("aTA%d" % i, [KC, 512], BF16) for i in range(2)]
        ktst = [ar.alloc("ktst%d" % i, [H, 512], BF16) for i in range(2)]
        vst = [ar.alloc("vst%d" % i, [1024], BF16) for i in range(2)]
        kmsum, B_kmsum = ar.alloc("kmsum", [H, NB], F32)
        NG = S // 512
        KT_v = KT_d.rearrange("h d t -> d h t")
        TB = (0, 1)
        KB = (2, 3)
        VB = (4, 5)
        tile_ctr = [0]

        def A_norm(g):
            aT, B_aT = aTA[g % 2]
            for t in range(4):
                i = tile_ctr[0] % 2
                tile_ctr[0] += 1
                r0 = g * 512 + t * P
                norm_T_tile(xall_d[r0:r0 + P, :], xsA[i][0], xsA[i][1], "xsA%d" % i, gpre_b, B_gpre,
                            abA[i][0], abA[i][1], aT, B_aT, t * P, TB)

        def A_kv(g):
            aT, B_aT = aTA[g % 2]
            kst, B_kst = ktst[g % 2]
            for h in range(H):
                bank = KB[h % 2]
                pk = ps_f32(bank)
                mm_group(pk, [(wkv[:, k, h * DH:(h + 1) * DH], aT[:, k, :]) for k in range(KC)],
                         reads=[B_aT] + B_wkvS, writes=[pbuf[bank]])
                pr.op("act", lambda e, pk=pk, h=h: e.copy(out=kst[:, h, :], in_=pk), reads=[pbuf[bank]], writes=[B_kst])
                pr.op("dve", lambda e, h=h: e.tensor_reduce(
                    out=kmsum[:, h, 2 * g:2 * g + 2], in_=kst[:, h, :].rearrange("p (a b) -> p a b", a=2, b=L),
                    axis=AX.X, op=ALU.add), reads=[B_kst], writes=[B_kmsum])
            pr.dma("sp", KT_v[:, :, g * 512:(g + 1) * 512], kst, "ktst%d" % (g % 2), reads=[B_kst], writes=[B_KT])
            for t in range(4):
                vi = (g * 4 + t) % 2
                vs, B_vs = vst[vi]
                for half in range(2):
                    bank = VB[half]
                    pv = ps_f32(bank)
                    mm_group(pv, [(aT[:, k, t * P:(t + 1) * P], wkv[:, k, 1024 + half * 512:1024 + (half + 1) * 512])
                                  for k in range(KC)], reads=[B_aT] + B_wkvS, writes=[pbuf[bank]])
                    if half == 0:
                        pr.op("dve", lambda e, pv=pv, vs=vs: e.tensor_copy(out=vs[:, 0:512], in_=pv),
                              reads=[pbuf[bank]], writes=[B_vs])
                    else:
                        pr.op("act", lambda e, pv=pv, vs=vs: e.copy(out=vs[:, 512:1024], in_=pv),
                              reads=[pbuf[bank]], writes=[B_vs])
                r0 = g * 512 + t * P
                pr.dma("sp", V_d[r0:r0 + P, :], vs, "vst%d" % vi, reads=[B_vs], writes=[B_V])

        B_KT = Buf("KT_d")
        B_V = Buf("V_d")
        A_norm(0)
        for g in range(NG):
            if g + 1 < NG:
                A_norm(g + 1)
            A_kv(g)
        pr.op("dve", lambda e: e.tensor_scalar(out=kmean, in0=kmsum, scalar1=1.0 / L, scalar2=None, op0=ALU.mult),
              reads=[B_kmsum], writes=[B_kmean])
        if debug:
            o = pr.dma("sp", kmean_dbg, kmean.rearrange("p h n -> p (h n)"), "dbg0", reads=[B_kmean])
            pr.must_finish(o)
        ar.release(mA)

        last_ops = []
        if stop_after == "A":
            return _finish(nc, pr, st, ar, [B_KT, B_V])

        mB = ar.mark()
        gpre_b, B_gpre = ar.alloc("gpre_b2", [D], F32)
        pr.dma("sp", gpre_b, grow_d[0:1, :].partition_broadcast(P), "c7", writes=[B_gpre])
        aTo, B_aTo = ar.alloc("aTo", [KC, NOWNH], BF16)
        Fall, B_Fall = ar.alloc("Fall", [16, H, NB], F32)
        mB1 = ar.mark()
        xsB = [ar.alloc("xsB%d" % i, [D], F32) for i in range(2)]
        abB = [ar.alloc("abB%d" % i, [D], BF16) for i in range(2)]
        for t in range(NOWNH // P):
            i = t % 2
            norm_T_tile(xown_d[t * P:(t + 1) * P, :], xsB[i][0], xsB[i][1], "xsA%d" % i, gpre_b, B_gpre,
                        abB[i][0], abB[i][1], aTo, B_aTo, t * P, TB)
        ar.release(mB1)
        NWS = 3
        wch = [ar.alloc("wch%d" % i, [KC, 512], BF16) for i in range(NWS)]
        wch_ctr = [0]

        def load_wchunk(src_ap, reads):
            i = wch_ctr[0] % NWS
            wch_ctr[0] += 1
            w, B_w = wch[i]
            pr.dma("sp", w, src_ap, "wch%d" % i, reads=reads, writes=[B_w])
            return w, B_w

        def in_chunk_src(c0):
            return w_in_bf[:, c0:c0 + 512].rearrange("(k p) n -> p k n", p=P)

        qst = [ar.alloc("qst%d" % i, [L], BF16) for i in range(2)]
        q32 = [ar.alloc("q32_%d" % i, [L], F32) for i in range(2)]
        kost = [ar.alloc("kost%d" % i, [L], BF16) for i in range(2)]
        vost = [ar.alloc("vost%d" % i, [512], BF16) for i in range(2)]
        fdt = [ar.alloc("fdt%d" % i, [2, H, NB], F32) for i in range(2)]
        gsb = [ar.alloc("gsb%d" % i, [NB], F32) for i in range(2)]
        top8 = [ar.alloc("top8_%d" % i, [8], F32) for i in range(2)]
        sig = [ar.alloc("sig%d" % i, [SLOTW], F32) for i in range(2)]
        hst = [ar.alloc("hst%d" % i, [SLOTW], F32) for i in range(2)]
        accP = [ar.alloc("accP%d" % i, [L], F32) for i in range(2)]
        accD = [ar.alloc("accD%d" % i, [L], F32) for i in range(2)]
        B_QT = Buf("QT_d")
        B_KTo = Buf("KTo_d")
        B_Vo = Buf("Vo_d")
        B_yT = Buf("yT_d")
        B_F = Buf("F_d")
        PB = (2, 3, 4, 5)
        GB = 6
        pb_ctr = [0]

        def nbank():
            b = PB[pb_ctr[0] % len(PB)]
            pb_ctr[0] += 1
            return b
        ctr = {"q": 0, "k": 0, "v": 0, "u": 0, "g": 0}

        chunk_list = [("q", 0), ("q", 512), ("k", 1024), ("k", 1536), ("v", 2048), ("v", 2560),
                      ("uv", 3072), ("ug", 4096), ("uv", 3584), ("ug", 4608)]
        pending = None
        nxt = load_wchunk(in_chunk_src(chunk_list[0][1]), in_col_bufs(chunk_list[0][1]))
        for ci, (kind, c0) in enumerate(chunk_list):
            w, B_w = nxt
            if ci + 1 < len(chunk_list):
                nxt = load_wchunk(in_chunk_src(chunk_list[ci + 1][1]), in_col_bufs(chunk_list[ci + 1][1]))
            if kind in ("q", "k"):
                for s in range(NSLOT):
                    if kind == "q":
                        fi = ctr["g"] % 2
                        ctr["g"] += 1
                        fdv, B_fd = fdt[fi]
                        pr.dma("sp", fdv, fd_d[s].rearrange("p (t h n) -> p t h n", t=2, h=H, n=NB), "fdt%d" % fi,
                               writes=[B_fd])
                    for sub in range(4):
                        h = (c0 % 1024) // DH + sub
                        bank = nbank()
                        pq = ps_f32(bank, SLOTW)
                        mm_group(pq, [(w[:, k, sub * DH:(sub + 1) * DH], aTo[:, k, s * SLOTW:(s + 1) * SLOTW])
                                      for k in range(KC)], reads=[B_aTo, B_w], writes=[pbuf[bank]])
                        if kind == "k":
                            i = ctr["k"] % 2
                            ctr["k"] += 1
                            ks, B_ks = kost[i]
                            pr.op("act", lambda e, ks=ks, pq=pq: e.copy(out=ks, in_=pq[:, HALO:SLOTW]),
                                  reads=[pbuf[bank]], writes=[B_ks])
                            pr.dma("sp", KTo_d[h, :, s * L:(s + 1) * L], ks, "kost%d" % i, reads=[B_ks], writes=[B_KTo])
                            continue
                        i = ctr["q"] % 2
                        ctr["q"] += 1
                        qs, B_qs = qst[i]
                        qf, B_qf = q32[i]
                        pr.op("act", lambda e, qf=qf, pq=pq: e.copy(out=qf, in_=pq[:, HALO:SLOTW]),
                              reads=[pbuf[bank]], writes=[B_qf])
                        pr.op("dve", lambda e, qs=qs, qf=qf: e.tensor_copy(out=qs, in_=qf), reads=[B_qf], writes=[B_qs])
                        pr.dma("sp", QT_d[h, :, s * L:(s + 1) * L], qs, "qst%d" % i, reads=[B_qs], writes=[B_QT])
                        pg = ps_f32(GB, 2 * NB).rearrange("p (t n) -> p t n", t=2, n=NB)

                        def gmm(e, qf=qf, h=h, pg=pg):
                            e.matmul(pg[:, 0, :], lhsT=qf[:, 0:P], rhs=kmean[:, h, :], start=True, stop=True)
                            return e.matmul(pg[:, 1, :], lhsT=qf[:, P:2 * P], rhs=kmean[:, h, :], start=True, stop=True)
                        pr.op("pe", gmm, reads=[B_qf, B_kmean], writes=[pbuf[GB]])
                        for qt in range(2):
                            gi = ctr["u"] % 2
                            ctr["u"] += 1
                            gs, B_gs = gsb[gi]
                            t8, B_t8 = top8[gi]
                            pr.op("dve", lambda e, gs=gs, qt=qt, pg=pg, s=s: e.tensor_tensor(
                                out=gs, in0=pg[:, qt, :], in1=gbias[:, s, :], op=ALU.add),
                                reads=[pbuf[GB], B_gbias], writes=[B_gs])
                            pr.op("dve", lambda e, gs=gs, t8=t8: e.max(out=t8, in_=gs), reads=[B_gs], writes=[B_t8])
                            pr.op("dve", lambda e, gs=gs, t8=t8, qt=qt, h=h, s=s, fdv=fdv: e.scalar_tensor_tensor(
                                out=Fall[:, 2 * s + qt, h, :], in0=gs, scalar=t8[:, 2:3], in1=fdv[:, qt, h, :],
                                op0=ALU.is_ge, op1=ALU.mult), reads=[B_gs, B_t8, B_fd], writes=[B_Fall])
            elif kind == "v":
                for s in range(NSLOT):
                    for t in range(2):
                        bank = nbank()
                        pv = ps_f32(bank)
                        col = s * SLOTW + HALO + t * P
                        mm_group(pv, [(aTo[:, k, col:col + P], w[:, k, :]) for k in range(KC)],
                                 reads=[B_aTo, B_w], writes=[pbuf[bank]])
                        i = ctr["v"] % 2
                        ctr["v"] += 1
                        vs, B_vs = vost[i]
                        pr.op("act", lambda e, vs=vs, pv=pv: e.copy(out=vs, in_=pv), reads=[pbuf[bank]], writes=[B_vs])
                        r0 = s * L + t * P
                        cv = c0 - 2048
                        pr.dma("sp", Vo_d[r0:r0 + P, cv:cv + 512], vs, "vost%d" % i, reads=[B_vs], writes=[B_Vo])
            elif kind == "uv":
                pending = (w, B_w, c0)
            else:
                wv, B_wv, cv0 = pending
                for s in range(NSLOT):
                    for sub in range(4):
                        cch = (cv0 - 3072) // P + sub
                        bv = nbank()
                        bg = nbank()
                        pval = ps_f32(bv, SLOTW)
                        pgt = ps_f32(bg, SLOTW)
                        rhs_sl = slice(s * SLOTW, (s + 1) * SLOTW)
                        mm_group(pval, [(wv[:, k, sub * P:(sub + 1) * P], aTo[:, k, rhs_sl]) for k in range(KC)],
                                 reads=[B_aTo, B_wv], writes=[pbuf[bv]])
                        mm_group(pgt, [(w[:, k, sub * P:(sub + 1) * P], aTo[:, k, rhs_sl]) for k in range(KC)],
                                 reads=[B_aTo, B_w], writes=[pbuf[bg]])
                        i = ctr["u"] % 2
                        ctr["u"] += 1
                        sg, B_sg = sig[i]
                        hs, B_hs = hst[i]
                        aP, B_aP = accP[i]
                        aD, B_aD = accD[i]
                        pr.op("act", lambda e, sg=sg, pgt=pgt, cch=cch: e.activation(
                            out=sg, in_=pgt, func=AF.Sigmoid, bias=bglu[:, 8 + cch:9 + cch], scale=1.0),
                            reads=[pbuf[bg], B_cols], writes=[B_sg])
                        pr.op("dve", lambda e, hs=hs, pval=pval, sg=sg, cch=cch: e.scalar_tensor_tensor(
                            out=hs, in0=pval, scalar=bglu[:, cch:cch + 1], in1=sg, op0=ALU.add, op1=ALU.mult),
                            reads=[pbuf[bv], B_sg, B_cols], writes=[B_hs])
                        pr.op("dve", lambda e, hs=hs, s=s: e.tensor_scalar(
                            out=hs[:, 0:HALO], in0=hs[:, 0:HALO], scalar1=hmask[:, s:s + 1], scalar2=None, op0=ALU.mult),
                            reads=[B_hs, B_hmask], writes=[B_hs])
                        pr.op("dve", lambda e, hs=hs, aD=aD, cch=cch: e.tensor_scalar(
                            out=aD, in0=hs[:, 2:2 + L], scalar1=wdw[:, cch, 0:1], scalar2=bdw[:, cch:cch + 1],
                            op0=ALU.mult, op1=ALU.add), reads=[B_hs, B_cols], writes=[B_aD])
                        for jt in range(1, CW):
                            pr.op("dve", lambda e, hs=hs, aD=aD, cch=cch, jt=jt: e.scalar_tensor_tensor(
                                out=aD, in0=hs[:, 2 + jt:2 + jt + L], scalar=wdw[:, cch, jt:jt + 1], in1=aD,
                                op0=ALU.mult, op1=ALU.add), reads=[B_hs, B_aD], writes=[B_aD])
                        pr.dma("sp", yT_d[cch, :, s * L:(s + 1) * L], aD, "accD%d" % i, reads=[B_aD], writes=[B_yT])
        for h in range(H):
            pr.dma("sp", F_d[h].rearrange("p (q n) -> p q n", q=16, n=NB), Fall[:, :, h, :], "fst", reads=[B_Fall],
                   writes=[B_F])
        ar.release(mB)
        if stop_after == "B":
            return _finish(nc, pr, st, ar, [B_KT, B_V, B_QT, B_KTo, B_Vo, B_yT, B_F])

        B_mixT = Buf("mixT_d")
        mL = ar.mark()
        ysl = [ar.alloc("ysl%d" % i, [CC, L], F32) for i in range(2)]
        sqs = [ar.alloc("sqs%d" % i, [CC, L], F32) for i in range(2)]
        cst = [ar.alloc("cst%d" % i, [CC, L], BF16) for i in range(2)]
        mean_t, B_mean = ar.alloc("mean_t", [L], F32)
        msq_t, B_msq = ar.alloc("msq_t", [L], F32)
        rstd_t, B_rstdL = ar.alloc("rstd_t", [L], F32)
        mr_t, B_mr = ar.alloc("mr_t", [L], F32)
        tmpL = [ar.alloc("tmpL%d" % i, [L], F32) for i in range(2)]
        yT_v = yT_d.rearrange("c p t -> p c t")
        for s in range(NSLOT):
            i = s % 2
            ys, B_ys = ysl[i]
            sq, B_sq = sqs[i]
            cs, B_cs = cst[i]
            pr.dma("sp", ys, yT_v[:, :, s * L:(s + 1) * L], "ysl%d" % i, reads=[B_yT], writes=[B_ys])
            pr.op("act", lambda e, ys=ys, sq=sq: e.activation(out=sq, in_=ys, func=AF.Square), reads=[B_ys], writes=[B_sq])
            pm = ps_f32(2, L)
            pq2 = ps_f32(3, L)
            mm_group(pm, [(ones32, ys[:, c, :]) for c in range(CC)], reads=[B_ones, B_ys], writes=[pbuf[2]])
            mm_group(pq2, [(ones32, sq[:, c, :]) for c in range(CC)], reads=[B_ones, B_sq], writes=[pbuf[3]])
            pr.op("dve", lambda e, pm=pm: e.tensor_scalar(out=mean_t, in0=pm, scalar1=1.0 / C, scalar2=None, op0=ALU.mult),
                  reads=[pbuf[2]], writes=[B_mean])
            pr.op("dve", lambda e: e.tensor_tensor(out=msq_t, in0=mean_t, in1=mean_t, op=ALU.mult),
                  reads=[B_mean], writes=[B_msq])
            pr.op("dve", lambda e, pq2=pq2: e.scalar_tensor_tensor(out=rstd_t, in0=pq2, scalar=1.0 / C, in1=msq_t,
                                                                  op0=ALU.mult, op1=ALU.subtract),
                  reads=[pbuf[3], B_msq], writes=[B_rstdL])
            pr.op("act", lambda e: e.activation(out=rstd_t, in_=rstd_t, func=AF.Sqrt, bias=LN_EPS, scale=1.0),
                  reads=[B_rstdL], writes=[B_rstdL])
            pr.op("dve", lambda e: e.reciprocal(out=rstd_t, in_=rstd_t), reads=[B_rstdL], writes=[B_rstdL])
            pr.op("dve", lambda e: e.tensor_tensor(out=mr_t, in0=mean_t, in1=rstd_t, op=ALU.mult),
                  reads=[B_mean, B_rstdL], writes=[B_mr])
            for c in range(CC):
                tl, B_tl = tmpL[c % 2]
                pr.op("dve", lambda e, tl=tl, ys=ys, c=c: e.tensor_tensor(out=tl, in0=ys[:, c, :], in1=rstd_t, op=ALU.mult),
                      reads=[B_ys, B_rstdL], writes=[B_tl])
                pr.op("dve", lambda e, tl=tl: e.tensor_tensor(out=tl, in0=tl, in1=mr_t, op=ALU.subtract),
                      reads=[B_tl, B_mr], writes=[B_tl])
                pr.op("act", lambda e, tl=tl, cs=cs, c=c: e.activation(out=cs[:, c, :], in_=tl, func=AF.Silu,
                                                                     bias=lnb[:, c:c + 1], scale=lng[:, c:c + 1]),
                      reads=[B_tl, B_cols], writes=[B_cs])
            pr.dma("sp", mixT_d[C:2 * C, s * L:(s + 1) * L].rearrange("(c p) t -> p c t", p=P), cs, "cst%d" % i,
                   reads=[B_cs], writes=[B_mixT])
        ar.release(mL)
        if stop_after == "L":
            return _finish(nc, pr, st, ar, [B_KT, B_V, B_QT, B_KTo, B_Vo, B_yT, B_F, B_mixT])

        mT = ar.mark()
        NT = S // P
        VW = DH + 2
        KTh = [ar.alloc("KTh%d" % i, [S], BF16) for i in range(2)]
        Vh = [ar.alloc("Vh%d" % i, [NT, VW], BF16) for i in range(2)]
        KToh = [ar.alloc("KToh%d" % i, [NOWN], BF16) for i in range(2)]
        Voh = [ar.alloc("Voh%d" % i, [NOWN // P, VW], BF16) for i in range(2)]
        QTh = [ar.alloc("QTh%d" % i, [NOWN], BF16) for i in range(2)]
        Fh = [ar.alloc("Fh%d" % i, [16, NB], F32) for i in range(2)]
        pTs = [ar.alloc("pT%d" % i, [2, L], BF16) for i in range(3)]
        pTo = [ar.alloc("pTo%d" % i, [3 * P], BF16) for i in range(2)]
        accs = [ar.alloc("acc%d" % i, [2, VW], F32) for i in range(2)]
        rcs = [ar.alloc("rc%d" % i, [2], F32) for i in range(2)]
        obf = [ar.alloc("obf%d" % i, [2, DH], BF16) for i in range(2)]
        ast = [ar.alloc("ast%d" % i, [L], BF16) for i in range(2)]
        for i in range(2):
            pr.op("pool", lambda e, i=i: e.memset(Vh[i][0][:, :, DH:VW], 1.0), writes=[Vh[i][1]])
            pr.op("pool", lambda e, i=i: e.memset(Voh[i][0][:, :, DH:VW], 1.0), writes=[Voh[i][1]])
        SBK = (0, 1, 2)
        OBK = (3, 4, 5)
        TBK = 6

        def T_load(h):
            i = h % 2
            pr.dma("sp", KTh[i][0], KT_d[h], "KTh%d" % i, reads=[B_KT], writes=[KTh[i][1]])
            vsrc = V_d[:, h * DH:(h + 1) * DH].rearrange("(t p) d -> p t d", p=P)
            for q4 in range(4):
                pr.dma("sp", Vh[i][0][:, q4 * 16:(q4 + 1) * 16, 0:DH], vsrc[:, q4 * 16:(q4 + 1) * 16, :], "Vh%d_%d" % (i, q4),
                       reads=[B_V], writes=[Vh[i][1]])
            pr.dma("sp", KToh[i][0], KTo_d[h], "KToh%d" % i, reads=[B_KTo], writes=[KToh[i][1]])
            pr.dma("sp", Voh[i][0][:, :, 0:DH], Vo_d[:, h * DH:(h + 1) * DH].rearrange("(t p) d -> p t d", p=P),
                   "Voh%d" % i, reads=[B_Vo], writes=[Voh[i][1]])
            pr.dma("sp", QTh[i][0], QT_d[h], "QTh%d" % i, reads=[B_QT], writes=[QTh[i][1]])
            pr.dma("sp", Fh[i][0], F_d[h].rearrange("p (q n) -> p q n", q=16, n=NB), "Fh%d" % i, reads=[B_F],
                   writes=[Fh[i][1]])

        uctr = [0]

        def T_head(h):
            i = h % 2
            kth, B_kth = KTh[i]
            vh, B_vh = Vh[i]
            kto, B_kto = KToh[i]
            voh, B_voh = Voh[i]
            qth, B_qth = QTh[i]
            fh, B_fh = Fh[i]
            units = []
            for s in range(NSLOT):
                units.append((s, -1))
                for n in range(PAST[s]):
                    units.append((s, n))
            state = {}
            deferred = []

            def emit_qk(idx):
                s, n = units[idx]
                u = uctr[0]
                uctr[0] += 1
                sb = SBK[u % 3]
                qsl = slice(s * L, (s + 1) * L)
                if n >= 0:
                    sv = ps_f32(sb).rearrange("p (t q) -> p t q", t=2, q=L)

                    def qk(e, sv=sv, n=n, qsl=qsl):
                        e.matmul(sv[:, 0, :], lhsT=kth[:, (2 * n) * P:(2 * n + 1) * P], rhs=qth[:, qsl], start=True, stop=True)
                        return e.matmul(sv[:, 1, :], lhsT=kth[:, (2 * n + 1) * P:(2 * n + 2) * P], rhs=qth[:, qsl],
                                        start=True, stop=True)
                    pr.op("pe", qk, reads=[B_kth, B_qth], writes=[pbuf[sb]])
                    pt, B_pt = pTs[u % 3]
                    for t in range(2):
                        pr.op("act", lambda e, pt=pt, sv=sv, t=t: e.activation(
                            out=pt[:, t, :], in_=sv[:, t, :], func=AF.Exp, bias=bkt[:, h, t:t + 1], scale=SCALE),
                            reads=[pbuf[sb], B_bkt], writes=[B_pt])
                    state[idx] = (u, pt, B_pt)
                else:
                    sv = ps_f32(sb, 3 * P)
                    q0 = s * L

                    def qk(e, sv=sv, q0=q0):
                        e.matmul(sv[:, 0:P], lhsT=kto[:, q0:q0 + P], rhs=qth[:, q0:q0 + P], start=True, stop=True)
                        e.matmul(sv[:, P:2 * P], lhsT=kto[:, q0 + P:q0 + 2 * P], rhs=qth[:, q0 + P:q0 + 2 * P],
                                 start=True, stop=True)
                        return e.matmul(sv[:, 2 * P:3 * P], lhsT=kto[:, q0:q0 + P], rhs=qth[:, q0 + P:q0 + 2 * P],
                                        start=True, stop=True)
                    pr.op("pe", qk, reads=[B_kto, B_qth], writes=[pbuf[sb]])
                    pt, B_pt = pTo[s % 2]
                    pr.op("act", lambda e, pt=pt, sv=sv: e.activation(
                        out=pt[:, 0:2 * P], in_=sv[:, 0:2 * P], func=AF.Exp, bias=bkt[:, h, 1:2], scale=SCALE),
                        reads=[pbuf[sb], B_bkt], writes=[B_pt])
                    pr.op("act", lambda e, pt=pt, sv=sv: e.activation(
                        out=pt[:, 2 * P:3 * P], in_=sv[:, 2 * P:3 * P], func=AF.Exp, bias=bkt[:, h, 0:1], scale=SCALE),
                        reads=[pbuf[sb], B_bkt], writes=[B_pt])
                    for a in range(2):
                        pr.op("pool", lambda e, pt=pt, a=a: e.tensor_tensor(
                            out=pt[:, a * P:(a + 1) * P], in0=pt[:, a * P:(a + 1) * P], in1=tri, op=ALU.mult),
                            reads=[B_pt, B_tri], writes=[B_pt])
                    state[idx] = (u, pt, B_pt)

            def emit_pv(idx):
                s, n = units[idx]
                u, pt, B_pt = state.pop(idx)
                ob = OBK[u % 3]
                ov = ps_f32(ob, 2 * VW).rearrange("p (t d) -> p t d", t=2, d=VW)
                acc, B_acc = accs[s % 2]
                NV = DH + 1
                if n >= 0:
                    def pv(e, ov=ov, pt=pt, n=n):
                        ins = None
                        for qt in range(2):
                            for t in range(2):
                                ins = e.matmul(ov[:, qt, 0:NV], lhsT=pt[:, t, qt * P:(qt + 1) * P],
                                               rhs=vh[:, 2 * n + t, 0:NV], start=(t == 0), stop=(t == 1))
                        return ins
                    pr.op("pe", pv, reads=[B_pt, B_vh], writes=[pbuf[ob]])
                    for qt in range(2):
                        pr.op("dve", lambda e, ov=ov, acc=acc, qt=qt, n=n, s=s: e.scalar_tensor_tensor(
                            out=acc[:, qt, 0:NV], in0=ov[:, qt, 0:NV], scalar=fh[:, 2 * s + qt, n:n + 1],
                            in1=acc[:, qt, 0:NV], op0=ALU.mult, op1=ALU.add),
                            reads=[pbuf[ob], B_fh, B_acc], writes=[B_acc])
                else:
                    def pv(e, ov=ov, pt=pt, s=s):
                        e.matmul(ov[:, 0, 0:NV], lhsT=pt[:, 0:P], rhs=voh[:, 2 * s, 0:NV], start=True, stop=True)
                        e.matmul(ov[:, 1, 0:NV], lhsT=pt[:, 2 * P:3 * P], rhs=voh[:, 2 * s, 0:NV], start=True, stop=False)
                        return e.matmul(ov[:, 1, 0:NV], lhsT=pt[:, P:2 * P], rhs=voh[:, 2 * s + 1, 0:NV],
                                        start=False, stop=True)
                    pr.op("pe", pv, reads=[B_pt, B_voh], writes=[pbuf[ob]])
                    for qt in range(2):
                        pr.op("dve", lambda e, ov=ov, acc=acc, qt=qt: e.tensor_scalar(
                            out=acc[:, qt, 0:NV], in0=ov[:, qt, 0:NV], scalar1=fown[:, h:h + 1], scalar2=None,
                            op0=ALU.mult), reads=[pbuf[ob], B_fown], writes=[B_acc])
                last = (idx + 1 == len(units)) or (units[idx + 1][0] != s)
                if last:
                    deferred.append([2, s])

            def finalize(s):
                acc, B_acc = accs[s % 2]
                rc, B_rc = rcs[s % 2]
                ob_, B_ob = obf[s % 2]
                asv, B_as = ast[s % 2]
                pr.op("dve", lambda e: e.reciprocal(out=rc, in_=acc[:, :, DH]), reads=[B_acc], writes=[B_rc])
                for qt in range(2):
                    pr.op("dve", lambda e, qt=qt: e.tensor_scalar(out=ob_[:, qt, :], in0=acc[:, qt, 0:DH],
                                                                  scalar1=rc[:, qt:qt + 1], scalar2=None, op0=ALU.mult),
                          reads=[B_acc, B_rc], writes=[B_ob])
                ptv = ps_bf16(TBK)[:, 0:L].rearrange("p (t q) -> p t q", t=2, q=P)

                def tr(e):
                    e.transpose(out=ptv[:, 0, :], in_=ob_[:, 0, :], identity=ident)
                    return e.transpose(out=ptv[:, 1, :], in_=ob_[:, 1, :], identity=ident)
                pr.op("pe", tr, reads=[B_ob, B_ident], writes=[pbuf[TBK]])
                pr.op("act", lambda e: e.copy(out=asv, in_=ps_bf16(TBK)[:, 0:L]), reads=[pbuf[TBK]], writes=[B_as])
                pr.dma("sp", mixT_d[h * DH:(h + 1) * DH, s * L:(s + 1) * L], asv, "ast%d" % (s % 2), reads=[B_as],
                       writes=[B_mixT])

            def tick():
                for dd in list(deferred):
                    dd[0] -= 1
                    if dd[0] <= 0:
                        deferred.remove(dd)
                        finalize(dd[1])

            emit_qk(0)
            for idx in range(len(units)):
                if idx + 1 < len(units):
                    emit_qk(idx + 1)
                emit_pv(idx)
                tick()
            for dd in list(deferred):
                finalize(dd[1])
            deferred.clear()

        T_load(0)
        for h in range(H):
            if h + 1 < H:
                T_load(h + 1)
            T_head(h)
        ar.release(mT)
        if stop_after == "T":
            return _finish(nc, pr, st, ar, [B_mixT])

        gb_t = []
        for gi_, nm in ((1, "gpost"), (2, "gffn"), (3, "gfpost")):
            t_, b_ = ar.alloc(nm, [D], F32)
            pr.dma("sp", t_, grow_d[gi_:gi_ + 1, :].partition_broadcast(P), "c7_%d" % gi_, writes=[b_])
            gb_t.append((t_, b_))
        (gpost_b, B_gpost), (gffn_b, B_gffn), (gfpost_b, B_gfpost) = gb_t
        mixS, B_mixS = ar.alloc("mixS", [KC, 512], BF16)
        fT, B_fT = ar.alloc("fT", [KC, 512], BF16)
        accF, _ = ar.alloc("accF", [4, D], F32)
        B_accF = [Buf("accF%d" % t) for t in range(4)]
        hTs = [ar.alloc("hT%d" % i, [4, 512], BF16) for i in range(2)]
        rts = [ar.alloc("rt%d" % i, [512], F32) for i in range(2)]
        NWS2 = 4
        wc2 = [ar.alloc("wc2_%d" % i, [KC, 512], BF16) for i in range(NWS2)]
        xsC = [ar.alloc("xsC%d" % i, [D], F32) for i in range(2)]
        abC = [ar.alloc("abC%d" % i, [D], BF16) for i in range(2)]
        w2ctr = [0]

        def load_w2(src_ap, reads):
            i = w2ctr[0] % NWS2
            w2ctr[0] += 1
            w, B_w = wc2[i]
            pr.dma("sp", w, src_ap, "wch%d" % i, reads=reads, writes=[B_w])
            return w, B_w
        B_y = [Buf("y%d" % t) for t in range(NOWN // P)]
        CB = (2, 3)
        HBK = (4, 5)
        cb_ctr = [0]
        xc_ctr = [0]

        def rstd_of(src, B_src, junk, B_junk):
            i = stat_ctr[0] % 8
            stat_ctr[0] += 1
            ssv = ss_t[:, i:i + 1]
            rsv = rs_t[:, i:i + 1]
            pr.op("act", lambda e: e.activation(out=junk, in_=src, func=AF.Square, accum_out=ssv),
                  reads=[B_src], writes=[B_ss[i], B_junk])
            pr.op("act", lambda e: e.activation(out=rsv, in_=ssv, func=AF.Sqrt, bias=RMS_EPS, scale=1.0 / D),
                  reads=[B_ss[i]], writes=[B_rs[i]])
            pr.op("dve", lambda e: e.reciprocal(out=rsv, in_=rsv), reads=[B_rs[i]], writes=[B_rs[i]])
            return rsv, B_rs[i]

        for gi in range(4):
            pr.dma("sp", mixS, mixT_d[:, gi * 512:(gi + 1) * 512].rearrange("(k p) t -> p k t", p=P), "mixS",
                   reads=[B_mixT], writes=[B_mixS])
            nxt = load_w2(w_out_bf[:, 0:512].rearrange("(k p) n -> p k n", p=P), B_wout)
            for n in range(4):
                w, B_w = nxt
                if n + 1 < 4:
                    nxt = load_w2(w_out_bf[:, (n + 1) * 512:(n + 2) * 512].rearrange("(k p) n -> p k n", p=P), B_wout)
                else:
                    nxtW1 = [load_w2(w_ff1_bf[:, 0:512].rearrange("(k p) n -> p k n", p=P), B_wff1[0])]
                for t in range(4):
                    bank = CB[cb_ctr[0] % 2]
                    cb_ctr[0] += 1
                    po = ps_f32(bank)
                    mm_group(po, [(mixS[:, k, t * P:(t + 1) * P], w[:, k, :]) for k in range(KC)],
                             reads=[B_mixS, B_w], writes=[pbuf[bank]])
                    pr.op("act", lambda e, po=po, t=t, n=n: e.copy(out=accF[:, t, n * 512:(n + 1) * 512], in_=po),
                          reads=[pbuf[bank]], writes=[B_accF[t]])
            for t in range(4):
                i = xc_ctr[0] % 2
                xc_ctr[0] += 1
                xs, B_xs = xsC[i]
                ab, B_ab = abC[i]
                s_ = 2 * gi + t // 2
                r0 = s_ * SLOTW + HALO + (t % 2) * P
                pr.dma("sp", xs, xown_d[r0:r0 + P, :], "xsA%d" % i, writes=[B_xs])
                rsv, B_r = rstd_of(accF[:, t, :], B_accF[t], ab, B_ab)
                pr.op("dve", lambda e, t=t, rsv=rsv: e.scalar_tensor_tensor(
                    out=accF[:, t, :], in0=accF[:, t, :], scalar=rsv, in1=gpost_b, op0=ALU.mult, op1=ALU.mult),
                    reads=[B_accF[t], B_r, B_gpost], writes=[B_accF[t]])
                pr.op("dve", lambda e, t=t, xs=xs: e.tensor_tensor(out=xs, in0=accF[:, t, :], in1=xs, op=ALU.add),
                      reads=[B_accF[t], B_xs], writes=[B_xs])
                yt = gi * 4 + t
                pr.dma("sp", y_d[yt * P:(yt + 1) * P, :], xs, "xsSt%d" % i, reads=[B_xs], writes=[B_y[yt]])
                norm_T_tile(None, xs, B_xs, None, gffn_b, B_gffn, ab, B_ab, fT, B_fT, t * P, TB)
            NFC = DFF // 512

            def w1src(c):
                return w_ff1_bf[:, c * 512:(c + 1) * 512].rearrange("(k p) n -> p k n", p=P), B_wff1[c // 4]

            def w2src(c):
                return w_ff2_bf[c * 512:(c + 1) * 512, :].rearrange("(j p) n -> p j n", p=P), B_wff2[c // 4]
            W1 = {0: nxtW1[0]}
            W2 = {}
            W1[1] = load_w2(*w1src(1))
            W2[0] = load_w2(*w2src(0))

            def ffn_H(c):
                w, B_w = W1.pop(c)
                hT, B_hT = hTs[c % 2]
                for j in range(4):
                    bank = HBK[j % 2]
                    ph = ps_f32(bank)
                    mm_group(ph, [(w[:, k, j * P:(j + 1) * P], fT[:, k, :]) for k in range(KC)],
                             reads=[B_w, B_fT], writes=[pbuf[bank]])
                    rt, B_rt = rts[j % 2]
                    pr.op("act", lambda e, rt=rt, ph=ph: e.activation(out=rt, in_=ph, func=AF.Relu),
                          reads=[pbuf[bank]], writes=[B_rt])
                    pr.op("act", lambda e, rt=rt, hT=hT, j=j: e.activation(out=hT[:, j, :], in_=rt, func=AF.Square),
                          reads=[B_rt], writes=[B_hT])

            def ffn_O(c):
                w, B_w = W2.pop(c)
                wv_ = w.rearrange("p k n -> p (k n)").rearrange("p (j n) -> p j n", j=4, n=D)
                hT, B_hT = hTs[c % 2]
                for t in range(4):
                    for n in range(4):
                        bank = CB[cb_ctr[0] % 2]
                        cb_ctr[0] += 1
                        po = ps_f32(bank)
                        mm_group(po, [(hT[:, j, t * P:(t + 1) * P], wv_[:, j, n * 512:(n + 1) * 512]) for j in range(4)],
                                 reads=[B_hT, B_w], writes=[pbuf[bank]])
                        dst = accF[:, t, n * 512:(n + 1) * 512]
                        if c == 0:
                            pr.op("dve", lambda e, dst=dst, po=po: e.tensor_copy(out=dst, in_=po),
                                  reads=[pbuf[bank]], writes=[B_accF[t]])
                        else:
                            pr.op("dve", lambda e, dst=dst, po=po: e.tensor_tensor(out=dst, in0=po, in1=dst, op=ALU.add),
                                  reads=[pbuf[bank], B_accF[t]], writes=[B_accF[t]])
            ffn_H(0)
            for c in range(NFC):
                if c + 2 < NFC:
                    W1[c + 2] = load_w2(*w1src(c + 2))
                if c + 1 < NFC:
                    W2[c + 1] = load_w2(*w2src(c + 1))
                    ffn_H(c + 1)
                ffn_O(c)
            for t in range(4):
                i = xc_ctr[0] % 2
                xc_ctr[0] += 1
                xs, B_xs = xsC[i]
                ab, B_ab = abC[i]
                yt = gi * 4 + t
                pr.dma("sp", xs, y_d[yt * P:(yt + 1) * P, :], "xsA%d" % i, reads=[B_y[yt]], writes=[B_xs])
                rsv, B_r = rstd_of(accF[:, t, :], B_accF[t], ab, B_ab)
                pr.op("dve", lambda e, t=t, rsv=rsv: e.scalar_tensor_tensor(
                    out=accF[:, t, :], in0=accF[:, t, :], scalar=rsv, in1=gfpost_b, op0=ALU.mult, op1=ALU.mult),
                    reads=[B_accF[t], B_r, B_gfpost], writes=[B_accF[t]])
                pr.op("dve", lambda e, t=t, xs=xs: e.tensor_tensor(out=xs, in0=accF[:, t, :], in1=xs, op=ALU.add),
                      reads=[B_accF[t], B_xs], writes=[B_xs])
                o = pr.dma("sp", y_d[yt * P:(yt + 1) * P, :], xs, "xsSt%d" % i, reads=[B_xs], writes=[B_y[yt]])
                pr.must_finish(o)
        return _finish(nc, pr, st, ar, [])


def _finish(nc, pr, st, ar, bufs):
    for b in bufs:
        if b.writer is not None:
            pr.must_finish(b.writer)
    pr.emit_all(st)
    nc._mk_info = dict(n_sems=pr.n_sems, peak=ar.peak, nops=len(pr.all_ops), counts=pr.max_count)
    return nc


def alibi_slopes():
    return (2.0 ** (-8.0 * np.arange(1, H + 1) / H)).astype(np.float64)


def core_tables(j):
    blks = own_blocks(j)
    sl = alibi_slopes()
    fd = np.zeros((NSLOT, P, 2, H, NB), np.float64)
    gb = np.full((P, NSLOT, NB), -1e30, np.float32)
    hm = np.ones((P, NSLOT), np.float32)
    q = np.arange(P)
    for s, blk in enumerate(blks):
        if blk == 0:
            hm[:, s] = 0.0
        for n in range(blk):
            gb[:, s, n] = 0.0
            for qt in range(2):
                dist = 256 * (n - blk) + 255 - (128 * qt + q)
                fd[s, :, qt, :, n] = np.exp(sl[None, :] * dist[:, None])
    return (fd.reshape(NSLOT, P, 2 * H * NB).astype(np.float32), gb.reshape(P, NSLOT * NB), hm)


def const_tables():
    sl = alibi_slopes()
    p = np.arange(P)
    bkt = np.zeros((P, H, 2), np.float64)
    for t in range(2):
        bkt[:, :, t] = sl[None, :] * (t * 128 + p[:, None] - 255)
    fown = np.exp(sl[None, :] * (127 - p[:, None]))
    tri = (p[None, :] >= p[:, None]).astype(np.float32)
    return (bkt.reshape(P, 2 * H).astype(np.float32), fown.astype(np.float32),
            tri.astype(ml_dtypes.bfloat16), np.eye(P, dtype=np.float32).astype(ml_dtypes.bfloat16))


def make_in_maps(inputs, cores=range(8)):
    x = np.asarray(inputs["x"], np.float32)
    f = lambda k: np.ascontiguousarray(np.asarray(inputs[k], np.float32)[0])
    w_in, w_out, w_ff1, w_ff2 = f("w_in"), f("w_out"), f("w_ff1"), f("w_ff2")
    grow = np.stack([f("g_mix_pre"), f("g_mix_post"), f("g_ffn_pre"), f("g_ffn_post")]).astype(np.float32)
    b_glu, w_dw, b_dw, ln_g, ln_b = f("b_glu"), f("w_dw"), f("b_dw"), f("ln_conv_g"), f("ln_conv_b")
    cols = np.concatenate([
        b_glu.reshape(16, P).T,
        w_dw.reshape(CW, CC, P).transpose(2, 1, 0).reshape(P, CC * CW),
        b_dw.reshape(CC, P).T, ln_g.reshape(CC, P).T, ln_b.reshape(CC, P).T], axis=1).astype(np.float32)
    bkt, fown, tri, ident = const_tables()
    maps = []
    for c in cores:
        bi, j = c // 4, c % 4
        fd, gb, hm = core_tables(j)
        xo = np.zeros((NSLOT, SLOTW, D), np.float32)
        for s, blk in enumerate(own_blocks(j)):
            lo = L * blk - HALO
            if lo < 0:
                xo[s, HALO:] = x[bi, 0:L]
            else:
                xo[s] = x[bi, lo:lo + SLOTW]
        maps.append(dict(xall=np.ascontiguousarray(x[bi]), xown=xo.reshape(NOWNH, D), w_in=w_in, w_out=w_out,
                         w_ff1=w_ff1, w_ff2=w_ff2, grow=grow, cols=np.ascontiguousarray(cols), fd=fd, gbias=gb,
                         hmask=hm, bkt=bkt, fown=fown, tri=tri, ident=ident))
    return maps


_NC = None


def kernel(**inputs):
    global _NC
    if _NC is None:
        _NC = build_nc()
    maps = make_in_maps(inputs)
    res = run_bass_kernel_spmd(_NC, maps, core_ids=list(range(8)))
    x = np.asarray(inputs["x"])
    out = np.zeros(x.shape, np.float32)
    for c in range(8):
        bi, j = c // 4, c % 4
        y = np.asarray(res.results[c]["y"])
        for s, blk in enumerate(own_blocks(j)):
            out[bi, L * blk:L * (blk + 1)] = y[s * L:(s + 1) * L]
    return out
```

```python
import contextlib
import numpy as np
import ml_dtypes
import concourse.bass as bass
import concourse.mybir as mybir
from concourse.bass_utils import run_bass_kernel_spmd

F32 = mybir.dt.float32
BF16 = mybir.dt.bfloat16
ALU = mybir.AluOpType
AF = mybir.ActivationFunctionType
AX = mybir.AxisListType

P = 128
D = 2048
KC = 16
S = 8192
NB = 32
L = 256
H = 8
DH = 128
C = 1024
CC = 8
DFF = 8192
INC = 5120
NSLOT = 8
HALO = 32
SLOTW = L + HALO
NOWN = NSLOT * L
NOWNH = NSLOT * SLOTW
SCALE = DH ** -0.5
RMS_EPS = 1e-6
LN_EPS = 1e-5
CW = 31

STREAMS = ("pe", "act", "dve", "pool", "sp")


class Buf:
    __slots__ = ("name", "writer", "readers", "inherit", "excl")

    def __init__(self, name, inherit=(), excl=False):
        self.name = name
        self.excl = excl
        self.writer = None
        self.readers = []
        self.inherit = list(inherit)


class Op:
    __slots__ = ("stream", "emit", "deps", "is_dma", "semkey", "signal", "name")

    def __init__(self, stream, emit, is_dma=False, semkey=None, name=""):
        self.stream = stream
        self.emit = emit
        self.deps = []
        self.is_dma = is_dma
        self.semkey = semkey
        self.signal = False
        self.name = name


class Prog:
    def __init__(self, nc):
        self.nc = nc
        self.ops = {s: [] for s in STREAMS}
        self.all_ops = []
        self.final_waits = []

    def _add(self, op, reads, writes):
        deps = []
        for b in reads:
            if b.writer is not None:
                deps.append(b.writer)
            elif b.inherit:
                deps.extend(b.inherit)
            if b.excl:
                deps.extend(r for r in b.readers if r.stream != op.stream)
        for b in writes:
            if b.writer is not None:
                deps.append(b.writer)
            deps.extend(b.readers)
            if b.inherit:
                deps.extend(b.inherit)
                b.inherit = []
        seen = set()
        for d in deps:
            if d is op or id(d) in seen:
                continue
            seen.add(id(d))
            op.deps.append(d)
        for b in reads:
            b.readers.append(op)
        for b in writes:
            b.writer = op
            b.readers = []
        self.ops[op.stream].append(op)
        self.all_ops.append(op)
        return op

    def op(self, stream, emit, reads=(), writes=(), name=""):
        return self._add(Op(stream, emit, name=name), reads, writes)

    def dma(self, stream, out, in_, semkey, reads=(), writes=(), name=""):
        def emit(eng):
            return eng.dma_start(out=out, in_=in_)
        return self._add(Op(stream, emit, is_dma=True, semkey=semkey, name=name), reads, writes)

    def must_finish(self, op):
        self.final_waits.append(op)

    def emit_all(self, stack):
        nc = self.nc
        for op in self.all_ops:
            for d in op.deps:
                d.signal = True
        for op in self.final_waits:
            op.signal = True
        eng_sem = {s: stack.enter_context(nc.semaphore("done_" + s)) for s in STREAMS}
        dma_sems = {}
        dma_cnt = {}
        cnt = {s: 0 for s in STREAMS}
        comp = {}
        for op in self.all_ops:
            s = op.stream
            if op.is_dma:
                k = op.semkey
                if k not in dma_sems:
                    dma_sems[k] = stack.enter_context(nc.semaphore("dq_%d" % len(dma_sems)))
                    dma_cnt[k] = 0
                dma_cnt[k] += 16
                comp[id(op)] = (dma_sems[k], dma_cnt[k])
            elif op.signal:
                cnt[s] += 1
                comp[id(op)] = (eng_sem[s], cnt[s])
        self.n_sems = len(dma_sems) + len(STREAMS)
        self.max_count = dict(cnt)
        block = stack.enter_context(nc.Block())
        prog = self

        def run_stream(s, eng):
            waited = {}
            for op in prog.ops[s]:
                need = {}
                for d in op.deps:
                    sem, val = comp[id(d)]
                    key = id(sem)
                    if waited.get(key, 0) >= val:
                        continue
                    if key not in need or need[key][1] < val:
                        need[key] = (sem, val)
                for key, (sem, val) in need.items():
                    waited[key] = val
                    eng.wait_ge(sem, val)
                ins = op.emit(eng)
                if op.is_dma:
                    ins.then_inc(comp[id(op)][0], 16)
                elif op.signal:
                    ins.then_inc(eng_sem[s], 1)
            if s == "sp":
                for op in prog.final_waits:
                    sem, val = comp[id(op)]
                    eng.wait_ge(sem, val)

        @block.tensor
        def _(e):
            run_stream("pe", e)

        @block.scalar
        def _(e):
            run_stream("act", e)

        @block.vector
        def _(e):
            run_stream("dve", e)

        @block.gpsimd
        def _(e):
            run_stream("pool", e)

        @block.sync
        def _(e):
            run_stream("sp", e)


DT_SIZE = {F32: 4, BF16: 2}


class Arena:
    def __init__(self, nc, stack, kib):
        self.words = kib * 256
        self.t = stack.enter_context(nc.sbuf_tensor("arena", [P, self.words], F32))
        self.top = 0
        self.peak = 0
        self.retired = []
        self.live = []

    def alloc(self, name, shape, dtype):
        n = 1
        for s in shape:
            n *= s
        nbytes = (n * DT_SIZE[dtype] + 31) // 32 * 32
        lo = self.top
        hi = lo + nbytes
        assert hi <= self.words * 4, "SBUF arena overflow at %s: %d > %d" % (name, hi, self.words * 4)
        self.top = hi
        self.peak = max(self.peak, hi)
        inh = []
        keep = []
        for (l, h, ops) in self.retired:
            if l < hi and h > lo:
                inh.extend(ops)
                if l >= lo and h <= hi:
                    continue
            keep.append((l, h, ops))
        self.retired = keep
        buf = Buf(name, inherit=inh)
        v = self.t[:, lo // 4:hi // 4]
        if dtype != F32:
            v = v.bitcast(dtype)
        v = v[:, 0:n]
        if len(shape) == 2:
            v = v.rearrange("p (a b) -> p a b", a=shape[0], b=shape[1])
        elif len(shape) == 3:
            v = v.rearrange("p (a b c) -> p a b c", a=shape[0], b=shape[1], c=shape[2])
        elif len(shape) == 4:
            v = v.rearrange("p (a b c d) -> p a b c d", a=shape[0], b=shape[1], c=shape[2], d=shape[3])
        self.live.append((lo, hi, buf))
        return v, buf

    def mark(self):
        return self.top

    def release(self, mark):
        keep = []
        for (lo, hi, buf) in self.live:
            if lo >= mark:
                ops = list(buf.readers) + list(buf.inherit)
                if buf.writer is not None:
                    ops.append(buf.writer)
                if ops:
                    self.retired.append((lo, hi, ops))
            else:
                keep.append((lo, hi, buf))
        self.live = keep
        self.top = mark


def own_blocks(j):
    return [8 * (s // 2) + (j if s % 2 == 0 else 7 - j) for s in range(NSLOT)]


PAST = [8 * (s // 2) + (3 if s % 2 == 0 else 7) for s in range(NSLOT)]


def build_nc(debug=False, stop_after="all"):
    nc = bass.Bass("TRN2", target_bir_lowering=False)
    skind = "ExternalOutput" if debug else "Internal"

    def din(name, shape, dt=F32):
        return nc.dram_tensor(name, list(shape), dt, kind="ExternalInput").ap()

    def dscr(name, shape, dt):
        return nc.dram_tensor(name, list(shape), dt, kind=skind).ap()

    xall_d = din("xall", [S, D])
    xown_d = din("xown", [NOWNH, D])
    w_in_d = din("w_in", [D, INC])
    w_out_d = din("w_out", [D, D])
    w_ff1_d = din("w_ff1", [D, DFF])
    w_ff2_d = din("w_ff2", [DFF, D])
    grow_d = din("grow", [4, D])
    cols_d = din("cols", [P, 16 + CC * CW + 3 * CC])
    fd_d = din("fd", [NSLOT, P, 2 * H * NB])
    gbias_d = din("gbias", [P, NSLOT * NB])
    hmask_d = din("hmask", [P, NSLOT])
    bkt_d = din("bkt", [P, H * 2])
    fown_d = din("fown", [P, H])
    tri_d = din("tri", [P, P], BF16)
    ident_d = din("ident", [P, P], BF16)

    y_d = nc.dram_tensor("y", [NOWN, D], F32, kind="ExternalOutput").ap()

    w_in_bf = dscr("w_in_bf", [D, INC], BF16)
    w_out_bf = dscr("w_out_bf", [D, D], BF16)
    w_ff1_bf = dscr("w_ff1_bf", [D, DFF], BF16)
    w_ff2_bf = dscr("w_ff2_bf", [DFF, D], BF16)
    KT_d = dscr("KT", [H, DH, S], BF16)
    V_d = dscr("V", [S, H * DH], BF16)
    KTo_d = dscr("KTo", [H, DH, NOWN], BF16)
    Vo_d = dscr("Vo", [NOWN, H * DH], BF16)
    QT_d = dscr("QT", [H, DH, NOWN], BF16)
    F_d = dscr("Fsel", [H, P, 16 * NB], F32)
    yT_d = dscr("yT", [CC, P, NOWN], F32)
    mixT_d = dscr("mixT", [D, NOWN], BF16)
    kmean_dbg = dscr("kmean_dbg", [P, H * NB], F32) if debug else None

    with contextlib.ExitStack() as st:
        pr = Prog(nc)
        ar = Arena(nc, st, 204)
        psum = []
        pbuf = []
        for i in range(8):
            psum.append(st.enter_context(nc.psum_tensor("ps%d" % i, [P, 512], F32)))
            pbuf.append(Buf("ps%d" % i, excl=True))

        def ps_f32(i, n=512):
            return psum[i][:, 0:n]

        def ps_bf16(i):
            return psum[i][:, :].bitcast(BF16)

        cols, B_cols = ar.alloc("cols", [16 + CC * CW + 3 * CC], F32)
        pr.dma("sp", cols, cols_d, "c0", writes=[B_cols])
        bglu = cols[:, 0:16]
        wdw = cols[:, 16:16 + CC * CW].rearrange("p (c j) -> p c j", c=CC, j=CW)
        o0 = 16 + CC * CW
        bdw = cols[:, o0:o0 + CC]
        lng = cols[:, o0 + CC:o0 + 2 * CC]
        lnb = cols[:, o0 + 2 * CC:o0 + 3 * CC]
        ident, B_ident = ar.alloc("ident", [P], BF16)
        pr.dma("sp", ident, ident_d, "c1", writes=[B_ident])
        tri, B_tri = ar.alloc("tri", [P], BF16)
        pr.dma("sp", tri, tri_d, "c2", writes=[B_tri])
        bkt, B_bkt = ar.alloc("bkt", [H, 2], F32)
        pr.dma("sp", bkt, bkt_d.rearrange("p (h t) -> p h t", h=H, t=2), "c3", writes=[B_bkt])
        fown, B_fown = ar.alloc("fown", [H], F32)
        pr.dma("sp", fown, fown_d, "c4", writes=[B_fown])
        gbias, B_gbias = ar.alloc("gbias", [NSLOT, NB], F32)
        pr.dma("sp", gbias, gbias_d.rearrange("p (s n) -> p s n", s=NSLOT, n=NB), "c5", writes=[B_gbias])
        hmask, B_hmask = ar.alloc("hmask", [NSLOT], F32)
        pr.dma("sp", hmask, hmask_d, "c6", writes=[B_hmask])
        kmean, B_kmean = ar.alloc("kmean", [H, NB], F32)
        ones32, B_ones = ar.alloc("ones32", [P], F32)
        pr.op("pool", lambda e: e.memset(ones32, 1.0), writes=[B_ones])
        ss_t, _ = ar.alloc("ss", [8], F32)
        rs_t, _ = ar.alloc("rs", [8], F32)
        B_ss = [Buf("ss%d" % i) for i in range(8)]
        B_rs = [Buf("rs%d" % i) for i in range(8)]
        stat_ctr = [0]

        def in_col_bufs(c0):
            if c0 < 1024:
                return B_wq
            if c0 < 3072:
                return B_wkv
            return B_wu

        def norm_T_tile(x_src, xs, B_xs, xs_key, g_b, B_gb, abf, B_abf, dstT, B_dstT, col0, tbanks):
            i = stat_ctr[0] % 8
            stat_ctr[0] += 1
            ssv = ss_t[:, i:i + 1]
            rsv = rs_t[:, i:i + 1]
            if x_src is not None:
                pr.dma("sp", xs, x_src, xs_key, writes=[B_xs])
            pr.op("act", lambda e: e.activation(out=abf, in_=xs, func=AF.Square, accum_out=ssv),
                  reads=[B_xs], writes=[B_ss[i], B_abf])
            pr.op("act", lambda e: e.activation(out=rsv, in_=ssv, func=AF.Sqrt, bias=RMS_EPS, scale=1.0 / D),
                  reads=[B_ss[i]], writes=[B_rs[i]])
            pr.op("dve", lambda e: e.reciprocal(out=rsv, in_=rsv), reads=[B_rs[i]], writes=[B_rs[i]])
            pr.op("dve", lambda e: e.scalar_tensor_tensor(out=abf, in0=xs, scalar=rsv, in1=g_b,
                                                          op0=ALU.mult, op1=ALU.mult),
                  reads=[B_xs, B_rs[i], B_gb], writes=[B_abf])
            for half in range(2):
                bank = tbanks[half]
                pt = ps_bf16(bank).rearrange("p (k n) -> p k n", k=8, n=P)

                def tr(e, half=half, pt=pt):
                    ins = None
                    for kk in range(8):
                        k = half * 8 + kk
                        ins = e.transpose(out=pt[:, kk, :], in_=abf[:, k * P:(k + 1) * P], identity=ident)
                    return ins
                pr.op("pe", tr, reads=[B_abf, B_ident], writes=[pbuf[bank]])
                dst = dstT[:, half * 8:(half + 1) * 8, col0:col0 + P]
                if half == 0:
                    pr.op("act", lambda e, dst=dst, pt=pt: e.copy(out=dst, in_=pt), reads=[pbuf[bank]], writes=[B_dstT])
                else:
                    pr.op("dve", lambda e, dst=dst, pt=pt: e.tensor_copy(out=dst, in_=pt), reads=[pbuf[bank]],
                          writes=[B_dstT])

        def mm_group(out_ap, pairs, reads, writes, name=""):
            def emit(e):
                ins = None
                n = len(pairs)
                for i, (l, r) in enumerate(pairs):
                    ins = e.matmul(out_ap, lhsT=l, rhs=r, start=(i == 0), stop=(i == n - 1))
                return ins
            return pr.op("pe", emit, reads=reads, writes=writes, name=name)

        mA = ar.mark()
        gpre_b, B_gpre = ar.alloc("gpre_b", [D], F32)
        pr.dma("sp", gpre_b, grow_d[0:1, :].partition_broadcast(P), "c7", writes=[B_gpre])
        wkv, _ = ar.alloc("wkv", [KC, 2048], BF16)
        B_wkvS = [Buf("wkvS%d" % i) for i in range(4)]
        wsrc = w_in_d[:, 1024:3072].rearrange("(k p) n -> p k n", p=P)
        for i in range(4):
            pr.dma("pool", wkv[:, 4 * i:4 * i + 4, :], wsrc[:, 4 * i:4 * i + 4, :], "wkvS%d" % i, writes=[B_wkvS[i]])
        def cast_group(name, dst, src, pieces):
            bufs = []
            for i, (dsl, ssl) in enumerate(pieces):
                b = Buf("%s_%d" % (name, i))
                pr.dma("pool", dst[dsl], src[ssl], "cast_" + name, writes=[b])
                bufs.append(b)
            return bufs

        def rows4(c0, c1, nrows=D):
            q = nrows // 4
            return [((slice(i * q, (i + 1) * q), slice(c0, c1)),) * 2 for i in range(4)]

        B_wkv = cast_group("wkv", w_in_bf, w_in_d, rows4(1024, 3072))
        B_wq = cast_group("wq", w_in_bf, w_in_d, rows4(0, 1024))
        B_wu = cast_group("wu", w_in_bf, w_in_d, rows4(3072, 5120))
        B_wout = cast_group("wout", w_out_bf, w_out_d, rows4(0, D))
        B_wff1 = [cast_group("wff1_%d" % g, w_ff1_bf, w_ff1_d, rows4(g * 2048, (g + 1) * 2048)) for g in range(4)]
        B_wff2 = []
        for g in range(4):
            pcs = [((slice(g * 2048 + i * 512, g * 2048 + (i + 1) * 512), slice(0, D)),) * 2 for i in range(4)]
            B_wff2.append(cast_group("wff2_%d" % g, w_ff2_bf, w_ff2_d, pcs))

        xsA = [ar.alloc("xsA%d" % i, [D], F32) for i in range(4)]
        abA = [ar.alloc("abA%d" % i, [D], BF16) for i in range(4)]
        aTA = [ar.alloc("aTA%d" % i, [KC, 512], BF16) for i in range(2)]
        ktst = [ar.alloc("ktst%d" % i, [H, 512], BF16) for i in range(2)]
        vst = [ar.alloc("vst%d" % i, [1024], BF16) for i in range(2)]
        kmsum, B_kmsum = ar.alloc("kmsum", [H, NB], F32)
        NG = S // 512
        KT_v = KT_d.rearrange("h d t -> d h t")
        TB = (0, 1)
        KB = (2, 3)
        VB = (4, 5)
        tile_ctr = [0]

        def A_norm(g):
            aT, B_aT = aTA[g % 2]
            for t in range(4):
                i = tile_ctr[0] % 4
                tile_ctr[0] += 1
                r0 = g * 512 + t * P
                norm_T_tile(xall_d[r0:r0 + P, :], xsA[i][0], xsA[i][1], "xsA%d" % i, gpre_b, B_gpre,
                            abA[i][0], abA[i][1], aT, B_aT, t * P, TB)

        def A_kv(g):
            aT, B_aT = aTA[g % 2]
            kst, B_kst = ktst[g % 2]
            for h in range(H):
                bank = KB[h % 2]
                pk = ps_f32(bank)
                mm_group(pk, [(wkv[:, k, h * DH:(h + 1) * DH], aT[:, k, :]) for k in range(KC)],
                         reads=[B_aT] + B_wkvS, writes=[pbuf[bank]])
                pr.op("act", lambda e, pk=pk, h=h: e.copy(out=kst[:, h, :], in_=pk), reads=[pbuf[bank]], writes=[B_kst])
                pr.op("dve", lambda e, h=h: e.tensor_reduce(
                    out=kmsum[:, h, 2 * g:2 * g + 2], in_=kst[:, h, :].rearrange("p (a b) -> p a b", a=2, b=L),
                    axis=AX.X, op=ALU.add), reads=[B_kst], writes=[B_kmsum])
            pr.dma("sp", KT_v[:, :, g * 512:(g + 1) * 512], kst, "ktst%d" % (g % 2), reads=[B_kst], writes=[B_KT])
            for t in range(4):
                vi = (g * 4 + t) % 2
                vs, B_vs = vst[vi]
                for half in range(2):
                    bank = VB[half]
                    pv = ps_f32(bank)
                    mm_group(pv, [(aT[:, k, t * P:(t + 1) * P], wkv[:, k, 1024 + half * 512:1024 + (half + 1) * 512])
                                  for k in range(KC)], reads=[B_aT] + B_wkvS, writes=[pbuf[bank]])
                    if half == 0:
                        pr.op("dve", lambda e, pv=pv, vs=vs: e.tensor_copy(out=vs[:, 0:512], in_=pv),
                              reads=[pbuf[bank]], writes=[B_vs])
                    else:
                        pr.op("act", lambda e, pv=pv, vs=vs: e.copy(out=vs[:, 512:1024], in_=pv),
                              reads=[pbuf[bank]], writes=[B_vs])
                r0 = g * 512 + t * P
                pr.dma("sp", V_d[r0:r0 + P, :], vs, "vst%d" % vi, reads=[B_vs], writes=[B_V])

        B_KT = Buf("KT_d")
        B_V = Buf("V_d")
        A_norm(0)
        for g in range(NG):
            if g + 1 < NG:
                A_norm(g + 1)
            A_kv(g)
        pr.op("dve", lambda e: e.tensor_scalar(out=kmean, in0=kmsum, scalar1=1.0 / L, scalar2=None, op0=ALU.mult),
              reads=[B_kmsum], writes=[B_kmean])
        if debug:
            o = pr.dma("sp", kmean_dbg, kmean.rearrange("p h n -> p (h n)"), "dbg0", reads=[B_kmean])
            pr.must_finish(o)
        ar.release(mA)

        last_ops = []
        if stop_after == "A":
            return _finish(nc, pr, st, ar, [B_KT, B_V])

        mB = ar.mark()
        gpre_b, B_gpre = ar.alloc("gpre_b2", [D], F32)
        pr.dma("sp", gpre_b, grow_d[0:1, :].partition_broadcast(P), "c7", writes=[B_gpre])
        aTo, B_aTo = ar.alloc("aTo", [KC, NOWNH], BF16)
        Fall, B_Fall = ar.alloc("Fall", [16, H, NB], F32)
        mB1 = ar.mark()
        xsB = [ar.alloc("xsB%d" % i, [D], F32) for i in range(2)]
        abB = [ar.alloc("abB%d" % i, [D], BF16) for i in range(2)]
        for t in range(NOWNH // P):
            i = t % 2
            norm_T_tile(xown_d[t * P:(t + 1) * P, :], xsB[i][0], xsB[i][1], "xsA%d" % i, gpre_b, B_gpre,
                        abB[i][0], abB[i][1], aTo, B_aTo, t * P, TB)
        ar.release(mB1)
        NWS = 3
        wch = [ar.alloc("wch%d" % i, [KC, 512], BF16) for i in range(NWS)]
        wch_ctr = [0]

        def load_wchunk(src_ap, reads):
            i = wch_ctr[0] % NWS
            wch_ctr[0] += 1
            w, B_w = wch[i]
            pr.dma("sp", w, src_ap, "wch%d" % i, reads=reads, writes=[B_w])
            return w, B_w

        def in_chunk_src(c0):
            return w_in_bf[:, c0:c0 + 512].rearrange("(k p) n -> p k n", p=P)

        qst = [ar.alloc("qst%d" % i, [L], BF16) for i in range(2)]
        q32 = [ar.alloc("q32_%d" % i, [L], F32) for i in range(2)]
        kost = [ar.alloc("kost%d" % i, [L], BF16) for i in range(2)]
        vost = [ar.alloc("vost%d" % i, [512], BF16) for i in range(2)]
        fdt = [ar.alloc("fdt%d" % i, [2, H, NB], F32) for i in range(2)]
        gsb = [ar.alloc("gsb%d" % i, [NB], F32) for i in range(2)]
        top8 = [ar.alloc("top8_%d" % i, [8], F32) for i in range(2)]
        sig = [ar.alloc("sig%d" % i, [SLOTW], F32) for i in range(2)]
        hst = [ar.alloc("hst%d" % i, [SLOTW], BF16) for i in range(2)]
        dgb = [ar.alloc("dgb%d" % i, [CW, P], BF16) for i in range(2)]
        CVB = (6, 7)
        accD = [ar.alloc("accD%d" % i, [L], F32) for i in range(2)]
        B_QT = Buf("QT_d")
        B_KTo = Buf("KTo_d")
        B_Vo = Buf("Vo_d")
        B_yT = Buf("yT_d")
        B_F = Buf("F_d")
        PB = (2, 3, 4, 5)
        GB = 6
        pb_ctr = [0]

        def nbank():
            b = PB[pb_ctr[0] % len(PB)]
            pb_ctr[0] += 1
            return b
        ctr = {"q": 0, "k": 0, "v": 0, "u": 0, "g": 0, "c": 0}

        chunk_list = [("q", 0), ("q", 512), ("k", 1024), ("k", 1536), ("v", 2048), ("v", 2560),
                      ("uv", 3072), ("ug", 4096), ("uv", 3584), ("ug", 4608)]
        pending = None
        nxt = load_wchunk(in_chunk_src(chunk_list[0][1]), in_col_bufs(chunk_list[0][1]))
        for ci, (kind, c0) in enumerate(chunk_list):
            w, B_w = nxt
            if ci + 1 < len(chunk_list):
                nxt = load_wchunk(in_chunk_src(chunk_list[ci + 1][1]), in_col_bufs(chunk_list[ci + 1][1]))
            if kind in ("q", "k"):
                for s in range(NSLOT):
                    if kind == "q":
                        fi = ctr["g"] % 2
                        ctr["g"] += 1
                        fdv, B_fd = fdt[fi]
                        pr.dma("sp", fdv, fd_d[s].rearrange("p (t h n) -> p t h n", t=2, h=H, n=NB), "fdt%d" % fi,
                               writes=[B_fd])
                    for sub in range(4):
                        h = (c0 % 1024) // DH + sub
                        bank = nbank()
                        pq = ps_f32(bank, SLOTW)
                        mm_group(pq, [(w[:, k, sub * DH:(sub + 1) * DH], aTo[:, k, s * SLOTW:(s + 1) * SLOTW])
                                      for k in range(KC)], reads=[B_aTo, B_w], writes=[pbuf[bank]])
                        if kind == "k":
                            i = ctr["k"] % 2
                            ctr["k"] += 1
                            ks, B_ks = kost[i]
                            pr.op("act", lambda e, ks=ks, pq=pq: e.copy(out=ks, in_=pq[:, HALO:SLOTW]),
                                  reads=[pbuf[bank]], writes=[B_ks])
                            pr.dma("sp", KTo_d[h, :, s * L:(s + 1) * L], ks, "kost%d" % i, reads=[B_ks], writes=[B_KTo])
                            continue
                        i = ctr["q"] % 2
                        ctr["q"] += 1
                        qs, B_qs = qst[i]
                        qf, B_qf = q32[i]
                        pr.op("act", lambda e, qf=qf, pq=pq: e.copy(out=qf, in_=pq[:, HALO:SLOTW]),
                              reads=[pbuf[bank]], writes=[B_qf])
                        pr.op("dve", lambda e, qs=qs, qf=qf: e.tensor_copy(out=qs, in_=qf), reads=[B_qf], writes=[B_qs])
                        pr.dma("sp", QT_d[h, :, s * L:(s + 1) * L], qs, "qst%d" % i, reads=[B_qs], writes=[B_QT])
                        pg = ps_f32(GB, 2 * NB).rearrange("p (t n) -> p t n", t=2, n=NB)

                        def gmm(e, qf=qf, h=h, pg=pg):
                            e.matmul(pg[:, 0, :], lhsT=qf[:, 0:P], rhs=kmean[:, h, :], start=True, stop=True)
                            return e.matmul(pg[:, 1, :], lhsT=qf[:, P:2 * P], rhs=kmean[:, h, :], start=True, stop=True)
                        pr.op("pe", gmm, reads=[B_qf, B_kmean], writes=[pbuf[GB]])
                        for qt in range(2):
                            gi = ctr["u"] % 2
                            ctr["u"] += 1
                            gs, B_gs = gsb[gi]
                            t8, B_t8 = top8[gi]
                            pr.op("dve", lambda e, gs=gs, qt=qt, pg=pg, s=s: e.tensor_tensor(
                                out=gs, in0=pg[:, qt, :], in1=gbias[:, s, :], op=ALU.add),
                                reads=[pbuf[GB], B_gbias], writes=[B_gs])
                            pr.op("dve", lambda e, gs=gs, t8=t8: e.max(out=t8, in_=gs), reads=[B_gs], writes=[B_t8])
                            pr.op("dve", lambda e, gs=gs, t8=t8, qt=qt, h=h, s=s, fdv=fdv: e.scalar_tensor_tensor(
                                out=Fall[:, 2 * s + qt, h, :], in0=gs, scalar=t8[:, 2:3], in1=fdv[:, qt, h, :],
                                op0=ALU.is_ge, op1=ALU.mult), reads=[B_gs, B_t8, B_fd], writes=[B_Fall])
            elif kind == "v":
                for s in range(NSLOT):
                    for t in range(2):
                        bank = nbank()
                        pv = ps_f32(bank)
                        col = s * SLOTW + HALO + t * P
                        mm_group(pv, [(aTo[:, k, col:col + P], w[:, k, :]) for k in range(KC)],
                                 reads=[B_aTo, B_w], writes=[pbuf[bank]])
                        i = ctr["v"] % 2
                        ctr["v"] += 1
                        vs, B_vs = vost[i]
                        pr.op("act", lambda e, vs=vs, pv=pv: e.copy(out=vs, in_=pv), reads=[pbuf[bank]], writes=[B_vs])
                        r0 = s * L + t * P
                        cv = c0 - 2048
                        pr.dma("sp", Vo_d[r0:r0 + P, cv:cv + 512], vs, "vost%d" % i, reads=[B_vs], writes=[B_Vo])
            elif kind == "uv":
                pending = (w, B_w, c0)
            else:
                wv, B_wv, cv0 = pending
                for sub in range(4):
                    cch = (cv0 - 3072) // P + sub
                    dgv, B_dg = dgb[cch % 2]
                    for jt in range(CW):
                        pr.op("dve", lambda e, dgv=dgv, cch=cch, jt=jt: e.tensor_scalar(
                            out=dgv[:, jt, :], in0=ident, scalar1=wdw[:, cch, jt:jt + 1], scalar2=None, op0=ALU.mult),
                            reads=[B_ident, B_cols], writes=[B_dg])
                    for s in range(NSLOT):
                        bv = nbank()
                        bg = nbank()
                        pval = ps_f32(bv, SLOTW)
                        pgt = ps_f32(bg, SLOTW)
                        rhs_sl = slice(s * SLOTW, (s + 1) * SLOTW)
                        mm_group(pval, [(wv[:, k, sub * P:(sub + 1) * P], aTo[:, k, rhs_sl]) for k in range(KC)],
                                 reads=[B_aTo, B_wv], writes=[pbuf[bv]])
                        mm_group(pgt, [(w[:, k, sub * P:(sub + 1) * P], aTo[:, k, rhs_sl]) for k in range(KC)],
                                 reads=[B_aTo, B_w], writes=[pbuf[bg]])
                        i = ctr["c"] % 2
                        ctr["c"] += 1
                        sg, B_sg = sig[i]
                        hs, B_hs = hst[i]
                        aD, B_aD = accD[i]
                        pr.op("act", lambda e, sg=sg, pgt=pgt, cch=cch: e.activation(
                            out=sg, in_=pgt, func=AF.Sigmoid, bias=bglu[:, 8 + cch:9 + cch], scale=1.0),
                            reads=[pbuf[bg], B_cols], writes=[B_sg])
                        pr.op("dve", lambda e, hs=hs, pval=pval, sg=sg, cch=cch: e.scalar_tensor_tensor(
                            out=hs, in0=pval, scalar=bglu[:, cch:cch + 1], in1=sg, op0=ALU.add, op1=ALU.mult),
                            reads=[pbuf[bv], B_sg, B_cols], writes=[B_hs])
                        pr.op("dve", lambda e, hs=hs, s=s: e.tensor_scalar(
                            out=hs[:, 0:HALO], in0=hs[:, 0:HALO], scalar1=hmask[:, s:s + 1], scalar2=None, op0=ALU.mult),
                            reads=[B_hs, B_hmask], writes=[B_hs])
                        cb = CVB[ctr["c"] % 2]
                        pc = ps_f32(cb, L)
                        mm_group(pc, [(dgv[:, jt, :], hs[:, 2 + jt:2 + jt + L]) for jt in range(CW)],
                                 reads=[B_dg, B_hs], writes=[pbuf[cb]])
                        pr.op("dve", lambda e, aD=aD, pc=pc, cch=cch: e.tensor_scalar(
                            out=aD, in0=pc, scalar1=bdw[:, cch:cch + 1], scalar2=None, op0=ALU.add),
                            reads=[pbuf[cb], B_cols], writes=[B_aD])
                        pr.dma("sp", yT_d[cch, :, s * L:(s + 1) * L], aD, "accD%d" % i, reads=[B_aD], writes=[B_yT])
        for h in range(H):
            pr.dma("sp", F_d[h].rearrange("p (q n) -> p q n", q=16, n=NB), Fall[:, :, h, :], "fst", reads=[B_Fall],
                   writes=[B_F])
        ar.release(mB)
        if stop_after == "B":
            return _finish(nc, pr, st, ar, [B_KT, B_V, B_QT, B_KTo, B_Vo, B_yT, B_F])

        B_mixT = Buf("mixT_d")
        mL = ar.mark()
        ysl = [ar.alloc("ysl%d" % i, [CC, L], F32) for i in range(2)]
        sqs = [ar.alloc("sqs%d" % i, [CC, L], F32) for i in range(2)]
        cst = [ar.alloc("cst%d" % i, [CC, L], BF16) for i in range(2)]
        mean_t, B_mean = ar.alloc("mean_t", [L], F32)
        msq_t, B_msq = ar.alloc("msq_t", [L], F32)
        rstd_t, B_rstdL = ar.alloc("rstd_t", [L], F32)
        mr_t, B_mr = ar.alloc("mr_t", [L], F32)
        tmpL = [ar.alloc("tmpL%d" % i, [L], F32) for i in range(2)]
        yT_v = yT_d.rearrange("c p t -> p c t")
        for s in range(NSLOT):
            i = s % 2
            ys, B_ys = ysl[i]
            sq, B_sq = sqs[i]
            cs, B_cs = cst[i]
            pr.dma("sp", ys, yT_v[:, :, s * L:(s + 1) * L], "ysl%d" % i, reads=[B_yT], writes=[B_ys])
            pr.op("act", lambda e, ys=ys, sq=sq: e.activation(out=sq, in_=ys, func=AF.Square), reads=[B_ys], writes=[B_sq])
            pm = ps_f32(2, L)
            pq2 = ps_f32(3, L)
            mm_group(pm, [(ones32, ys[:, c, :]) for c in range(CC)], reads=[B_ones, B_ys], writes=[pbuf[2]])
            mm_group(pq2, [(ones32, sq[:, c, :]) for c in range(CC)], reads=[B_ones, B_sq], writes=[pbuf[3]])
            pr.op("dve", lambda e, pm=pm: e.tensor_scalar(out=mean_t, in0=pm, scalar1=1.0 / C, scalar2=None, op0=ALU.mult),
                  reads=[pbuf[2]], writes=[B_mean])
            pr.op("dve", lambda e: e.tensor_tensor(out=msq_t, in0=mean_t, in1=mean_t, op=ALU.mult),
                  reads=[B_mean], writes=[B_msq])
            pr.op("dve", lambda e, pq2=pq2: e.scalar_tensor_tensor(out=rstd_t, in0=pq2, scalar=1.0 / C, in1=msq_t,
                                                                  op0=ALU.mult, op1=ALU.subtract),
                  reads=[pbuf[3], B_msq], writes=[B_rstdL])
            pr.op("act", lambda e: e.activation(out=rstd_t, in_=rstd_t, func=AF.Sqrt, bias=LN_EPS, scale=1.0),
                  reads=[B_rstdL], writes=[B_rstdL])
            pr.op("dve", lambda e: e.reciprocal(out=rstd_t, in_=rstd_t), reads=[B_rstdL], writes=[B_rstdL])
            pr.op("dve", lambda e: e.tensor_tensor(out=mr_t, in0=mean_t, in1=rstd_t, op=ALU.mult),
                  reads=[B_mean, B_rstdL], writes=[B_mr])
            for c in range(CC):
                tl, B_tl = tmpL[c % 2]
                pr.op("dve", lambda e, tl=tl, ys=ys, c=c: e.tensor_tensor(out=tl, in0=ys[:, c, :], in1=rstd_t, op=ALU.mult),
                      reads=[B_ys, B_rstdL], writes=[B_tl])
                pr.op("dve", lambda e, tl=tl: e.tensor_tensor(out=tl, in0=tl, in1=mr_t, op=ALU.subtract),
                      reads=[B_tl, B_mr], writes=[B_tl])
                pr.op("act", lambda e, tl=tl, cs=cs, c=c: e.activation(out=cs[:, c, :], in_=tl, func=AF.Silu,
                                                                     bias=lnb[:, c:c + 1], scale=lng[:, c:c + 1]),
                      reads=[B_tl, B_cols], writes=[B_cs])
            pr.dma("sp", mixT_d[C:2 * C, s * L:(s + 1) * L].rearrange("(c p) t -> p c t", p=P), cs, "cst%d" % i,
                   reads=[B_cs], writes=[B_mixT])
        ar.release(mL)
        if stop_after == "L":
            return _finish(nc, pr, st, ar, [B_KT, B_V, B_QT, B_KTo, B_Vo, B_yT, B_F, B_mixT])

        mT = ar.mark()
        NT = S // P
        VW = DH + 2
        KTh = [ar.alloc("KTh%d" % i, [S], BF16) for i in range(2)]
        Vh = [ar.alloc("Vh%d" % i, [NT, VW], BF16) for i in range(2)]
        KToh = [ar.alloc("KToh%d" % i, [NOWN], BF16) for i in range(2)]
        Voh = [ar.alloc("Voh%d" % i, [NOWN // P, VW], BF16) for i in range(2)]
        QTh = [ar.alloc("QTh%d" % i, [NOWN], BF16) for i in range(2)]
        Fh = [ar.alloc("Fh%d" % i, [16, NB], F32) for i in range(2)]
        pTs = [ar.alloc("pT%d" % i, [2, L], BF16) for i in range(3)]
        pTo = [ar.alloc("pTo%d" % i, [3 * P], BF16) for i in range(2)]
        accs = [ar.alloc("acc%d" % i, [2, VW], F32) for i in range(2)]
        rcs = [ar.alloc("rc%d" % i, [2], F32) for i in range(2)]
        obf = [ar.alloc("obf%d" % i, [2, DH], BF16) for i in range(2)]
        ast = [ar.alloc("ast%d" % i, [L], BF16) for i in range(2)]
        for i in range(2):
            pr.op("pool", lambda e, i=i: e.memset(Vh[i][0][:, :, DH:VW], 1.0), writes=[Vh[i][1]])
            pr.op("pool", lambda e, i=i: e.memset(Voh[i][0][:, :, DH:VW], 1.0), writes=[Voh[i][1]])
        SBK = (0, 1, 2)
        OBK = (3, 4, 5)
        TBK = 6

        def T_load(h):
            i = h % 2
            pr.dma("sp", KTh[i][0], KT_d[h], "KTh%d" % i, reads=[B_KT], writes=[KTh[i][1]])
            vsrc = V_d[:, h * DH:(h + 1) * DH].rearrange("(t p) d -> p t d", p=P)
            for q4 in range(4):
                pr.dma("sp", Vh[i][0][:, q4 * 16:(q4 + 1) * 16, 0:DH], vsrc[:, q4 * 16:(q4 + 1) * 16, :], "Vh%d_%d" % (i, q4),
                       reads=[B_V], writes=[Vh[i][1]])
            pr.dma("sp", KToh[i][0], KTo_d[h], "KToh%d" % i, reads=[B_KTo], writes=[KToh[i][1]])
            pr.dma("sp", Voh[i][0][:, :, 0:DH], Vo_d[:, h * DH:(h + 1) * DH].rearrange("(t p) d -> p t d", p=P),
                   "Voh%d" % i, reads=[B_Vo], writes=[Voh[i][1]])
            pr.dma("sp", QTh[i][0], QT_d[h], "QTh%d" % i, reads=[B_QT], writes=[QTh[i][1]])
            pr.dma("sp", Fh[i][0], F_d[h].rearrange("p (q n) -> p q n", q=16, n=NB), "Fh%d" % i, reads=[B_F],
                   writes=[Fh[i][1]])

        uctr = [0]

        def T_head(h):
            i = h % 2
            kth, B_kth = KTh[i]
            vh, B_vh = Vh[i]
            kto, B_kto = KToh[i]
            voh, B_voh = Voh[i]
            qth, B_qth = QTh[i]
            fh, B_fh = Fh[i]
            units = []
            for s in range(NSLOT):
                units.append((s, -1))
                for n in range(PAST[s]):
                    units.append((s, n))
            state = {}
            deferred = []

            def emit_qk(idx):
                s, n = units[idx]
                u = uctr[0]
                uctr[0] += 1
                sb = SBK[u % 3]
                qsl = slice(s * L, (s + 1) * L)
                if n >= 0:
                    sv = ps_f32(sb).rearrange("p (t q) -> p t q", t=2, q=L)

                    def qk(e, sv=sv, n=n, qsl=qsl):
                        e.matmul(sv[:, 0, :], lhsT=kth[:, (2 * n) * P:(2 * n + 1) * P], rhs=qth[:, qsl], start=True, stop=True)
                        return e.matmul(sv[:, 1, :], lhsT=kth[:, (2 * n + 1) * P:(2 * n + 2) * P], rhs=qth[:, qsl],
                                        start=True, stop=True)
                    pr.op("pe", qk, reads=[B_kth, B_qth], writes=[pbuf[sb]])
                    pt, B_pt = pTs[u % 3]
                    for t in range(2):
                        pr.op("act", lambda e, pt=pt, sv=sv, t=t: e.activation(
                            out=pt[:, t, :], in_=sv[:, t, :], func=AF.Exp, bias=bkt[:, h, t:t + 1], scale=SCALE),
                            reads=[pbuf[sb], B_bkt], writes=[B_pt])
                    state[idx] = (u, pt, B_pt)
                else:
                    sv = ps_f32(sb, 3 * P)
                    q0 = s * L

                    def qk(e, sv=sv, q0=q0):
                        e.matmul(sv[:, 0:P], lhsT=kto[:, q0:q0 + P], rhs=qth[:, q0:q0 + P], start=True, stop=True)
                        e.matmul(sv[:, P:2 * P], lhsT=kto[:, q0 + P:q0 + 2 * P], rhs=qth[:, q0 + P:q0 + 2 * P],
                                 start=True, stop=True)
                        return e.matmul(sv[:, 2 * P:3 * P], lhsT=kto[:, q0:q0 + P], rhs=qth[:, q0 + P:q0 + 2 * P],
                                        start=True, stop=True)
                    pr.op("pe", qk, reads=[B_kto, B_qth], writes=[pbuf[sb]])
                    pt, B_pt = pTo[s % 2]
                    pr.op("act", lambda e, pt=pt, sv=sv: e.activation(
                        out=pt[:, 0:2 * P], in_=sv[:, 0:2 * P], func=AF.Exp, bias=bkt[:, h, 1:2], scale=SCALE),
                        reads=[pbuf[sb], B_bkt], writes=[B_pt])
                    pr.op("act", lambda e, pt=pt, sv=sv: e.activation(
                        out=pt[:, 2 * P:3 * P], in_=sv[:, 2 * P:3 * P], func=AF.Exp, bias=bkt[:, h, 0:1], scale=SCALE),
                        reads=[pbuf[sb], B_bkt], writes=[B_pt])
                    for a in range(2):
                        pr.op("pool", lambda e, pt=pt, a=a: e.tensor_tensor(
                            out=pt[:, a * P:(a + 1) * P], in0=pt[:, a * P:(a + 1) * P], in1=tri, op=ALU.mult),
                            reads=[B_pt, B_tri], writes=[B_pt])
                    state[idx] = (u, pt, B_pt)

            def emit_pv(idx):
                s, n = units[idx]
                u, pt, B_pt = state.pop(idx)
                ob = OBK[u % 3]
                ov = ps_f32(ob, 2 * VW).rearrange("p (t d) -> p t d", t=2, d=VW)
                acc, B_acc = accs[s % 2]
                NV = DH + 1
                if n >= 0:
                    def pv(e, ov=ov, pt=pt, n=n):
                        ins = None
                        for qt in range(2):
                            for t in range(2):
                                ins = e.matmul(ov[:, qt, 0:NV], lhsT=pt[:, t, qt * P:(qt + 1) * P],
                                               rhs=vh[:, 2 * n + t, 0:NV], start=(t == 0), stop=(t == 1))
                        return ins
                    pr.op("pe", pv, reads=[B_pt, B_vh], writes=[pbuf[ob]])
                    for qt in range(2):
                        pr.op("dve", lambda e, ov=ov, acc=acc, qt=qt, n=n, s=s: e.scalar_tensor_tensor(
                            out=acc[:, qt, 0:NV], in0=ov[:, qt, 0:NV], scalar=fh[:, 2 * s + qt, n:n + 1],
                            in1=acc[:, qt, 0:NV], op0=ALU.mult, op1=ALU.add),
                            reads=[pbuf[ob], B_fh, B_acc], writes=[B_acc])
                else:
                    def pv(e, ov=ov, pt=pt, s=s):
                        e.matmul(ov[:, 0, 0:NV], lhsT=pt[:, 0:P], rhs=voh[:, 2 * s, 0:NV], start=True, stop=True)
                        e.matmul(ov[:, 1, 0:NV], lhsT=pt[:, 2 * P:3 * P], rhs=voh[:, 2 * s, 0:NV], start=True, stop=False)
                        return e.matmul(ov[:, 1, 0:NV], lhsT=pt[:, P:2 * P], rhs=voh[:, 2 * s + 1, 0:NV],
                                        start=False, stop=True)
                    pr.op("pe", pv, reads=[B_pt, B_voh], writes=[pbuf[ob]])
                    for qt in range(2):
                        pr.op("dve", lambda e, ov=ov, acc=acc, qt=qt: e.tensor_scalar(
                            out=acc[:, qt, 0:NV], in0=ov[:, qt, 0:NV], scalar1=fown[:, h:h + 1], scalar2=None,
                            op0=ALU.mult), reads=[pbuf[ob], B_fown], writes=[B_acc])
                last = (idx + 1 == len(units)) or (units[idx + 1][0] != s)
                if last:
                    deferred.append([2, s])

            def finalize(s):
                acc, B_acc = accs[s % 2]
                rc, B_rc = rcs[s % 2]
                ob_, B_ob = obf[s % 2]
                asv, B_as = ast[s % 2]
                pr.op("dve", lambda e: e.reciprocal(out=rc, in_=acc[:, :, DH]), reads=[B_acc], writes=[B_rc])
                for qt in range(2):
                    pr.op("dve", lambda e, qt=qt: e.tensor_scalar(out=ob_[:, qt, :], in0=acc[:, qt, 0:DH],
                                                                  scalar1=rc[:, qt:qt + 1], scalar2=None, op0=ALU.mult),
                          reads=[B_acc, B_rc], writes=[B_ob])
                ptv = ps_bf16(TBK)[:, 0:L].rearrange("p (t q) -> p t q", t=2, q=P)

                def tr(e):
                    e.transpose(out=ptv[:, 0, :], in_=ob_[:, 0, :], identity=ident)
                    return e.transpose(out=ptv[:, 1, :], in_=ob_[:, 1, :], identity=ident)
                pr.op("pe", tr, reads=[B_ob, B_ident], writes=[pbuf[TBK]])
                pr.op("act", lambda e: e.copy(out=asv, in_=ps_bf16(TBK)[:, 0:L]), reads=[pbuf[TBK]], writes=[B_as])
                pr.dma("sp", mixT_d[h * DH:(h + 1) * DH, s * L:(s + 1) * L], asv, "ast%d" % (s % 2), reads=[B_as],
                       writes=[B_mixT])

            def tick():
                for dd in list(deferred):
                    dd[0] -= 1
                    if dd[0] <= 0:
                        deferred.remove(dd)
                        finalize(dd[1])

            emit_qk(0)
            for idx in range(len(units)):
                if idx + 1 < len(units):
                    emit_qk(idx + 1)
                emit_pv(idx)
                tick()
            for dd in list(deferred):
                finalize(dd[1])
            deferred.clear()

        T_load(0)
        for h in range(H):
            if h + 1 < H:
                T_load(h + 1)
            T_head(h)
        ar.release(mT)
        if stop_after == "T":
            return _finish(nc, pr, st, ar, [B_mixT])

        gb_t = []
        for gi_, nm in ((1, "gpost"), (2, "gffn"), (3, "gfpost")):
            t_, b_ = ar.alloc(nm, [D], F32)
            pr.dma("sp", t_, grow_d[gi_:gi_ + 1, :].partition_broadcast(P), "c7_%d" % gi_, writes=[b_])
            gb_t.append((t_, b_))
        (gpost_b, B_gpost), (gffn_b, B_gffn), (gfpost_b, B_gfpost) = gb_t
        mixS, B_mixS = ar.alloc("mixS", [KC, 512], BF16)
        fT, B_fT = ar.alloc("fT", [KC, 512], BF16)
        accF, _ = ar.alloc("accF", [4, D], F32)
        B_accF = [Buf("accF%d" % t) for t in range(4)]
        hTs = [ar.alloc("hT%d" % i, [4, 512], BF16) for i in range(2)]
        rts = [ar.alloc("rt%d" % i, [512], F32) for i in range(2)]
        NWS2 = 4
        wc2 = [ar.alloc("wc2_%d" % i, [KC, 512], BF16) for i in range(NWS2)]
        xsC = [ar.alloc("xsC%d" % i, [D], F32) for i in range(2)]
        abC = [ar.alloc("abC%d" % i, [D], BF16) for i in range(2)]
        w2ctr = [0]

        def load_w2(src_ap, reads):
            i = w2ctr[0] % NWS2
            w2ctr[0] += 1
            w, B_w = wc2[i]
            pr.dma("sp", w, src_ap, "wch%d" % i, reads=reads, writes=[B_w])
            return w, B_w
        B_y = [Buf("y%d" % t) for t in range(NOWN // P)]
        CB = (2, 3)
        HBK = (4, 5)
        cb_ctr = [0]
        xc_ctr = [0]

        def rstd_of(src, B_src, junk, B_junk):
            i = stat_ctr[0] % 8
            stat_ctr[0] += 1
            ssv = ss_t[:, i:i + 1]
            rsv = rs_t[:, i:i + 1]
            pr.op("act", lambda e: e.activation(out=junk, in_=src, func=AF.Square, accum_out=ssv),
                  reads=[B_src], writes=[B_ss[i], B_junk])
            pr.op("act", lambda e: e.activation(out=rsv, in_=ssv, func=AF.Sqrt, bias=RMS_EPS, scale=1.0 / D),
                  reads=[B_ss[i]], writes=[B_rs[i]])
            pr.op("dve", lambda e: e.reciprocal(out=rsv, in_=rsv), reads=[B_rs[i]], writes=[B_rs[i]])
            return rsv, B_rs[i]

        for gi in range(4):
            pr.dma("sp", mixS, mixT_d[:, gi * 512:(gi + 1) * 512].rearrange("(k p) t -> p k t", p=P), "mixS",
                   reads=[B_mixT], writes=[B_mixS])
            nxt = load_w2(w_out_bf[:, 0:512].rearrange("(k p) n -> p k n", p=P), B_wout)
            for n in range(4):
                w, B_w = nxt
                if n + 1 < 4:
                    nxt = load_w2(w_out_bf[:, (n + 1) * 512:(n + 2) * 512].rearrange("(k p) n -> p k n", p=P), B_wout)
                else:
                    nxtW1 = [load_w2(w_ff1_bf[:, 0:512].rearrange("(k p) n -> p k n", p=P), B_wff1[0])]
                for t in range(4):
                    bank = CB[cb_ctr[0] % 2]
                    cb_ctr[0] += 1
                    po = ps_f32(bank)
                    mm_group(po, [(mixS[:, k, t * P:(t + 1) * P], w[:, k, :]) for k in range(KC)],
                             reads=[B_mixS, B_w], writes=[pbuf[bank]])
                    pr.op("act", lambda e, po=po, t=t, n=n: e.copy(out=accF[:, t, n * 512:(n + 1) * 512], in_=po),
                          reads=[pbuf[bank]], writes=[B_accF[t]])
            for t in range(4):
                i = xc_ctr[0] % 2
                xc_ctr[0] += 1
                xs, B_xs = xsC[i]
                ab, B_ab = abC[i]
                s_ = 2 * gi + t // 2
                r0 = s_ * SLOTW + HALO + (t % 2) * P
                pr.dma("sp", xs, xown_d[r0:r0 + P, :], "xsA%d" % i, writes=[B_xs])
                rsv, B_r = rstd_of(accF[:, t, :], B_accF[t], ab, B_ab)
                pr.op("dve", lambda e, t=t, rsv=rsv: e.scalar_tensor_tensor(
                    out=accF[:, t, :], in0=accF[:, t, :], scalar=rsv, in1=gpost_b, op0=ALU.mult, op1=ALU.mult),
                    reads=[B_accF[t], B_r, B_gpost], writes=[B_accF[t]])
                pr.op("dve", lambda e, t=t, xs=xs: e.tensor_tensor(out=xs, in0=accF[:, t, :], in1=xs, op=ALU.add),
                      reads=[B_accF[t], B_xs], writes=[B_xs])
                yt = gi * 4 + t
                pr.dma("sp", y_d[yt * P:(yt + 1) * P, :], xs, "xsSt%d" % i, reads=[B_xs], writes=[B_y[yt]])
                norm_T_tile(None, xs, B_xs, None, gffn_b, B_gffn, ab, B_ab, fT, B_fT, t * P, TB)
            NFC = DFF // 512

            def w1src(c):
                return w_ff1_bf[:, c * 512:(c + 1) * 512].rearrange("(k p) n -> p k n", p=P), B_wff1[c // 4]

            def w2src(c):
                return w_ff2_bf[c * 512:(c + 1) * 512, :].rearrange("(j p) n -> p j n", p=P), B_wff2[c // 4]
            W1 = {0: nxtW1[0]}
            W2 = {}
            W1[1] = load_w2(*w1src(1))
            W2[0] = load_w2(*w2src(0))

            def ffn_H(c):
                w, B_w = W1.pop(c)
                hT, B_hT = hTs[c % 2]
                for j in range(4):
                    bank = HBK[j % 2]
                    ph = ps_f32(bank)
                    mm_group(ph, [(w[:, k, j * P:(j + 1) * P], fT[:, k, :]) for k in range(KC)],
                             reads=[B_w, B_fT], writes=[pbuf[bank]])
                    rt, B_rt = rts[j % 2]
                    pr.op("act", lambda e, rt=rt, ph=ph: e.activation(out=rt, in_=ph, func=AF.Relu),
                          reads=[pbuf[bank]], writes=[B_rt])
                    pr.op("act", lambda e, rt=rt, hT=hT, j=j: e.activation(out=hT[:, j, :], in_=rt, func=AF.Square),
                          reads=[B_rt], writes=[B_hT])

            def ffn_O(c):
                w, B_w = W2.pop(c)
                wv_ = w.rearrange("p k n -> p (k n)").rearrange("p (j n) -> p j n", j=4, n=D)
                hT, B_hT = hTs[c % 2]
                for t in range(4):
                    for n in range(4):
                        bank = CB[cb_ctr[0] % 2]
                        cb_ctr[0] += 1
                        po = ps_f32(bank)
                        mm_group(po, [(hT[:, j, t * P:(t + 1) * P], wv_[:, j, n * 512:(n + 1) * 512]) for j in range(4)],
                                 reads=[B_hT, B_w], writes=[pbuf[bank]])
                        dst = accF[:, t, n * 512:(n + 1) * 512]
                        if c == 0:
                            pr.op("dve", lambda e, dst=dst, po=po: e.tensor_copy(out=dst, in_=po),
                                  reads=[pbuf[bank]], writes=[B_accF[t]])
                        else:
                            pr.op("dve", lambda e, dst=dst, po=po: e.tensor_tensor(out=dst, in0=po, in1=dst, op=ALU.add),
                                  reads=[pbuf[bank], B_accF[t]], writes=[B_accF[t]])
            ffn_H(0)
            for c in range(NFC):
                if c + 2 < NFC:
                    W1[c + 2] = load_w2(*w1src(c + 2))
                if c + 1 < NFC:
                    W2[c + 1] = load_w2(*w2src(c + 1))
                    ffn_H(c + 1)
                ffn_O(c)
            for t in range(4):
                i = xc_ctr[0] % 2
                xc_ctr[0] += 1
                xs, B_xs = xsC[i]
                ab, B_ab = abC[i]
                yt = gi * 4 + t
                pr.dma("sp", xs, y_d[yt * P:(yt + 1) * P, :], "xsA%d" % i, reads=[B_y[yt]], writes=[B_xs])
                rsv, B_r = rstd_of(accF[:, t, :], B_accF[t], ab, B_ab)
                pr.op("dve", lambda e, t=t, rsv=rsv: e.scalar_tensor_tensor(
                    out=accF[:, t, :], in0=accF[:, t, :], scalar=rsv, in1=gfpost_b, op0=ALU.mult, op1=ALU.mult),
                    reads=[B_accF[t], B_r, B_gfpost], writes=[B_accF[t]])
                pr.op("dve", lambda e, t=t, xs=xs: e.tensor_tensor(out=xs, in0=accF[:, t, :], in1=xs, op=ALU.add),
                      reads=[B_accF[t], B_xs], writes=[B_xs])
                o = pr.dma("sp", y_d[yt * P:(yt + 1) * P, :], xs, "xsSt%d" % i, reads=[B_xs], writes=[B_y[yt]])
                pr.must_finish(o)
        return _finish(nc, pr, st, ar, [])


def _finish(nc, pr, st, ar, bufs):
    for b in bufs:
        if b.writer is not None:
            pr.must_finish(b.writer)
    pr.emit_all(st)
    nc._mk_info = dict(n_sems=pr.n_sems, peak=ar.peak, nops=len(pr.all_ops), counts=pr.max_count)
    return nc


def alibi_slopes():
    return (2.0 ** (-8.0 * np.arange(1, H + 1) / H)).astype(np.float64)


def core_tables(j):
    blks = own_blocks(j)
    sl = alibi_slopes()
    fd = np.zeros((NSLOT, P, 2, H, NB), np.float64)
    gb = np.full((P, NSLOT, NB), -1e30, np.float32)
    hm = np.ones((P, NSLOT), np.float32)
    q = np.arange(P)
    for s, blk in enumerate(blks):
        if blk == 0:
            hm[:, s] = 0.0
        for n in range(blk):
            gb[:, s, n] = 0.0
            for qt in range(2):
                dist = 256 * (n - blk) + 255 - (128 * qt + q)
                fd[s, :, qt, :, n] = np.exp(sl[None, :] * dist[:, None])
    return (fd.reshape(NSLOT, P, 2 * H * NB).astype(np.float32), gb.reshape(P, NSLOT * NB), hm)


def const_tables():
    sl = alibi_slopes()
    p = np.arange(P)
    bkt = np.zeros((P, H, 2), np.float64)
    for t in range(2):
        bkt[:, :, t] = sl[None, :] * (t * 128 + p[:, None] - 255)
    fown = np.exp(sl[None, :] * (127 - p[:, None]))
    tri = (p[None, :] >= p[:, None]).astype(np.float32)
    return (bkt.reshape(P, 2 * H).astype(np.float32), fown.astype(np.float32),
            tri.astype(ml_dtypes.bfloat16), np.eye(P, dtype=np.float32).astype(ml_dtypes.bfloat16))


def make_in_maps(inputs, cores=range(8)):
    x = np.asarray(inputs["x"], np.float32)
    f = lambda k: np.ascontiguousarray(np.asarray(inputs[k], np.float32)[0])
    w_in, w_out, w_ff1, w_ff2 = f("w_in"), f("w_out"), f("w_ff1"), f("w_ff2")
    grow = np.stack([f("g_mix_pre"), f("g_mix_post"), f("g_ffn_pre"), f("g_ffn_post")]).astype(np.float32)
    b_glu, w_dw, b_dw, ln_g, ln_b = f("b_glu"), f("w_dw"), f("b_dw"), f("ln_conv_g"), f("ln_conv_b")
    cols = np.concatenate([
        b_glu.reshape(16, P).T,
        w_dw.reshape(CW, CC, P).transpose(2, 1, 0).reshape(P, CC * CW),
        b_dw.reshape(CC, P).T, ln_g.reshape(CC, P).T, ln_b.reshape(CC, P).T], axis=1).astype(np.float32)
    bkt, fown, tri, ident = const_tables()
    maps = []
    for c in cores:
        bi, j = c // 4, c % 4
        fd, gb, hm = core_tables(j)
        xo = np.zeros((NSLOT, SLOTW, D), np.float32)
        for s, blk in enumerate(own_blocks(j)):
            lo = L * blk - HALO
            if lo < 0:
                xo[s, HALO:] = x[bi, 0:L]
            else:
                xo[s] = x[bi, lo:lo + SLOTW]
        maps.append(dict(xall=np.ascontiguousarray(x[bi]), xown=xo.reshape(NOWNH, D), w_in=w_in, w_out=w_out,
                         w_ff1=w_ff1, w_ff2=w_ff2, grow=grow, cols=np.ascontiguousarray(cols), fd=fd, gbias=gb,
                         hmask=hm, bkt=bkt, fown=fown, tri=tri, ident=ident))
    return maps


_NC = None


def kernel(**inputs):
    global _NC
    if _NC is None:
        _NC = build_nc()
    maps = make_in_maps(inputs)
    res = run_bass_kernel_spmd(_NC, maps, core_ids=list(range(8)))
    x = np.asarray(inputs["x"])
    out = np.zeros(x.shape, np.float32)
    for c in range(8):
        bi, j = c // 4, c % 4
        y = np.asarray(res.results[c]["y"])
        for s, blk in enumerate(own_blocks(j)):
            out[bi, L * blk:L * (blk + 1)] = y[s * L:(s + 1) * L]
    return out
```

```python
import contextlib
import numpy as np
import ml_dtypes
import concourse.bass as bass
import concourse.mybir as mybir
from concourse.bass_utils import run_bass_kernel_spmd

F32 = mybir.dt.float32
BF16 = mybir.dt.bfloat16
ALU = mybir.AluOpType
AF = mybir.ActivationFunctionType
AX = mybir.AxisListType

P = 128
D = 2048
KC = 16
S = 8192
NB = 32
L = 256
H = 8
DH = 128
C = 1024
CC = 8
DFF = 8192
INC = 5120
NSLOT = 8
HALO = 32
SLOTW = L + HALO
NOWN = NSLOT * L
NOWNH = NSLOT * SLOTW
SCALE = DH ** -0.5
RMS_EPS = 1e-6
LN_EPS = 1e-5
CW = 31

STREAMS = ("pe", "act", "dve", "pool", "sp")


class Buf:
    __slots__ = ("name", "writer", "readers", "inherit", "excl")

    def __init__(self, name, inherit=(), excl=False):
        self.name = name
        self.excl = excl
        self.writer = None
        self.readers = []
        self.inherit = list(inherit)


class Op:
    __slots__ = ("stream", "emit", "deps", "is_dma", "semkey", "signal", "name")

    def __init__(self, stream, emit, is_dma=False, semkey=None, name=""):
        self.stream = stream
        self.emit = emit
        self.deps = []
        self.is_dma = is_dma
        self.semkey = semkey
        self.signal = False
        self.name = name


class Prog:
    def __init__(self, nc):
        self.nc = nc
        self.ops = {s: [] for s in STREAMS}
        self.all_ops = []
        self.final_waits = []

    def _add(self, op, reads, writes, after=()):
        deps = []
        for b in after:
            if b.writer is not None:
                deps.append(b.writer)
        for b in reads:
            if b.writer is not None:
                deps.append(b.writer)
            elif b.inherit:
                deps.extend(b.inherit)
            if b.excl:
                deps.extend(r for r in b.readers if r.stream != op.stream)
        for b in writes:
            if b.writer is not None:
                deps.append(b.writer)
            deps.extend(b.readers)
            if b.inherit:
                deps.extend(b.inherit)
                b.inherit = []
        seen = set()
        for d in deps:
            if d is op or id(d) in seen:
                continue
            seen.add(id(d))
            op.deps.append(d)
        for b in reads:
            b.readers.append(op)
        for b in writes:
            b.writer = op
            b.readers = []
        self.ops[op.stream].append(op)
        self.all_ops.append(op)
        return op

    def op(self, stream, emit, reads=(), writes=(), name=""):
        return self._add(Op(stream, emit, name=name), reads, writes)

    def dma(self, stream, out, in_, semkey, reads=(), writes=(), name="", after=()):
        def emit(eng):
            return eng.dma_start(out=out, in_=in_)
        return self._add(Op(stream, emit, is_dma=True, semkey=semkey, name=name), reads, writes, after)

    def must_finish(self, op):
        self.final_waits.append(op)

    def emit_all(self, stack):
        nc = self.nc
        for op in self.all_ops:
            for d in op.deps:
                d.signal = True
        for op in self.final_waits:
            op.signal = True
        eng_sem = {s: stack.enter_context(nc.semaphore("done_" + s)) for s in STREAMS}
        dma_sems = {}
        dma_cnt = {}
        cnt = {s: 0 for s in STREAMS}
        comp = {}
        for op in self.all_ops:
            s = op.stream
            if op.is_dma:
                k = op.semkey
                if k not in dma_sems:
                    dma_sems[k] = stack.enter_context(nc.semaphore("dq_%d" % len(dma_sems)))
                    dma_cnt[k] = 0
                dma_cnt[k] += 16
                comp[id(op)] = (dma_sems[k], dma_cnt[k])
            elif op.signal:
                cnt[s] += 1
                comp[id(op)] = (eng_sem[s], cnt[s])
        self.n_sems = len(dma_sems) + len(STREAMS)
        self.max_count = dict(cnt)
        block = stack.enter_context(nc.Block())
        prog = self

        def run_stream(s, eng):
            waited = {}
            for op in prog.ops[s]:
                need = {}
                for d in op.deps:
                    sem, val = comp[id(d)]
                    key = id(sem)
                    if waited.get(key, 0) >= val:
                        continue
                    if key not in need or need[key][1] < val:
                        need[key] = (sem, val)
                for key, (sem, val) in need.items():
                    waited[key] = val
                    eng.wait_ge(sem, val)
                ins = op.emit(eng)
                if op.is_dma:
                    ins.then_inc(comp[id(op)][0], 16)
                elif op.signal:
                    ins.then_inc(eng_sem[s], 1)
            if s == "sp":
                for op in prog.final_waits:
                    sem, val = comp[id(op)]
                    eng.wait_ge(sem, val)

        @block.tensor
        def _(e):
            run_stream("pe", e)

        @block.scalar
        def _(e):
            run_stream("act", e)

        @block.vector
        def _(e):
            run_stream("dve", e)

        @block.gpsimd
        def _(e):
            run_stream("pool", e)

        @block.sync
        def _(e):
            run_stream("sp", e)


DT_SIZE = {F32: 4, BF16: 2}


class Arena:
    def __init__(self, nc, stack, kib):
        self.words = kib * 256
        self.t = stack.enter_context(nc.sbuf_tensor("arena", [P, self.words], F32))
        self.top = 0
        self.peak = 0
        self.retired = []
        self.live = []

    def alloc(self, name, shape, dtype):
        n = 1
        for s in shape:
            n *= s
        nbytes = (n * DT_SIZE[dtype] + 31) // 32 * 32
        lo = self.top
        hi = lo + nbytes
        assert hi <= self.words * 4, "SBUF arena overflow at %s: %d > %d" % (name, hi, self.words * 4)
        self.top = hi
        self.peak = max(self.peak, hi)
        inh = []
        keep = []
        for (l, h, ops) in self.retired:
            if l < hi and h > lo:
                inh.extend(ops)
                if l >= lo and h <= hi:
                    continue
            keep.append((l, h, ops))
        self.retired = keep
        buf = Buf(name, inherit=inh)
        v = self.t[:, lo // 4:hi // 4]
        if dtype != F32:
            v = v.bitcast(dtype)
        v = v[:, 0:n]
        if len(shape) == 2:
            v = v.rearrange("p (a b) -> p a b", a=shape[0], b=shape[1])
        elif len(shape) == 3:
            v = v.rearrange("p (a b c) -> p a b c", a=shape[0], b=shape[1], c=shape[2])
        elif len(shape) == 4:
            v = v.rearrange("p (a b c d) -> p a b c d", a=shape[0], b=shape[1], c=shape[2], d=shape[3])
        self.live.append((lo, hi, buf))
        return v, buf

    def mark(self):
        return self.top

    def release(self, mark):
        keep = []
        for (lo, hi, buf) in self.live:
            if lo >= mark:
                ops = list(buf.readers) + list(buf.inherit)
                if buf.writer is not None:
                    ops.append(buf.writer)
                if ops:
                    self.retired.append((lo, hi, ops))
            else:
                keep.append((lo, hi, buf))
        self.live = keep
        self.top = mark


def own_blocks(j):
    return [8 * (s // 2) + (j if s % 2 == 0 else 7 - j) for s in range(NSLOT)]


PAST = [8 * (s // 2) + (3 if s % 2 == 0 else 7) for s in range(NSLOT)]


def build_nc(debug=False, stop_after="all"):
    nc = bass.Bass("TRN2", target_bir_lowering=False)
    skind = "ExternalOutput" if debug else "Internal"

    def din(name, shape, dt=F32):
        return nc.dram_tensor(name, list(shape), dt, kind="ExternalInput").ap()

    def dscr(name, shape, dt):
        return nc.dram_tensor(name, list(shape), dt, kind=skind).ap()

    xall_d = din("xall", [S, D])
    xown_d = din("xown", [NOWNH, D])
    w_in_d = din("w_in", [D, INC])
    w_out_d = din("w_out", [D, D])
    w_ff1_d = din("w_ff1", [D, DFF])
    w_ff2_d = din("w_ff2", [DFF, D])
    grow_d = din("grow", [4, D])
    cols_d = din("cols", [P, 16 + CC * CW + 3 * CC])
    fd_d = din("fd", [NSLOT, P, 2 * H * NB])
    gbias_d = din("gbias", [P, NSLOT * NB])
    hmask_d = din("hmask", [P, NSLOT])
    bkt_d = din("bkt", [P, H * 2])
    fown_d = din("fown", [P, H])
    cvec_d = din("cvec", [P, H * DH])
    tri_d = din("tri", [P, P], BF16)
    ident_d = din("ident", [P, P], BF16)

    y_d = nc.dram_tensor("y", [NOWN, D], F32, kind="ExternalOutput").ap()

    w_in_bf = dscr("w_in_bf", [D, INC], BF16)
    w_out_bf = dscr("w_out_bf", [D, D], BF16)
    w_ff1_bf = dscr("w_ff1_bf", [D, DFF], BF16)
    w_ff2_bf = dscr("w_ff2_bf", [DFF, D], BF16)
    KT_d = dscr("KT", [H, DH, S], BF16)
    V_d = dscr("V", [S, H * DH], BF16)
    KTo_d = dscr("KTo", [H, DH, NOWN], BF16)
    Vo_d = dscr("Vo", [NOWN, H * DH], BF16)
    QT_d = dscr("QT", [H, DH, NOWN], BF16)
    F_d = dscr("Fsel", [H, P, 16 * NB], F32)
    yT_d = dscr("yT", [CC, P, NOWN], F32)
    mixT_d = dscr("mixT", [D, NOWN], BF16)
    kmean_dbg = dscr("kmean_dbg", [P, H * NB], F32) if debug else None

    with contextlib.ExitStack() as st:
        pr = Prog(nc)
        ar = Arena(nc, st, 204)
        psum = []
        pbuf = []
        for i in range(8):
            psum.append(st.enter_context(nc.psum_tensor("ps%d" % i, [P, 512], F32)))
            pbuf.append(Buf("ps%d" % i, excl=True))

        def ps_f32(i, n=512):
            return psum[i][:, 0:n]

        def ps_bf16(i):
            return psum[i][:, :].bitcast(BF16)

        cols, B_cols = ar.alloc("cols", [16 + CC * CW + 3 * CC], F32)
        pr.dma("sp", cols, cols_d, "c0", writes=[B_cols])
        bglu = cols[:, 0:16]
        wdw = cols[:, 16:16 + CC * CW].rearrange("p (c j) -> p c j", c=CC, j=CW)
        o0 = 16 + CC * CW
        bdw = cols[:, o0:o0 + CC]
        lng = cols[:, o0 + CC:o0 + 2 * CC]
        lnb = cols[:, o0 + 2 * CC:o0 + 3 * CC]
        ident, B_ident = ar.alloc("ident", [P], BF16)
        pr.dma("sp", ident, ident_d, "c1", writes=[B_ident])
        tri, B_tri = ar.alloc("tri", [P], BF16)
        pr.dma("sp", tri, tri_d, "c2", writes=[B_tri])
        bkt, B_bkt = ar.alloc("bkt", [H, 2], F32)
        pr.dma("sp", bkt, bkt_d.rearrange("p (h t) -> p h t", h=H, t=2), "c3", writes=[B_bkt])
        fown, B_fown = ar.alloc("fown", [H], F32)
        pr.dma("sp", fown, fown_d, "c4", writes=[B_fown])
        gbias, B_gbias = ar.alloc("gbias", [NSLOT, NB], F32)
        pr.dma("sp", gbias, gbias_d.rearrange("p (s n) -> p s n", s=NSLOT, n=NB), "c5", writes=[B_gbias])
        hmask, B_hmask = ar.alloc("hmask", [NSLOT], F32)
        pr.dma("sp", hmask, hmask_d, "c6", writes=[B_hmask])
        cvec, B_cvec = ar.alloc("cvec", [H * DH], F32)
        pr.dma("sp", cvec, cvec_d, "c8", writes=[B_cvec])
        kmean, B_kmean = ar.alloc("kmean", [H, NB], F32)
        ones32, B_ones = ar.alloc("ones32", [P], F32)
        pr.op("pool", lambda e: e.memset(ones32, 1.0), writes=[B_ones])
        ss_t, _ = ar.alloc("ss", [8], F32)
        rs_t, _ = ar.alloc("rs", [8], F32)
        B_ss = [Buf("ss%d" % i) for i in range(8)]
        B_rs = [Buf("rs%d" % i) for i in range(8)]
        stat_ctr = [0]

        def in_col_bufs(c0):
            if c0 < 1024:
                return B_wq
            if c0 < 3072:
                return B_wkv
            return B_wu

        def norm_T_tile(x_src, xs, B_xs, xs_key, g_b, B_gb, abf, B_abf, dstT, B_dstT, col0, tbanks):
            i = stat_ctr[0] % 8
            stat_ctr[0] += 1
            ssv = ss_t[:, i:i + 1]
            rsv = rs_t[:, i:i + 1]
            if x_src is not None:
                pr.dma("sp", xs, x_src, xs_key, writes=[B_xs])
            pr.op("act", lambda e: e.activation(out=abf, in_=xs, func=AF.Square, accum_out=ssv),
                  reads=[B_xs], writes=[B_ss[i], B_abf])
            pr.op("act", lambda e: e.activation(out=rsv, in_=ssv, func=AF.Sqrt, bias=RMS_EPS, scale=1.0 / D),
                  reads=[B_ss[i]], writes=[B_rs[i]])
            pr.op("dve", lambda e: e.reciprocal(out=rsv, in_=rsv), reads=[B_rs[i]], writes=[B_rs[i]])
            pr.op("dve", lambda e: e.scalar_tensor_tensor(out=abf, in0=xs, scalar=rsv, in1=g_b,
                                                          op0=ALU.mult, op1=ALU.mult),
                  reads=[B_xs, B_rs[i], B_gb], writes=[B_abf])
            for half in range(2):
                bank = tbanks[half]
                pt = ps_bf16(bank).rearrange("p (k n) -> p k n", k=8, n=P)

                def tr(e, half=half, pt=pt):
                    ins = None
                    for kk in range(8):
                        k = half * 8 + kk
                        ins = e.transpose(out=pt[:, kk, :], in_=abf[:, k * P:(k + 1) * P], identity=ident)
                    return ins
                pr.op("pe", tr, reads=[B_abf, B_ident], writes=[pbuf[bank]])
                dst = dstT[:, half * 8:(half + 1) * 8, col0:col0 + P]
                if half == 0:
                    pr.op("act", lambda e, dst=dst, pt=pt: e.copy(out=dst, in_=pt), reads=[pbuf[bank]], writes=[B_dstT])
                else:
                    pr.op("dve", lambda e, dst=dst, pt=pt: e.tensor_copy(out=dst, in_=pt), reads=[pbuf[bank]],
                          writes=[B_dstT])

        def mm_group(out_ap, pairs, reads, writes, name=""):
            def emit(e):
                ins = None
                n = len(pairs)
                for i, (l, r) in enumerate(pairs):
                    ins = e.matmul(out_ap, lhsT=l, rhs=r, start=(i == 0), stop=(i == n - 1))
                return ins
            return pr.op("pe", emit, reads=reads, writes=writes, name=name)

        mA = ar.mark()
        gpre_b, B_gpre = ar.alloc("gpre_b", [D], F32)
        pr.dma("sp", gpre_b, grow_d[0:1, :].partition_broadcast(P), "c7", writes=[B_gpre])
        wkv, _ = ar.alloc("wkv", [KC, 2048], BF16)
        B_wkvS = [Buf("wkvS%d" % i) for i in range(4)]
        wsrc = w_in_d[:, 1024:3072].rearrange("(k p) n -> p k n", p=P)
        for i in range(4):
            pr.dma("pool", wkv[:, 4 * i:4 * i + 4, :], wsrc[:, 4 * i:4 * i + 4, :], "wkvS%d" % i, writes=[B_wkvS[i]])
        bg_queue = []

        def cast_group(name, dst, src, pieces):
            bufs = []
            for i, (dsl, ssl) in enumerate(pieces):
                b = Buf("%s_%d" % (name, i))
                bg_queue.append((dst[dsl], src[ssl], "cast_" + name, b))
                bufs.append(b)
            return bufs

        def bg_issue(n, gates):
            for _ in range(n):
                if not bg_queue:
                    return
                d_, s_, k_, b_ = bg_queue.pop(0)
                pr.dma("pool", d_, s_, k_, writes=[b_], after=gates)

        def issued(bufs):
            assert all(b.writer is not None for b in bufs), "weight cast not issued before its consumer"
            return bufs

        def rows4(c0, c1, nrows=D):
            q = nrows // 4
            return [((slice(i * q, (i + 1) * q), slice(c0, c1)),) * 2 for i in range(4)]

        B_wkv = cast_group("wkv", w_in_bf, w_in_d, rows4(1024, 3072))
        B_wq = cast_group("wq", w_in_bf, w_in_d, rows4(0, 1024))
        B_wu = cast_group("wu", w_in_bf, w_in_d, rows4(3072, 5120))
        B_wout = cast_group("wout", w_out_bf, w_out_d, rows4(0, D))
        B_wff1 = []
        B_wff2 = []
        for g in range(4):
            B_wff1.append(cast_group("wff1_%d" % g, w_ff1_bf, w_ff1_d, rows4(g * 2048, (g + 1) * 2048)))
            pcs = [((slice(g * 2048 + i * 512, g * 2048 + (i + 1) * 512), slice(0, D)),) * 2 for i in range(4)]
            B_wff2.append(cast_group("wff2_%d" % g, w_ff2_bf, w_ff2_d, pcs))

        xsA = [ar.alloc("xsA%d" % i, [D], F32) for i in range(4)]
        abA = [ar.alloc("abA%d" % i, [D], BF16) for i in range(4)]
        aTA = [ar.alloc("aTA%d" % i, [KC, 512], BF16) for i in range(2)]
        ktst = [ar.alloc("ktst%d" % i, [H, 512], BF16) for i in range(2)]
        vst = [ar.alloc("vst%d" % i, [1024], BF16) for i in range(2)]
        kmsum, B_kmsum = ar.alloc("kmsum", [H, NB], F32)
        NG = S // 512
        KT_v = KT_d.rearrange("h d t -> d h t")
        TB = (0, 1)
        KB = (2, 3)
        VB = (4, 5)
        tile_ctr = [0]

        def A_norm(g):
            aT, B_aT = aTA[g % 2]
            for t in range(4):
                i = tile_ctr[0] % 4
                tile_ctr[0] += 1
                r0 = g * 512 + t * P
                norm_T_tile(xall_d[r0:r0 + P, :], xsA[i][0], xsA[i][1], "xsA%d" % i, gpre_b, B_gpre,
                            abA[i][0], abA[i][1], aT, B_aT, t * P, TB)

        def A_kv(g):
            aT, B_aT = aTA[g % 2]
            kst, B_kst = ktst[g % 2]
            for h in range(H):
                bank = KB[h % 2]
                pk = ps_f32(bank)
                mm_group(pk, [(wkv[:, k, h * DH:(h + 1) * DH], aT[:, k, :]) for k in range(KC)],
                         reads=[B_aT] + B_wkvS, writes=[pbuf[bank]])
                pr.op("act", lambda e, pk=pk, h=h: e.copy(out=kst[:, h, :], in_=pk), reads=[pbuf[bank]], writes=[B_kst])
                pr.op("dve", lambda e, h=h: e.tensor_reduce(
                    out=kmsum[:, h, 2 * g:2 * g + 2], in_=kst[:, h, :].rearrange("p (a b) -> p a b", a=2, b=L),
                    axis=AX.X, op=ALU.add), reads=[B_kst], writes=[B_kmsum])
            pr.dma("pool", KT_v[:, :, g * 512:(g + 1) * 512], kst, "ktst%d" % (g % 2), reads=[B_kst], writes=[B_KT])
            for t in range(4):
                vi = (g * 4 + t) % 2
                vs, B_vs = vst[vi]
                for half in range(2):
                    bank = VB[half]
                    pv = ps_f32(bank)
                    mm_group(pv, [(aT[:, k, t * P:(t + 1) * P], wkv[:, k, 1024 + half * 512:1024 + (half + 1) * 512])
                                  for k in range(KC)], reads=[B_aT] + B_wkvS, writes=[pbuf[bank]])
                    hs_ = slice(half * 512, (half + 1) * 512)
                    if t % 2 == 0:
                        pr.op("dve", lambda e, pv=pv, vs=vs, hs_=hs_: e.tensor_tensor(
                            out=vs[:, hs_], in0=pv, in1=cvec[:, hs_], op=ALU.mult),
                            reads=[pbuf[bank], B_cvec], writes=[B_vs])
                    else:
                        pr.op("act", lambda e, pv=pv, vs=vs, hs_=hs_: e.copy(out=vs[:, hs_], in_=pv),
                              reads=[pbuf[bank]], writes=[B_vs])
                r0 = g * 512 + t * P
                pr.dma("pool", V_d[r0:r0 + P, :], vs, "vst%d" % vi, reads=[B_vs], writes=[B_V])

        B_KT = Buf("KT_d")
        B_V = Buf("V_d")
        A_norm(0)
        for g in range(NG):
            if g + 1 < NG:
                A_norm(g + 1)
            A_kv(g)
            if 2 <= g < 14:
                bg_issue(1, [ktst[g % 2][1]])
        pr.op("dve", lambda e: e.tensor_scalar(out=kmean, in0=kmsum, scalar1=1.0 / L, scalar2=None, op0=ALU.mult),
              reads=[B_kmsum], writes=[B_kmean])
        if debug:
            o = pr.dma("sp", kmean_dbg, kmean.rearrange("p h n -> p (h n)"), "dbg0", reads=[B_kmean])
            pr.must_finish(o)
        ar.release(mA)

        last_ops = []
        if stop_after == "A":
            return _finish(nc, pr, st, ar, [B_KT, B_V])

        mB = ar.mark()
        gpre_b, B_gpre = ar.alloc("gpre_b2", [D], F32)
        pr.dma("sp", gpre_b, grow_d[0:1, :].partition_broadcast(P), "c7", writes=[B_gpre])
        aTo, B_aTo = ar.alloc("aTo", [KC, NOWNH], BF16)
        Fall, B_Fall = ar.alloc("Fall", [16, H, NB], F32)
        mB1 = ar.mark()
        xsB = [ar.alloc("xsB%d" % i, [D], F32) for i in range(2)]
        abB = [ar.alloc("abB%d" % i, [D], BF16) for i in range(2)]
        for t in range(NOWNH // P):
            i = t % 2
            norm_T_tile(xown_d[t * P:(t + 1) * P, :], xsB[i][0], xsB[i][1], "xsA%d" % i, gpre_b, B_gpre,
                        abB[i][0], abB[i][1], aTo, B_aTo, t * P, TB)
        ar.release(mB1)
        NWS = 3
        wch = [ar.alloc("wch%d" % i, [KC, 512], BF16) for i in range(NWS)]
        wch_ctr = [0]

        def load_wchunk(src_ap, reads):
            i = wch_ctr[0] % NWS
            wch_ctr[0] += 1
            w, B_w = wch[i]
            pr.dma("sp", w, src_ap, "wch%d" % i, reads=reads, writes=[B_w])
            return w, B_w

        def in_chunk_src(c0):
            return w_in_bf[:, c0:c0 + 512].rearrange("(k p) n -> p k n", p=P)

        qst = [ar.alloc("qst%d" % i, [L], BF16) for i in range(2)]
        q32 = [ar.alloc("q32_%d" % i, [L], F32) for i in range(2)]
        kost = [ar.alloc("kost%d" % i, [L], BF16) for i in range(2)]
        vost = [ar.alloc("vost%d" % i, [512], BF16) for i in range(2)]
        fdt = [ar.alloc("fdt%d" % i, [2, H, NB], F32) for i in range(2)]
        gsb = [ar.alloc("gsb%d" % i, [NB], F32) for i in range(2)]
        top8 = [ar.alloc("top8_%d" % i, [8], F32) for i in range(2)]
        sig = [ar.alloc("sig%d" % i, [SLOTW], F32) for i in range(2)]
        hst = [ar.alloc("hst%d" % i, [SLOTW], BF16) for i in range(2)]
        dgb = [ar.alloc("dgb%d" % i, [CW, P], BF16) for i in range(2)]
        CVB = (6, 7)
        accD = [ar.alloc("accD%d" % i, [L], F32) for i in range(2)]
        B_QT = Buf("QT_d")
        B_KTo = Buf("KTo_d")
        B_Vo = Buf("Vo_d")
        B_yT = Buf("yT_d")
        B_F = Buf("F_d")
        PB = (2, 3, 4, 5)
        GB = 6
        pb_ctr = [0]

        def nbank():
            b = PB[pb_ctr[0] % len(PB)]
            pb_ctr[0] += 1
            return b
        ctr = {"q": 0, "k": 0, "v": 0, "u": 0, "g": 0, "c": 0}

        chunk_list = [("q", 0), ("q", 512), ("k", 1024), ("k", 1536), ("v", 2048), ("v", 2560),
                      ("uv", 3072), ("ug", 4096), ("uv", 3584), ("ug", 4608)]
        pending = None
        nxt = load_wchunk(in_chunk_src(chunk_list[0][1]), in_col_bufs(chunk_list[0][1]))
        for ci, (kind, c0) in enumerate(chunk_list):
            w, B_w = nxt
            if ci + 1 < len(chunk_list):
                nxt = load_wchunk(in_chunk_src(chunk_list[ci + 1][1]), issued(in_col_bufs(chunk_list[ci + 1][1])))
            if ci >= 1:
                gate = {"q": B_QT, "k": B_KTo, "v": B_Vo, "uv": B_yT, "ug": B_yT}[chunk_list[ci - 1][0]]
                bg_issue(1, [gate])
            if kind in ("q", "k"):
                for s in range(NSLOT):
                    if kind == "q":
                        fi = ctr["g"] % 2
                        ctr["g"] += 1
                        fdv, B_fd = fdt[fi]
                        pr.dma("sp", fdv, fd_d[s].rearrange("p (t h n) -> p t h n", t=2, h=H, n=NB), "fdt%d" % fi,
                               writes=[B_fd])
                    for sub in range(4):
                        h = (c0 % 1024) // DH + sub
                        bank = nbank()
                        pq = ps_f32(bank, SLOTW)
                        mm_group(pq, [(w[:, k, sub * DH:(sub + 1) * DH], aTo[:, k, s * SLOTW:(s + 1) * SLOTW])
                                      for k in range(KC)], reads=[B_aTo, B_w], writes=[pbuf[bank]])
                        if kind == "k":
                            i = ctr["k"] % 2
                            ctr["k"] += 1
                            ks, B_ks = kost[i]
                            pr.op("act", lambda e, ks=ks, pq=pq: e.copy(out=ks, in_=pq[:, HALO:SLOTW]),
                                  reads=[pbuf[bank]], writes=[B_ks])
                            pr.dma("pool", KTo_d[h, :, s * L:(s + 1) * L], ks, "kost%d" % i, reads=[B_ks], writes=[B_KTo])
                            continue
                        i = ctr["q"] % 2
                        ctr["q"] += 1
                        qs, B_qs = qst[i]
                        qf, B_qf = q32[i]
                        pr.op("act", lambda e, qf=qf, pq=pq: e.copy(out=qf, in_=pq[:, HALO:SLOTW]),
                              reads=[pbuf[bank]], writes=[B_qf])
                        pr.op("dve", lambda e, qs=qs, qf=qf: e.tensor_copy(out=qs, in_=qf), reads=[B_qf], writes=[B_qs])
                        pr.dma("pool", QT_d[h, :, s * L:(s + 1) * L], qs, "qst%d" % i, reads=[B_qs], writes=[B_QT])
                        pg = ps_f32(GB, 2 * NB).rearrange("p (t n) -> p t n", t=2, n=NB)

                        def gmm(e, qf=qf, h=h, pg=pg):
                            e.matmul(pg[:, 0, :], lhsT=qf[:, 0:P], rhs=kmean[:, h, :], start=True, stop=True)
                            return e.matmul(pg[:, 1, :], lhsT=qf[:, P:2 * P], rhs=kmean[:, h, :], start=True, stop=True)
                        pr.op("pe", gmm, reads=[B_qf, B_kmean], writes=[pbuf[GB]])
                        for qt in range(2):
                            gi = ctr["u"] % 2
                            ctr["u"] += 1
                            gs, B_gs = gsb[gi]
                            t8, B_t8 = top8[gi]
                            pr.op("dve", lambda e, gs=gs, qt=qt, pg=pg, s=s: e.tensor_tensor(
                                out=gs, in0=pg[:, qt, :], in1=gbias[:, s, :], op=ALU.add),
                                reads=[pbuf[GB], B_gbias], writes=[B_gs])
                            pr.op("dve", lambda e, gs=gs, t8=t8: e.max(out=t8, in_=gs), reads=[B_gs], writes=[B_t8])
                            pr.op("dve", lambda e, gs=gs, t8=t8, qt=qt, h=h, s=s, fdv=fdv: e.scalar_tensor_tensor(
                                out=Fall[:, 2 * s + qt, h, :], in0=gs, scalar=t8[:, 2:3], in1=fdv[:, qt, h, :],
                                op0=ALU.is_ge, op1=ALU.mult), reads=[B_gs, B_t8, B_fd], writes=[B_Fall])
            elif kind == "v":
                for s in range(NSLOT):
                    for t in range(2):
                        bank = nbank()
                        pv = ps_f32(bank)
                        col = s * SLOTW + HALO + t * P
                        mm_group(pv, [(aTo[:, k, col:col + P], w[:, k, :]) for k in range(KC)],
                                 reads=[B_aTo, B_w], writes=[pbuf[bank]])
                        i = ctr["v"] % 2
                        ctr["v"] += 1
                        vs, B_vs = vost[i]
                        pr.op("act", lambda e, vs=vs, pv=pv: e.copy(out=vs, in_=pv), reads=[pbuf[bank]], writes=[B_vs])
                        r0 = s * L + t * P
                        cv = c0 - 2048
                        pr.dma("pool", Vo_d[r0:r0 + P, cv:cv + 512], vs, "vost%d" % i, reads=[B_vs], writes=[B_Vo])
            elif kind == "uv":
                pending = (w, B_w, c0)
            else:
                wv, B_wv, cv0 = pending
                for sub in range(4):
                    cch = (cv0 - 3072) // P + sub
                    dgv, B_dg = dgb[cch % 2]
                    for jt in range(CW):
                        pr.op("dve", lambda e, dgv=dgv, cch=cch, jt=jt: e.tensor_scalar(
                            out=dgv[:, jt, :], in0=ident, scalar1=wdw[:, cch, jt:jt + 1], scalar2=None, op0=ALU.mult),
                            reads=[B_ident, B_cols], writes=[B_dg])
                    for s in range(NSLOT):
                        bv = nbank()
                        bg = nbank()
                        pval = ps_f32(bv, SLOTW)
                        pgt = ps_f32(bg, SLOTW)
                        rhs_sl = slice(s * SLOTW, (s + 1) * SLOTW)
                        mm_group(pval, [(wv[:, k, sub * P:(sub + 1) * P], aTo[:, k, rhs_sl]) for k in range(KC)],
                                 reads=[B_aTo, B_wv], writes=[pbuf[bv]])
                        mm_group(pgt, [(w[:, k, sub * P:(sub + 1) * P], aTo[:, k, rhs_sl]) for k in range(KC)],
                                 reads=[B_aTo, B_w], writes=[pbuf[bg]])
                        i = ctr["c"] % 2
                        ctr["c"] += 1
                        sg, B_sg = sig[i]
                        hs, B_hs = hst[i]
                        aD, B_aD = accD[i]
                        pr.op("act", lambda e, sg=sg, pgt=pgt, cch=cch: e.activation(
                            out=sg, in_=pgt, func=AF.Sigmoid, bias=bglu[:, 8 + cch:9 + cch], scale=1.0),
                            reads=[pbuf[bg], B_cols], writes=[B_sg])
                        pr.op("dve", lambda e, hs=hs, pval=pval, sg=sg, cch=cch: e.scalar_tensor_tensor(
                            out=hs, in0=pval, scalar=bglu[:, cch:cch + 1], in1=sg, op0=ALU.add, op1=ALU.mult),
                            reads=[pbuf[bv], B_sg, B_cols], writes=[B_hs])
                        pr.op("dve", lambda e, hs=hs, s=s: e.tensor_scalar(
                            out=hs[:, 0:HALO], in0=hs[:, 0:HALO], scalar1=hmask[:, s:s + 1], scalar2=None, op0=ALU.mult),
                            reads=[B_hs, B_hmask], writes=[B_hs])
                        cb = CVB[ctr["c"] % 2]
                        pc = ps_f32(cb, L)
                        mm_group(pc, [(dgv[:, jt, :], hs[:, 2 + jt:2 + jt + L]) for jt in range(CW)],
                                 reads=[B_dg, B_hs], writes=[pbuf[cb]])
                        pr.op("dve", lambda e, aD=aD, pc=pc, cch=cch: e.tensor_scalar(
                            out=aD, in0=pc, scalar1=bdw[:, cch:cch + 1], scalar2=None, op0=ALU.add),
                            reads=[pbuf[cb], B_cols], writes=[B_aD])
                        pr.dma("pool", yT_d[cch, :, s * L:(s + 1) * L], aD, "accD%d" % i, reads=[B_aD], writes=[B_yT])
        for h in range(H):
            pr.dma("pool", F_d[h].rearrange("p (q n) -> p q n", q=16, n=NB), Fall[:, :, h, :], "fst", reads=[B_Fall],
                   writes=[B_F])
        ar.release(mB)
        if stop_after == "B":
            return _finish(nc, pr, st, ar, [B_KT, B_V, B_QT, B_KTo, B_Vo, B_yT, B_F])

        B_mixT = Buf("mixT_d")
        mL = ar.mark()
        ysl = [ar.alloc("ysl%d" % i, [CC, L], F32) for i in range(2)]
        sqs = [ar.alloc("sqs%d" % i, [CC, L], F32) for i in range(2)]
        cst = [ar.alloc("cst%d" % i, [CC, L], BF16) for i in range(2)]
        mean_t, B_mean = ar.alloc("mean_t", [L], F32)
        msq_t, B_msq = ar.alloc("msq_t", [L], F32)
        rstd_t, B_rstdL = ar.alloc("rstd_t", [L], F32)
        mr_t, B_mr = ar.alloc("mr_t", [L], F32)
        tmpL = [ar.alloc("tmpL%d" % i, [L], F32) for i in range(2)]
        yT_v = yT_d.rearrange("c p t -> p c t")
        for s in range(NSLOT):
            i = s % 2
            ys, B_ys = ysl[i]
            sq, B_sq = sqs[i]
            cs, B_cs = cst[i]
            pr.dma("sp", ys, yT_v[:, :, s * L:(s + 1) * L], "ysl%d" % i, reads=[B_yT], writes=[B_ys])
            pr.op("act", lambda e, ys=ys, sq=sq: e.activation(out=sq, in_=ys, func=AF.Square), reads=[B_ys], writes=[B_sq])
            pm = ps_f32(2, L)
            pq2 = ps_f32(3, L)
            mm_group(pm, [(ones32, ys[:, c, :]) for c in range(CC)], reads=[B_ones, B_ys], writes=[pbuf[2]])
            mm_group(pq2, [(ones32, sq[:, c, :]) for c in range(CC)], reads=[B_ones, B_sq], writes=[pbuf[3]])
            pr.op("dve", lambda e, pm=pm: e.tensor_scalar(out=mean_t, in0=pm, scalar1=1.0 / C, scalar2=None, op0=ALU.mult),
                  reads=[pbuf[2]], writes=[B_mean])
            pr.op("dve", lambda e: e.tensor_tensor(out=msq_t, in0=mean_t, in1=mean_t, op=ALU.mult),
                  reads=[B_mean], writes=[B_msq])
            pr.op("dve", lambda e, pq2=pq2: e.scalar_tensor_tensor(out=rstd_t, in0=pq2, scalar=1.0 / C, in1=msq_t,
                                                                  op0=ALU.mult, op1=ALU.subtract),
                  reads=[pbuf[3], B_msq], writes=[B_rstdL])
            pr.op("act", lambda e: e.activation(out=rstd_t, in_=rstd_t, func=AF.Sqrt, bias=LN_EPS, scale=1.0),
                  reads=[B_rstdL], writes=[B_rstdL])
            pr.op("dve", lambda e: e.reciprocal(out=rstd_t, in_=rstd_t), reads=[B_rstdL], writes=[B_rstdL])
            pr.op("dve", lambda e: e.tensor_tensor(out=mr_t, in0=mean_t, in1=rstd_t, op=ALU.mult),
                  reads=[B_mean, B_rstdL], writes=[B_mr])
            for c in range(CC):
                tl, B_tl = tmpL[c % 2]
                pr.op("dve", lambda e, tl=tl, ys=ys, c=c: e.tensor_tensor(out=tl, in0=ys[:, c, :], in1=rstd_t, op=ALU.mult),
                      reads=[B_ys, B_rstdL], writes=[B_tl])
                pr.op("dve", lambda e, tl=tl: e.tensor_tensor(out=tl, in0=tl, in1=mr_t, op=ALU.subtract),
                      reads=[B_tl, B_mr], writes=[B_tl])
                pr.op("act", lambda e, tl=tl, cs=cs, c=c: e.activation(out=cs[:, c, :], in_=tl, func=AF.Silu,
                                                                     bias=lnb[:, c:c + 1], scale=lng[:, c:c + 1]),
                      reads=[B_tl, B_cols], writes=[B_cs])
            pr.dma("pool", mixT_d[C:2 * C, s * L:(s + 1) * L].rearrange("(c p) t -> p c t", p=P), cs, "cst%d" % i,
                   reads=[B_cs], writes=[B_mixT])
        ar.release(mL)
        if stop_after == "L":
            return _finish(nc, pr, st, ar, [B_KT, B_V, B_QT, B_KTo, B_Vo, B_yT, B_F, B_mixT])

        mT = ar.mark()
        NT = S // P
        VW = DH + 2
        KTh = [ar.alloc("KTh%d" % i, [S], BF16) for i in range(2)]
        Vh = [ar.alloc("Vh%d" % i, [NT, VW], BF16) for i in range(2)]
        KToh = [ar.alloc("KToh%d" % i, [NOWN], BF16) for i in range(2)]
        Voh = [ar.alloc("Voh%d" % i, [NOWN // P, VW], BF16) for i in range(2)]
        QTh = [ar.alloc("QTh%d" % i, [NOWN], BF16) for i in range(2)]
        Fh = [ar.alloc("Fh%d" % i, [16, NB], F32) for i in range(2)]
        pTs = [ar.alloc("pT%d" % i, [2, L], BF16) for i in range(3)]
        pTo = [ar.alloc("pTo%d" % i, [3 * P], BF16) for i in range(2)]
        accs = [ar.alloc("acc%d" % i, [2, VW], F32) for i in range(2)]
        tmps = [ar.alloc("tmpT%d" % i, [2, VW], F32) for i in range(3)]
        rcs = [ar.alloc("rc%d" % i, [2], F32) for i in range(2)]
        obf = [ar.alloc("obf%d" % i, [2, DH], BF16) for i in range(2)]
        ast = [ar.alloc("ast%d" % i, [L], BF16) for i in range(2)]
        for i in range(2):
            pr.op("pool", lambda e, i=i: e.memset(Vh[i][0][:, :, DH:VW], 1.0), writes=[Vh[i][1]])
            pr.op("pool", lambda e, i=i: e.memset(Voh[i][0][:, :, DH:VW], 1.0), writes=[Voh[i][1]])
        SBK = (0, 1, 2)
        OBK = (3, 4, 5)
        TBK = 6

        def T_load(h):
            i = h % 2
            pr.dma("sp", KTh[i][0], KT_d[h], "KTh%d" % i, reads=[B_KT], writes=[KTh[i][1]])
            ch = float(np.exp(-128.0 * alibi_slopes()[h]))
            vev = Vh[i][0].rearrange("p (a two) w -> p a two w", two=2)[:, :, 0, DH:VW]
            pr.op("pool", lambda e, vev=vev, ch=ch: e.memset(vev, ch), writes=[Vh[i][1]])
            vsrc = V_d[:, h * DH:(h + 1) * DH].rearrange("(t p) d -> p t d", p=P)
            for q4 in range(4):
                pr.dma("sp", Vh[i][0][:, q4 * 16:(q4 + 1) * 16, 0:DH], vsrc[:, q4 * 16:(q4 + 1) * 16, :], "Vh%d_%d" % (i, q4),
                       reads=[B_V], writes=[Vh[i][1]])
            pr.dma("sp", KToh[i][0], KTo_d[h], "KToh%d" % i, reads=[B_KTo], writes=[KToh[i][1]])
            pr.dma("sp", Voh[i][0][:, :, 0:DH], Vo_d[:, h * DH:(h + 1) * DH].rearrange("(t p) d -> p t d", p=P),
                   "Voh%d" % i, reads=[B_Vo], writes=[Voh[i][1]])
            pr.dma("sp", QTh[i][0], QT_d[h], "QTh%d" % i, reads=[B_QT], writes=[QTh[i][1]])
            pr.dma("sp", Fh[i][0], F_d[h].rearrange("p (q n) -> p q n", q=16, n=NB), "Fh%d" % i, reads=[B_F],
                   writes=[Fh[i][1]])

        uctr = [0]

        def T_head(h):
            i = h % 2
            kth, B_kth = KTh[i]
            vh, B_vh = Vh[i]
            kto, B_kto = KToh[i]
            voh, B_voh = Voh[i]
            qth, B_qth = QTh[i]
            fh, B_fh = Fh[i]
            units = []
            for s in range(NSLOT):
                units.append((s, -1))
                for n in range(PAST[s]):
                    units.append((s, n))
            state = {}
            deferred = []

            def emit_qk(idx):
                s, n = units[idx]
                u = uctr[0]
                uctr[0] += 1
                sb = SBK[u % 3]
                qsl = slice(s * L, (s + 1) * L)
                if n >= 0:
                    sv = ps_f32(sb).rearrange("p (t q) -> p t q", t=2, q=L)

                    def qk(e, sv=sv, n=n, qsl=qsl):
                        e.matmul(sv[:, 0, :], lhsT=kth[:, (2 * n) * P:(2 * n + 1) * P], rhs=qth[:, qsl], start=True, stop=True)
                        return e.matmul(sv[:, 1, :], lhsT=kth[:, (2 * n + 1) * P:(2 * n + 2) * P], rhs=qth[:, qsl],
                                        start=True, stop=True)
                    pr.op("pe", qk, reads=[B_kth, B_qth], writes=[pbuf[sb]])
                    pt, B_pt = pTs[u % 3]
                    pr.op("act", lambda e, pt=pt, sv=sv: e.activation(
                        out=pt, in_=sv, func=AF.Exp, bias=bkt[:, h, 1:2], scale=SCALE),
                        reads=[pbuf[sb], B_bkt], writes=[B_pt])
                    state[idx] = (u, pt, B_pt)
                else:
                    sv = ps_f32(sb, 3 * P)
                    q0 = s * L

                    def qk(e, sv=sv, q0=q0):
                        e.matmul(sv[:, 0:P], lhsT=kto[:, q0:q0 + P], rhs=qth[:, q0:q0 + P], start=True, stop=True)
                        e.matmul(sv[:, P:2 * P], lhsT=kto[:, q0 + P:q0 + 2 * P], rhs=qth[:, q0 + P:q0 + 2 * P],
                                 start=True, stop=True)
                        return e.matmul(sv[:, 2 * P:3 * P], lhsT=kto[:, q0:q0 + P], rhs=qth[:, q0 + P:q0 + 2 * P],
                                        start=True, stop=True)
                    pr.op("pe", qk, reads=[B_kto, B_qth], writes=[pbuf[sb]])
                    pt, B_pt = pTo[s % 2]
                    pr.op("act", lambda e, pt=pt, sv=sv: e.activation(
                        out=pt[:, 0:2 * P], in_=sv[:, 0:2 * P], func=AF.Exp, bias=bkt[:, h, 1:2], scale=SCALE),
                        reads=[pbuf[sb], B_bkt], writes=[B_pt])
                    pr.op("act", lambda e, pt=pt, sv=sv: e.activation(
                        out=pt[:, 2 * P:3 * P], in_=sv[:, 2 * P:3 * P], func=AF.Exp, bias=bkt[:, h, 0:1], scale=SCALE),
                        reads=[pbuf[sb], B_bkt], writes=[B_pt])
                    for a in range(2):
                        pr.op("pool", lambda e, pt=pt, a=a: e.tensor_tensor(
                            out=pt[:, a * P:(a + 1) * P], in0=pt[:, a * P:(a + 1) * P], in1=tri, op=ALU.mult),
                            reads=[B_pt, B_tri], writes=[B_pt])
                    state[idx] = (u, pt, B_pt)

            def emit_pv(idx):
                s, n = units[idx]
                u, pt, B_pt = state.pop(idx)
                ob = OBK[u % 3]
                ov = ps_f32(ob, 2 * VW).rearrange("p (t d) -> p t d", t=2, d=VW)
                acc, B_acc = accs[s % 2]
                NV = DH + 1
                if n >= 0:
                    def pv(e, ov=ov, pt=pt, n=n):
                        ins = None
                        for qt in range(2):
                            for t in range(2):
                                ins = e.matmul(ov[:, qt, 0:NV], lhsT=pt[:, t, qt * P:(qt + 1) * P],
                                               rhs=vh[:, 2 * n + t, 0:NV], start=(t == 0), stop=(t == 1))
                        return ins
                    pr.op("pe", pv, reads=[B_pt, B_vh], writes=[pbuf[ob]])
                    tm, B_tm = tmps[u % 3]
                    pr.op("dve", lambda e, ov=ov, tm=tm, n=n, s=s: e.tensor_tensor(
                        out=tm[:, :, 0:NV], in0=ov[:, :, 0:NV],
                        in1=fh[:, 2 * s:2 * s + 2, n:n + 1].to_broadcast([P, 2, NV]), op=ALU.mult),
                        reads=[pbuf[ob], B_fh], writes=[B_tm])
                    pr.op("pool", lambda e, tm=tm, acc=acc: e.tensor_tensor(
                        out=acc[:, :, 0:NV], in0=acc[:, :, 0:NV], in1=tm[:, :, 0:NV], op=ALU.add),
                        reads=[B_tm, B_acc], writes=[B_acc])
                else:
                    def pv(e, ov=ov, pt=pt, s=s):
                        e.matmul(ov[:, 0, 0:NV], lhsT=pt[:, 0:P], rhs=voh[:, 2 * s, 0:NV], start=True, stop=True)
                        e.matmul(ov[:, 1, 0:NV], lhsT=pt[:, 2 * P:3 * P], rhs=voh[:, 2 * s, 0:NV], start=True, stop=False)
                        return e.matmul(ov[:, 1, 0:NV], lhsT=pt[:, P:2 * P], rhs=voh[:, 2 * s + 1, 0:NV],
                                        start=False, stop=True)
                    pr.op("pe", pv, reads=[B_pt, B_voh], writes=[pbuf[ob]])
                    pr.op("dve", lambda e, ov=ov, acc=acc: e.tensor_scalar(
                        out=acc[:, :, 0:NV], in0=ov[:, :, 0:NV], scalar1=fown[:, h:h + 1], scalar2=None,
                        op0=ALU.mult), reads=[pbuf[ob], B_fown], writes=[B_acc])
                last = (idx + 1 == len(units)) or (units[idx + 1][0] != s)
                if last:
                    deferred.append([2, s])

            def finalize(s):
                acc, B_acc = accs[s % 2]
                rc, B_rc = rcs[s % 2]
                ob_, B_ob = obf[s % 2]
                asv, B_as = ast[s % 2]
                pr.op("dve", lambda e: e.reciprocal(out=rc, in_=acc[:, :, DH]), reads=[B_acc], writes=[B_rc])
                for qt in range(2):
                    pr.op("dve", lambda e, qt=qt: e.tensor_scalar(out=ob_[:, qt, :], in0=acc[:, qt, 0:DH],
                                                                  scalar1=rc[:, qt:qt + 1], scalar2=None, op0=ALU.mult),
                          reads=[B_acc, B_rc], writes=[B_ob])
                ptv = ps_bf16(TBK)[:, 0:L].rearrange("p (t q) -> p t q", t=2, q=P)

                def tr(e):
                    e.transpose(out=ptv[:, 0, :], in_=ob_[:, 0, :], identity=ident)
                    return e.transpose(out=ptv[:, 1, :], in_=ob_[:, 1, :], identity=ident)
                pr.op("pe", tr, reads=[B_ob, B_ident], writes=[pbuf[TBK]])
                pr.op("act", lambda e: e.copy(out=asv, in_=ps_bf16(TBK)[:, 0:L]), reads=[pbuf[TBK]], writes=[B_as])
                pr.dma("pool", mixT_d[h * DH:(h + 1) * DH, s * L:(s + 1) * L], asv, "ast%d" % (s % 2), reads=[B_as],
                       writes=[B_mixT])

            def tick():
                for dd in list(deferred):
                    dd[0] -= 1
                    if dd[0] <= 0:
                        deferred.remove(dd)
                        finalize(dd[1])

            emit_qk(0)
            emit_qk(1)
            for idx in range(len(units)):
                if idx + 2 < len(units):
                    emit_qk(idx + 2)
                emit_pv(idx)
                tick()
            for dd in list(deferred):
                finalize(dd[1])
            deferred.clear()

        T_load(0)
        for h in range(H):
            if h + 1 < H:
                T_load(h + 1)
            bg_issue(4, [B_mixT])
            T_head(h)
        bg_issue(100, [B_mixT])
        ar.release(mT)
        if stop_after == "T":
            return _finish(nc, pr, st, ar, [B_mixT])

        gb_t = []
        for gi_, nm in ((1, "gpost"), (2, "gffn"), (3, "gfpost")):
            t_, b_ = ar.alloc(nm, [D], F32)
            pr.dma("sp", t_, grow_d[gi_:gi_ + 1, :].partition_broadcast(P), "c7_%d" % gi_, writes=[b_])
            gb_t.append((t_, b_))
        (gpost_b, B_gpost), (gffn_b, B_gffn), (gfpost_b, B_gfpost) = gb_t
        mixS, B_mixS = ar.alloc("mixS", [KC, 512], BF16)
        fT, B_fT = ar.alloc("fT", [KC, 512], BF16)
        accF, _ = ar.alloc("accF", [4, D], F32)
        B_accF = [Buf("accF%d" % t) for t in range(4)]
        hTs = [ar.alloc("hT%d" % i, [4, 512], BF16) for i in range(2)]
        rts = [ar.alloc("rt%d" % i, [512], F32) for i in range(2)]
        NWS2 = 4
        wc2 = [ar.alloc("wc2_%d" % i, [KC, 512], BF16) for i in range(NWS2)]
        xsC = [ar.alloc("xsC%d" % i, [D], F32) for i in range(2)]
        abC = [ar.alloc("abC%d" % i, [D], BF16) for i in range(2)]
        w2ctr = [0]

        def load_w2(src_ap, reads):
            i = w2ctr[0] % NWS2
            w2ctr[0] += 1
            w, B_w = wc2[i]
            pr.dma("sp", w, src_ap, "wch%d" % i, reads=reads, writes=[B_w])
            return w, B_w
        B_y = [Buf("y%d" % t) for t in range(NOWN // P)]
        CB = (2, 3)
        HBK = (4, 5)
        cb_ctr = [0]
        xc_ctr = [0]

        def rstd_of(src, B_src, junk, B_junk):
            i = stat_ctr[0] % 8
            stat_ctr[0] += 1
            ssv = ss_t[:, i:i + 1]
            rsv = rs_t[:, i:i + 1]
            pr.op("act", lambda e: e.activation(out=junk, in_=src, func=AF.Square, accum_out=ssv),
                  reads=[B_src], writes=[B_ss[i], B_junk])
            pr.op("act", lambda e: e.activation(out=rsv, in_=ssv, func=AF.Sqrt, bias=RMS_EPS, scale=1.0 / D),
                  reads=[B_ss[i]], writes=[B_rs[i]])
            pr.op("dve", lambda e: e.reciprocal(out=rsv, in_=rsv), reads=[B_rs[i]], writes=[B_rs[i]])
            return rsv, B_rs[i]

        for gi in range(4):
            pr.dma("sp", mixS, mixT_d[:, gi * 512:(gi + 1) * 512].rearrange("(k p) t -> p k t", p=P), "mixS",
                   reads=[B_mixT], writes=[B_mixS])
            nxt = load_w2(w_out_bf[:, 0:512].rearrange("(k p) n -> p k n", p=P), issued(B_wout))
            for n in range(4):
                w, B_w = nxt
                if n + 1 < 4:
                    nxt = load_w2(w_out_bf[:, (n + 1) * 512:(n + 2) * 512].rearrange("(k p) n -> p k n", p=P), B_wout)
                else:
                    nxtW1 = [load_w2(w_ff1_bf[:, 0:512].rearrange("(k p) n -> p k n", p=P), B_wff1[0])]
                for t in range(4):
                    bank = CB[cb_ctr[0] % 2]
                    cb_ctr[0] += 1
                    po = ps_f32(bank)
                    mm_group(po, [(mixS[:, k, t * P:(t + 1) * P], w[:, k, :]) for k in range(KC)],
                             reads=[B_mixS, B_w], writes=[pbuf[bank]])
                    pr.op("act", lambda e, po=po, t=t, n=n: e.copy(out=accF[:, t, n * 512:(n + 1) * 512], in_=po),
                          reads=[pbuf[bank]], writes=[B_accF[t]])
            for t in range(4):
                i = xc_ctr[0] % 2
                xc_ctr[0] += 1
                xs, B_xs = xsC[i]
                ab, B_ab = abC[i]
                s_ = 2 * gi + t // 2
                r0 = s_ * SLOTW + HALO + (t % 2) * P
                pr.dma("sp", xs, xown_d[r0:r0 + P, :], "xsA%d" % i, writes=[B_xs])
                rsv, B_r = rstd_of(accF[:, t, :], B_accF[t], ab, B_ab)
                pr.op("dve", lambda e, t=t, rsv=rsv: e.scalar_tensor_tensor(
                    out=accF[:, t, :], in0=accF[:, t, :], scalar=rsv, in1=gpost_b, op0=ALU.mult, op1=ALU.mult),
                    reads=[B_accF[t], B_r, B_gpost], writes=[B_accF[t]])
                pr.op("dve", lambda e, t=t, xs=xs: e.tensor_tensor(out=xs, in0=accF[:, t, :], in1=xs, op=ALU.add),
                      reads=[B_accF[t], B_xs], writes=[B_xs])
                yt = gi * 4 + t
                pr.dma("pool", y_d[yt * P:(yt + 1) * P, :], xs, "xsSt%d" % i, reads=[B_xs], writes=[B_y[yt]])
                norm_T_tile(None, xs, B_xs, None, gffn_b, B_gffn, ab, B_ab, fT, B_fT, t * P, TB)
            NFC = DFF // 512

            def w1src(c):
                return w_ff1_bf[:, c * 512:(c + 1) * 512].rearrange("(k p) n -> p k n", p=P), B_wff1[c // 4]

            def w2src(c):
                return w_ff2_bf[c * 512:(c + 1) * 512, :].rearrange("(j p) n -> p j n", p=P), B_wff2[c // 4]
            W1 = {0: nxtW1[0]}
            W2 = {}
            W1[1] = load_w2(*w1src(1))
            W2[0] = load_w2(*w2src(0))

            def ffn_H(c):
                w, B_w = W1.pop(c)
                hT, B_hT = hTs[c % 2]
                for j in range(4):
                    bank = HBK[j % 2]
                    ph = ps_f32(bank)
                    mm_group(ph, [(w[:, k, j * P:(j + 1) * P], fT[:, k, :]) for k in range(KC)],
                             reads=[B_w, B_fT], writes=[pbuf[bank]])
                    rt, B_rt = rts[j % 2]
                    pr.op("act", lambda e, rt=rt, ph=ph: e.activation(out=rt, in_=ph, func=AF.Relu),
                          reads=[pbuf[bank]], writes=[B_rt])
                    pr.op("act", lambda e, rt=rt, hT=hT, j=j: e.activation(out=hT[:, j, :], in_=rt, func=AF.Square),
                          reads=[B_rt], writes=[B_hT])

            def ffn_O(c):
                w, B_w = W2.pop(c)
                wv_ = w.rearrange("p k n -> p (k n)").rearrange("p (j n) -> p j n", j=4, n=D)
                hT, B_hT = hTs[c % 2]
                for t in range(4):
                    for n in range(4):
                        bank = CB[cb_ctr[0] % 2]
                        cb_ctr[0] += 1
                        po = ps_f32(bank)
                        mm_group(po, [(hT[:, j, t * P:(t + 1) * P], wv_[:, j, n * 512:(n + 1) * 512]) for j in range(4)],
                                 reads=[B_hT, B_w], writes=[pbuf[bank]])
                        dst = accF[:, t, n * 512:(n + 1) * 512]
                        if c == 0:
                            pr.op("dve", lambda e, dst=dst, po=po: e.tensor_copy(out=dst, in_=po),
                                  reads=[pbuf[bank]], writes=[B_accF[t]])
                        else:
                            pr.op("dve", lambda e, dst=dst, po=po: e.tensor_tensor(out=dst, in0=po, in1=dst, op=ALU.add),
                                  reads=[pbuf[bank], B_accF[t]], writes=[B_accF[t]])
            ffn_H(0)
            for c in range(NFC):
                if c + 2 < NFC:
                    W1[c + 2] = load_w2(*w1src(c + 2))
                if c + 1 < NFC:
                    W2[c + 1] = load_w2(*w2src(c + 1))
                    ffn_H(c + 1)
                ffn_O(c)
            for t in range(4):
                i = xc_ctr[0] % 2
                xc_ctr[0] += 1
                xs, B_xs = xsC[i]
                ab, B_ab = abC[i]
                yt = gi * 4 + t
                pr.dma("sp", xs, y_d[yt * P:(yt + 1) * P, :], "xsA%d" % i, reads=[B_y[yt]], writes=[B_xs])
                rsv, B_r = rstd_of(accF[:, t, :], B_accF[t], ab, B_ab)
                pr.op("dve", lambda e, t=t, rsv=rsv: e.scalar_tensor_tensor(
                    out=accF[:, t, :], in0=accF[:, t, :], scalar=rsv, in1=gfpost_b, op0=ALU.mult, op1=ALU.mult),
                    reads=[B_accF[t], B_r, B_gfpost], writes=[B_accF[t]])
                pr.op("dve", lambda e, t=t, xs=xs: e.tensor_tensor(out=xs, in0=accF[:, t, :], in1=xs, op=ALU.add),
                      reads=[B_accF[t], B_xs], writes=[B_xs])
                o = pr.dma("pool", y_d[yt * P:(yt + 1) * P, :], xs, "xsSt%d" % i, reads=[B_xs], writes=[B_y[yt]])
                pr.must_finish(o)
        return _finish(nc, pr, st, ar, [])


def _finish(nc, pr, st, ar, bufs):
    for b in bufs:
        if b.writer is not None:
            pr.must_finish(b.writer)
    pr.emit_all(st)
    nc._mk_info = dict(n_sems=pr.n_sems, peak=ar.peak, nops=len(pr.all_ops), counts=pr.max_count)
    return nc


def alibi_slopes():
    return (2.0 ** (-8.0 * np.arange(1, H + 1) / H)).astype(np.float64)


def core_tables(j):
    blks = own_blocks(j)
    sl = alibi_slopes()
    fd = np.zeros((NSLOT, P, 2, H, NB), np.float64)
    gb = np.full((P, NSLOT, NB), -1e30, np.float32)
    hm = np.ones((P, NSLOT), np.float32)
    q = np.arange(P)
    for s, blk in enumerate(blks):
        if blk == 0:
            hm[:, s] = 0.0
        for n in range(blk):
            gb[:, s, n] = 0.0
            for qt in range(2):
                dist = 256 * (n - blk) + 255 - (128 * qt + q)
                fd[s, :, qt, :, n] = np.exp(sl[None, :] * dist[:, None])
    return (fd.reshape(NSLOT, P, 2 * H * NB).astype(np.float32), gb.reshape(P, NSLOT * NB), hm)


def const_tables():
    sl = alibi_slopes()
    p = np.arange(P)
    bkt = np.zeros((P, H, 2), np.float64)
    for t in range(2):
        bkt[:, :, t] = sl[None, :] * (t * 128 + p[:, None] - 255)
    fown = np.exp(sl[None, :] * (127 - p[:, None]))
    tri = (p[None, :] >= p[:, None]).astype(np.float32)
    cvec = np.repeat(np.exp(-128.0 * sl), DH)[None, :].repeat(P, 0)
    return (bkt.reshape(P, 2 * H).astype(np.float32), fown.astype(np.float32),
            tri.astype(ml_dtypes.bfloat16), np.eye(P, dtype=np.float32).astype(ml_dtypes.bfloat16),
            np.ascontiguousarray(cvec.astype(np.float32)))


def make_in_maps(inputs, cores=range(8)):
    x = np.asarray(inputs["x"], np.float32)
    f = lambda k: np.ascontiguousarray(np.asarray(inputs[k], np.float32)[0])
    w_in, w_out, w_ff1, w_ff2 = f("w_in"), f("w_out"), f("w_ff1"), f("w_ff2")
    grow = np.stack([f("g_mix_pre"), f("g_mix_post"), f("g_ffn_pre"), f("g_ffn_post")]).astype(np.float32)
    b_glu, w_dw, b_dw, ln_g, ln_b = f("b_glu"), f("w_dw"), f("b_dw"), f("ln_conv_g"), f("ln_conv_b")
    cols = np.concatenate([
        b_glu.reshape(16, P).T,
        w_dw.reshape(CW, CC, P).transpose(2, 1, 0).reshape(P, CC * CW),
        b_dw.reshape(CC, P).T, ln_g.reshape(CC, P).T, ln_b.reshape(CC, P).T], axis=1).astype(np.float32)
    bkt, fown, tri, ident, cvec = const_tables()
    maps = []
    for c in cores:
        bi, j = c // 4, c % 4
        fd, gb, hm = core_tables(j)
        xo = np.zeros((NSLOT, SLOTW, D), np.float32)
        for s, blk in enumerate(own_blocks(j)):
            lo = L * blk - HALO
            if lo < 0:
                xo[s, HALO:] = x[bi, 0:L]
            else:
                xo[s] = x[bi, lo:lo + SLOTW]
        maps.append(dict(xall=np.ascontiguousarray(x[bi]), xown=xo.reshape(NOWNH, D), w_in=w_in, w_out=w_out,
                         w_ff1=w_ff1, w_ff2=w_ff2, grow=grow, cols=np.ascontiguousarray(cols), fd=fd, gbias=gb,
                         hmask=hm, bkt=bkt, fown=fown, tri=tri, ident=ident, cvec=cvec))
    return maps


_NC = None


def kernel(**inputs):
    global _NC
    if _NC is None:
        _NC = build_nc()
    maps = make_in_maps(inputs)
    res = run_bass_kernel_spmd(_NC, maps, core_ids=list(range(8)))
    x = np.asarray(inputs["x"])
    out = np.zeros(x.shape, np.float32)
    for c in range(8):
        bi, j = c // 4, c % 4
        y = np.asarray(res.results[c]["y"])
        for s, blk in enumerate(own_blocks(j)):
            out[bi, L * blk:L * (blk + 1)] = y[s * L:(s + 1) * L]
    return out
```

```python
import contextlib
import numpy as np
import ml_dtypes
import concourse.bass as bass
import concourse.mybir as mybir
from concourse.bass_utils import run_bass_kernel_spmd

F32 = mybir.dt.float32
BF16 = mybir.dt.bfloat16
ALU = mybir.AluOpType
AF = mybir.ActivationFunctionType
AX = mybir.AxisListType

P = 128
D = 2048
KC = 16
S = 8192
NB = 32
L = 256
H = 8
DH = 128
C = 1024
CC = 8
DFF = 8192
INC = 5120
NSLOT = 8
HALO = 32
SLOTW = L + HALO
NOWN = NSLOT * L
NOWNH = NSLOT * SLOTW
SCALE = DH ** -0.5
RMS_EPS = 1e-6
LN_EPS = 1e-5
CW = 31

STREAMS = ("pe", "act", "dve", "pool", "sp")


class Buf:
    __slots__ = ("name", "writer", "readers", "inherit", "excl")

    def __init__(self, name, inherit=(), excl=False):
        self.name = name
        self.excl = excl
        self.writer = None
        self.readers = []
        self.inherit = list(inherit)


class Op:
    __slots__ = ("stream", "emit", "deps", "is_dma", "semkey", "signal", "name")

    def __init__(self, stream, emit, is_dma=False, semkey=None, name=""):
        self.stream = stream
        self.emit = emit
        self.deps = []
        self.is_dma = is_dma
        self.semkey = semkey
        self.signal = False
        self.name = name


class Prog:
    def __init__(self, nc):
        self.nc = nc
        self.ops = {s: [] for s in STREAMS}
        self.all_ops = []
        self.final_waits = []

    def _add(self, op, reads, writes, after=()):
        deps = []
        for b in after:
            if b.writer is not None:
                deps.append(b.writer)
        for b in reads:
            if b.writer is not None:
                deps.append(b.writer)
            elif b.inherit:
                deps.extend(b.inherit)
            if b.excl:
                deps.extend(r for r in b.readers if r.stream != op.stream)
        for b in writes:
            if b.writer is not None:
                deps.append(b.writer)
            deps.extend(b.readers)
            if b.inherit:
                deps.extend(b.inherit)
                b.inherit = []
        seen = set()
        for d in deps:
            if d is op or id(d) in seen:
                continue
            seen.add(id(d))
            op.deps.append(d)
        for b in reads:
            b.readers.append(op)
        for b in writes:
            b.writer = op
            b.readers = []
        self.ops[op.stream].append(op)
        self.all_ops.append(op)
        return op

    def op(self, stream, emit, reads=(), writes=(), name=""):
        return self._add(Op(stream, emit, name=name), reads, writes)

    def dma(self, stream, out, in_, semkey, reads=(), writes=(), name="", after=()):
        def emit(eng):
            return eng.dma_start(out=out, in_=in_)
        return self._add(Op(stream, emit, is_dma=True, semkey=semkey, name=name), reads, writes, after)

    def must_finish(self, op):
        self.final_waits.append(op)

    def emit_all(self, stack):
        nc = self.nc
        for op in self.all_ops:
            for d in op.deps:
                d.signal = True
        for op in self.final_waits:
            op.signal = True
        eng_sem = {s: stack.enter_context(nc.semaphore("done_" + s)) for s in STREAMS}
        dma_sems = {}
        dma_cnt = {}
        cnt = {s: 0 for s in STREAMS}
        comp = {}
        for op in self.all_ops:
            s = op.stream
            if op.is_dma:
                k = op.semkey
                if k not in dma_sems:
                    dma_sems[k] = stack.enter_context(nc.semaphore("dq_%d" % len(dma_sems)))
                    dma_cnt[k] = 0
                dma_cnt[k] += 16
                comp[id(op)] = (dma_sems[k], dma_cnt[k])
            elif op.signal:
                cnt[s] += 1
                comp[id(op)] = (eng_sem[s], cnt[s])
        self.n_sems = len(dma_sems) + len(STREAMS)
        self.max_count = dict(cnt)
        block = stack.enter_context(nc.Block())
        prog = self

        def run_stream(s, eng):
            waited = {}
            for op in prog.ops[s]:
                need = {}
                for d in op.deps:
                    sem, val = comp[id(d)]
                    key = id(sem)
                    if waited.get(key, 0) >= val:
                        continue
                    if key not in need or need[key][1] < val:
                        need[key] = (sem, val)
                for key, (sem, val) in need.items():
                    waited[key] = val
                    eng.wait_ge(sem, val)
                ins = op.emit(eng)
                if op.is_dma:
                    ins.then_inc(comp[id(op)][0], 16)
                elif op.signal:
                    ins.then_inc(eng_sem[s], 1)
            if s == "sp":
                for op in prog.final_waits:
                    sem, val = comp[id(op)]
                    eng.wait_ge(sem, val)

        @block.tensor
        def _(e):
            run_stream("pe", e)

        @block.scalar
        def _(e):
            run_stream("act", e)

        @block.vector
        def _(e):
            run_stream("dve", e)

        @block.gpsimd
        def _(e):
            run_stream("pool", e)

        @block.sync
        def _(e):
            run_stream("sp", e)


DT_SIZE = {F32: 4, BF16: 2}


class Arena:
    def __init__(self, nc, stack, kib):
        self.words = kib * 256
        self.t = stack.enter_context(nc.sbuf_tensor("arena", [P, self.words], F32))
        self.top = 0
        self.peak = 0
        self.retired = []
        self.live = []

    def alloc(self, name, shape, dtype):
        n = 1
        for s in shape:
            n *= s
        nbytes = (n * DT_SIZE[dtype] + 31) // 32 * 32
        lo = self.top
        hi = lo + nbytes
        assert hi <= self.words * 4, "SBUF arena overflow at %s: %d > %d" % (name, hi, self.words * 4)
        self.top = hi
        self.peak = max(self.peak, hi)
        inh = []
        keep = []
        for (l, h, ops) in self.retired:
            if l < hi and h > lo:
                inh.extend(ops)
                if l >= lo and h <= hi:
                    continue
            keep.append((l, h, ops))
        self.retired = keep
        buf = Buf(name, inherit=inh)
        v = self.t[:, lo // 4:hi // 4]
        if dtype != F32:
            v = v.bitcast(dtype)
        v = v[:, 0:n]
        if len(shape) == 2:
            v = v.rearrange("p (a b) -> p a b", a=shape[0], b=shape[1])
        elif len(shape) == 3:
            v = v.rearrange("p (a b c) -> p a b c", a=shape[0], b=shape[1], c=shape[2])
        elif len(shape) == 4:
            v = v.rearrange("p (a b c d) -> p a b c d", a=shape[0], b=shape[1], c=shape[2], d=shape[3])
        self.live.append((lo, hi, buf))
        return v, buf

    def mark(self):
        return self.top

    def release(self, mark):
        keep = []
        for (lo, hi, buf) in self.live:
            if lo >= mark:
                ops = list(buf.readers) + list(buf.inherit)
                if buf.writer is not None:
                    ops.append(buf.writer)
                if ops:
                    self.retired.append((lo, hi, ops))
            else:
                keep.append((lo, hi, buf))
        self.live = keep
        self.top = mark


def own_blocks(j):
    return [8 * (s // 2) + (j if s % 2 == 0 else 7 - j) for s in range(NSLOT)]


PAST = [8 * (s // 2) + (3 if s % 2 == 0 else 7) for s in range(NSLOT)]


def build_nc(debug=False, stop_after="all"):
    nc = bass.Bass("TRN2", target_bir_lowering=False)
    skind = "ExternalOutput" if debug else "Internal"

    def din(name, shape, dt=F32):
        return nc.dram_tensor(name, list(shape), dt, kind="ExternalInput").ap()

    def dscr(name, shape, dt):
        return nc.dram_tensor(name, list(shape), dt, kind=skind).ap()

    xall_d = din("xall", [S, D])
    xown_d = din("xown", [NOWNH, D])
    w_in_d = din("w_in", [D, INC])
    w_out_d = din("w_out", [D, D])
    w_ff1_d = din("w_ff1", [D, DFF])
    w_ff2_d = din("w_ff2", [DFF, D])
    grow_d = din("grow", [4, D])
    cols_d = din("cols", [P, 16 + CC * CW + 3 * CC])
    fd_d = din("fd", [NSLOT, P, 2 * H * NB])
    gbias_d = din("gbias", [P, NSLOT * NB])
    hmask_d = din("hmask", [P, NSLOT])
    bkt_d = din("bkt", [P, H * 2])
    fown_d = din("fown", [P, H])
    cvec_d = din("cvec", [P, H * DH])
    tri_d = din("tri", [P, P], BF16)
    ident_d = din("ident", [P, P], BF16)

    y_d = nc.dram_tensor("y", [NOWN, D], F32, kind="ExternalOutput").ap()

    w_in_bf = dscr("w_in_bf", [D, INC], BF16)
    w_out_bf = dscr("w_out_bf", [D, D], BF16)
    w_ff1_bf = dscr("w_ff1_bf", [D, DFF], BF16)
    w_ff2_bf = dscr("w_ff2_bf", [DFF, D], BF16)
    KT_d = dscr("KT", [H, DH, S], BF16)
    V_d = dscr("V", [S, H * DH], BF16)
    KTo_d = dscr("KTo", [H, DH, NOWN], BF16)
    Vo_d = dscr("Vo", [NOWN, H * DH], BF16)
    QT_d = dscr("QT", [H, DH, NOWN], BF16)
    F_d = dscr("Fsel", [H, P, 16 * NB], F32)
    yT_d = dscr("yT", [CC, P, NOWN], F32)
    mixT_d = dscr("mixT", [D, NOWN], BF16)
    kmean_dbg = dscr("kmean_dbg", [P, H * NB], F32) if debug else None

    with contextlib.ExitStack() as st:
        pr = Prog(nc)
        ar = Arena(nc, st, 204)
        psum = []
        pbuf = []
        for i in range(8):
            psum.append(st.enter_context(nc.psum_tensor("ps%d" % i, [P, 512], F32)))
            pbuf.append(Buf("ps%d" % i, excl=True))

        def ps_f32(i, n=512):
            return psum[i][:, 0:n]

        def ps_bf16(i):
            return psum[i][:, :].bitcast(BF16)

        cols, B_cols = ar.alloc("cols", [16 + CC * CW + 3 * CC], F32)
        pr.dma("sp", cols, cols_d, "c0", writes=[B_cols])
        bglu = cols[:, 0:16]
        wdw = cols[:, 16:16 + CC * CW].rearrange("p (c j) -> p c j", c=CC, j=CW)
        o0 = 16 + CC * CW
        bdw = cols[:, o0:o0 + CC]
        lng = cols[:, o0 + CC:o0 + 2 * CC]
        lnb = cols[:, o0 + 2 * CC:o0 + 3 * CC]
        ident, B_ident = ar.alloc("ident", [P], BF16)
        pr.dma("sp", ident, ident_d, "c1", writes=[B_ident])
        tri, B_tri = ar.alloc("tri", [P], BF16)
        pr.dma("sp", tri, tri_d, "c2", writes=[B_tri])
        bkt, B_bkt = ar.alloc("bkt", [H, 2], F32)
        pr.dma("sp", bkt, bkt_d.rearrange("p (h t) -> p h t", h=H, t=2), "c3", writes=[B_bkt])
        fown, B_fown = ar.alloc("fown", [H], F32)
        pr.dma("sp", fown, fown_d, "c4", writes=[B_fown])
        gbias, B_gbias = ar.alloc("gbias", [NSLOT, NB], F32)
        pr.dma("sp", gbias, gbias_d.rearrange("p (s n) -> p s n", s=NSLOT, n=NB), "c5", writes=[B_gbias])
        hmask, B_hmask = ar.alloc("hmask", [NSLOT], F32)
        pr.dma("sp", hmask, hmask_d, "c6", writes=[B_hmask])
        cvec, B_cvec = ar.alloc("cvec", [H * DH], F32)
        pr.dma("sp", cvec, cvec_d, "c8", writes=[B_cvec])
        kmean, B_kmean = ar.alloc("kmean", [H, NB], F32)
        ones32, B_ones = ar.alloc("ones32", [P], F32)
        pr.op("pool", lambda e: e.memset(ones32, 1.0), writes=[B_ones])
        ss_t, _ = ar.alloc("ss", [8], F32)
        rs_t, _ = ar.alloc("rs", [8], F32)
        B_ss = [Buf("ss%d" % i) for i in range(8)]
        B_rs = [Buf("rs%d" % i) for i in range(8)]
        stat_ctr = [0]

        def in_col_bufs(c0):
            if c0 < 1024:
                return B_wq
            if c0 < 3072:
                return B_wkv
            return B_wu

        def norm_T_tile(x_src, xs, B_xs, xs_key, g_b, B_gb, abf, B_abf, dstT, B_dstT, col0, tbanks):
            norm_tile(x_src, xs, B_xs, xs_key, g_b, B_gb, abf, B_abf)
            T_tile(abf, B_abf, dstT, B_dstT, col0, tbanks)

        def norm_tile(x_src, xs, B_xs, xs_key, g_b, B_gb, abf, B_abf):
            i = stat_ctr[0] % 8
            stat_ctr[0] += 1
            ssv = ss_t[:, i:i + 1]
            rsv = rs_t[:, i:i + 1]
            if x_src is not None:
                pr.dma("sp", xs, x_src, xs_key, writes=[B_xs])
            pr.op("act", lambda e: e.activation(out=abf, in_=xs, func=AF.Square, accum_out=ssv),
                  reads=[B_xs], writes=[B_ss[i], B_abf])
            pr.op("act", lambda e: e.activation(out=rsv, in_=ssv, func=AF.Sqrt, bias=RMS_EPS, scale=1.0 / D),
                  reads=[B_ss[i]], writes=[B_rs[i]])
            pr.op("dve", lambda e: e.reciprocal(out=rsv, in_=rsv), reads=[B_rs[i]], writes=[B_rs[i]])
            pr.op("dve", lambda e: e.scalar_tensor_tensor(out=abf, in0=xs, scalar=rsv, in1=g_b,
                                                          op0=ALU.mult, op1=ALU.mult),
                  reads=[B_xs, B_rs[i], B_gb], writes=[B_abf])

        def T_tile(abf, B_abf, dstT, B_dstT, col0, tbanks):
            for half in range(2):
                bank = tbanks[half]
                pt = ps_bf16(bank).rearrange("p (k n) -> p k n", k=8, n=P)

                def tr(e, half=half, pt=pt):
                    ins = None
                    for kk in range(8):
                        k = half * 8 + kk
                        ins = e.transpose(out=pt[:, kk, :], in_=abf[:, k * P:(k + 1) * P], identity=ident)
                    return ins
                pr.op("pe", tr, reads=[B_abf, B_ident], writes=[pbuf[bank]])
                dst = dstT[:, half * 8:(half + 1) * 8, col0:col0 + P]
                if half == 0:
                    pr.op("act", lambda e, dst=dst, pt=pt: e.copy(out=dst, in_=pt), reads=[pbuf[bank]], writes=[B_dstT])
                else:
                    pr.op("dve", lambda e, dst=dst, pt=pt: e.tensor_copy(out=dst, in_=pt), reads=[pbuf[bank]],
                          writes=[B_dstT])

        def mm_group(out_ap, pairs, reads, writes, name=""):
            def emit(e):
                ins = None
                n = len(pairs)
                for i, (l, r) in enumerate(pairs):
                    ins = e.matmul(out_ap, lhsT=l, rhs=r, start=(i == 0), stop=(i == n - 1))
                return ins
            return pr.op("pe", emit, reads=reads, writes=writes, name=name)

        mA = ar.mark()
        gpre_b, B_gpre = ar.alloc("gpre_b", [D], F32)
        pr.dma("sp", gpre_b, grow_d[0:1, :].partition_broadcast(P), "c7", writes=[B_gpre])
        wkv, _ = ar.alloc("wkv", [KC, 2048], BF16)
        B_wkvS = [Buf("wkvS%d" % i) for i in range(4)]
        wsrc = w_in_d[:, 1024:3072].rearrange("(k p) n -> p k n", p=P)
        for i in range(4):
            pr.dma("pool", wkv[:, 4 * i:4 * i + 4, :], wsrc[:, 4 * i:4 * i + 4, :], "wkvS%d" % i, writes=[B_wkvS[i]])
        bg_queue = []

        def cast_group(name, dst, src, pieces):
            bufs = []
            for i, (dsl, ssl) in enumerate(pieces):
                b = Buf("%s_%d" % (name, i))
                bg_queue.append((dst[dsl], src[ssl], "cast_" + name, b))
                bufs.append(b)
            return bufs

        def bg_issue(n, gates):
            for _ in range(n):
                if not bg_queue:
                    return
                d_, s_, k_, b_ = bg_queue.pop(0)
                pr.dma("pool", d_, s_, k_, writes=[b_], after=gates)

        def issued(bufs):
            assert all(b.writer is not None for b in bufs), "weight cast not issued before its consumer"
            return bufs

        def rows4(c0, c1, nrows=D):
            q = nrows // 4
            return [((slice(i * q, (i + 1) * q), slice(c0, c1)),) * 2 for i in range(4)]

        B_wkv = cast_group("wkv", w_in_bf, w_in_d, rows4(1024, 3072))
        B_wq = cast_group("wq", w_in_bf, w_in_d, rows4(0, 1024))
        B_wu = cast_group("wu", w_in_bf, w_in_d, rows4(3072, 5120))
        B_wout = cast_group("wout", w_out_bf, w_out_d, rows4(0, D))
        B_wff1 = []
        B_wff2 = []
        for g in range(4):
            B_wff1.append(cast_group("wff1_%d" % g, w_ff1_bf, w_ff1_d, rows4(g * 2048, (g + 1) * 2048)))
            pcs = [((slice(g * 2048 + i * 512, g * 2048 + (i + 1) * 512), slice(0, D)),) * 2 for i in range(4)]
            B_wff2.append(cast_group("wff2_%d" % g, w_ff2_bf, w_ff2_d, pcs))

        xsA = [ar.alloc("xsA%d" % i, [D], F32) for i in range(4)]
        abA = [ar.alloc("abA%d" % i, [D], BF16) for i in range(4)]
        aTA = [ar.alloc("aTA%d" % i, [KC, 512], BF16) for i in range(2)]
        ktst = [ar.alloc("ktst%d" % i, [H, 512], BF16) for i in range(2)]
        vst = [ar.alloc("vst%d" % i, [1024], BF16) for i in range(2)]
        kmsum, B_kmsum = ar.alloc("kmsum", [H, NB], F32)
        NG = S // 512
        KT_v = KT_d.rearrange("h d t -> d h t")
        TB = (0, 1)
        KB = (2, 3)
        VB = (4, 5)
        tile_ctr = [0]

        def A_norm_tile(g, t):
            r0 = g * 512 + t * P
            norm_tile(xall_d[r0:r0 + P, :], xsA[t][0], xsA[t][1], "xsA%d" % t, gpre_b, B_gpre, abA[t][0], abA[t][1])

        def A_T(g):
            aT, B_aT = aTA[g % 2]
            for t in range(4):
                T_tile(abA[t][0], abA[t][1], aT, B_aT, t * P, TB)

        def A_kv(g):
            aT, B_aT = aTA[g % 2]
            kst, B_kst = ktst[g % 2]
            for h in range(H):
                if h % 2 == 1 and g + 2 < NG:
                    A_norm_tile(g + 2, h // 2)
                bank = KB[h % 2]
                pk = ps_f32(bank)
                mm_group(pk, [(wkv[:, k, h * DH:(h + 1) * DH], aT[:, k, :]) for k in range(KC)],
                         reads=[B_aT] + B_wkvS, writes=[pbuf[bank]])
                pr.op("act", lambda e, pk=pk, h=h: e.copy(out=kst[:, h, :], in_=pk), reads=[pbuf[bank]], writes=[B_kst])
                pr.op("dve", lambda e, h=h: e.tensor_reduce(
                    out=kmsum[:, h, 2 * g:2 * g + 2], in_=kst[:, h, :].rearrange("p (a b) -> p a b", a=2, b=L),
                    axis=AX.X, op=ALU.add), reads=[B_kst], writes=[B_kmsum])
            pr.dma("pool", KT_v[:, :, g * 512:(g + 1) * 512], kst, "ktst%d" % (g % 2), reads=[B_kst], writes=[B_KT])
            for t in range(4):
                vi = (g * 4 + t) % 2
                vs, B_vs = vst[vi]
                for half in range(2):
                    bank = VB[half]
                    pv = ps_f32(bank)
                    mm_group(pv, [(aT[:, k, t * P:(t + 1) * P], wkv[:, k, 1024 + half * 512:1024 + (half + 1) * 512])
                                  for k in range(KC)], reads=[B_aT] + B_wkvS, writes=[pbuf[bank]])
                    hs_ = slice(half * 512, (half + 1) * 512)
                    if t % 2 == 0:
                        pr.op("dve", lambda e, pv=pv, vs=vs, hs_=hs_: e.tensor_tensor(
                            out=vs[:, hs_], in0=pv, in1=cvec[:, hs_], op=ALU.mult),
                            reads=[pbuf[bank], B_cvec], writes=[B_vs])
                    else:
                        pr.op("act", lambda e, pv=pv, vs=vs, hs_=hs_: e.copy(out=vs[:, hs_], in_=pv),
                              reads=[pbuf[bank]], writes=[B_vs])
                r0 = g * 512 + t * P
                pr.dma("pool", V_d[r0:r0 + P, :], vs, "vst%d" % vi, reads=[B_vs], writes=[B_V])

        B_KT = Buf("KT_d")
        B_V = Buf("V_d")
        for t in range(4):
            A_norm_tile(0, t)
        A_T(0)
        for t in range(4):
            A_norm_tile(1, t)
        for g in range(NG):
            if g + 1 < NG:
                A_T(g + 1)
            A_kv(g)
            if 2 <= g < 14:
                bg_issue(1, [ktst[g % 2][1]])
        pr.op("dve", lambda e: e.tensor_scalar(out=kmean, in0=kmsum, scalar1=1.0 / L, scalar2=None, op0=ALU.mult),
              reads=[B_kmsum], writes=[B_kmean])
        if debug:
            o = pr.dma("sp", kmean_dbg, kmean.rearrange("p h n -> p (h n)"), "dbg0", reads=[B_kmean])
            pr.must_finish(o)
        ar.release(mA)

        last_ops = []
        if stop_after == "A":
            return _finish(nc, pr, st, ar, [B_KT, B_V])

        mB = ar.mark()
        gpre_b, B_gpre = ar.alloc("gpre_b2", [D], F32)
        pr.dma("sp", gpre_b, grow_d[0:1, :].partition_broadcast(P), "c7", writes=[B_gpre])
        aTo, B_aTo = ar.alloc("aTo", [KC, NOWNH], BF16)
        Fall, B_Fall = ar.alloc("Fall", [16, H, NB], F32)
        mB1 = ar.mark()
        xsB = [ar.alloc("xsB%d" % i, [D], F32) for i in range(2)]
        abB = [ar.alloc("abB%d" % i, [D], BF16) for i in range(2)]
        for t in range(NOWNH // P):
            i = t % 2
            norm_T_tile(xown_d[t * P:(t + 1) * P, :], xsB[i][0], xsB[i][1], "xsA%d" % i, gpre_b, B_gpre,
                        abB[i][0], abB[i][1], aTo, B_aTo, t * P, TB)
        ar.release(mB1)
        NWS = 3
        wch = [ar.alloc("wch%d" % i, [KC, 512], BF16) for i in range(NWS)]
        wch_ctr = [0]

        def load_wchunk(src_ap, reads):
            i = wch_ctr[0] % NWS
            wch_ctr[0] += 1
            w, B_w = wch[i]
            pr.dma("sp", w, src_ap, "wch%d" % i, reads=reads, writes=[B_w])
            return w, B_w

        def in_chunk_src(c0):
            return w_in_bf[:, c0:c0 + 512].rearrange("(k p) n -> p k n", p=P)

        qst = [ar.alloc("qst%d" % i, [L], BF16) for i in range(2)]
        q32 = [ar.alloc("q32_%d" % i, [L], F32) for i in range(2)]
        kost = [ar.alloc("kost%d" % i, [L], BF16) for i in range(2)]
        vost = [ar.alloc("vost%d" % i, [512], BF16) for i in range(2)]
        fdt = [ar.alloc("fdt%d" % i, [2, H, NB], F32) for i in range(2)]
        gsb = [ar.alloc("gsb%d" % i, [NB], F32) for i in range(2)]
        top8 = [ar.alloc("top8_%d" % i, [8], F32) for i in range(2)]
        sig = [ar.alloc("sig%d" % i, [SLOTW], F32) for i in range(2)]
        hst = [ar.alloc("hst%d" % i, [SLOTW], BF16) for i in range(2)]
        dgb = [ar.alloc("dgb%d" % i, [CW, P], BF16) for i in range(2)]
        CVB = (6, 7)
        accD = [ar.alloc("accD%d" % i, [L], F32) for i in range(2)]
        B_QT = Buf("QT_d")
        B_KTo = Buf("KTo_d")
        B_Vo = Buf("Vo_d")
        B_yT = Buf("yT_d")
        B_F = Buf("F_d")
        PB = (2, 3, 4, 5)
        GB = 6
        pb_ctr = [0]

        def nbank():
            b = PB[pb_ctr[0] % len(PB)]
            pb_ctr[0] += 1
            return b
        ctr = {"q": 0, "k": 0, "v": 0, "u": 0, "g": 0, "c": 0}
        conv_pending = []

        chunk_list = [("q", 0), ("q", 512), ("k", 1024), ("k", 1536), ("v", 2048), ("v", 2560),
                      ("uv", 3072), ("ug", 4096), ("uv", 3584), ("ug", 4608)]
        pending = None
        nxt = load_wchunk(in_chunk_src(chunk_list[0][1]), in_col_bufs(chunk_list[0][1]))
        for ci, (kind, c0) in enumerate(chunk_list):
            w, B_w = nxt
            if ci + 1 < len(chunk_list):
                nxt = load_wchunk(in_chunk_src(chunk_list[ci + 1][1]), issued(in_col_bufs(chunk_list[ci + 1][1])))
            if ci >= 1:
                gate = {"q": B_QT, "k": B_KTo, "v": B_Vo, "uv": B_yT, "ug": B_yT}[chunk_list[ci - 1][0]]
                bg_issue(1, [gate])
            if kind in ("q", "k"):
                for s in range(NSLOT):
                    if kind == "q":
                        fi = ctr["g"] % 2
                        ctr["g"] += 1
                        fdv, B_fd = fdt[fi]
                        pr.dma("sp", fdv, fd_d[s].rearrange("p (t h n) -> p t h n", t=2, h=H, n=NB), "fdt%d" % fi,
                               writes=[B_fd])
                    for sub in range(4):
                        h = (c0 % 1024) // DH + sub
                        bank = nbank()
                        pq = ps_f32(bank, SLOTW)
                        mm_group(pq, [(w[:, k, sub * DH:(sub + 1) * DH], aTo[:, k, s * SLOTW:(s + 1) * SLOTW])
                                      for k in range(KC)], reads=[B_aTo, B_w], writes=[pbuf[bank]])
                        if kind == "k":
                            i = ctr["k"] % 2
                            ctr["k"] += 1
                            ks, B_ks = kost[i]
                            pr.op("act", lambda e, ks=ks, pq=pq: e.copy(out=ks, in_=pq[:, HALO:SLOTW]),
                                  reads=[pbuf[bank]], writes=[B_ks])
                            pr.dma("pool", KTo_d[h, :, s * L:(s + 1) * L], ks, "kost%d" % i, reads=[B_ks], writes=[B_KTo])
                            continue
                        i = ctr["q"] % 2
                        ctr["q"] += 1
                        qs, B_qs = qst[i]
                        qf, B_qf = q32[i]
                        pr.op("act", lambda e, qf=qf, pq=pq: e.copy(out=qf, in_=pq[:, HALO:SLOTW]),
                              reads=[pbuf[bank]], writes=[B_qf])
                        pr.op("dve", lambda e, qs=qs, qf=qf: e.tensor_copy(out=qs, in_=qf), reads=[B_qf], writes=[B_qs])
                        pr.dma("pool", QT_d[h, :, s * L:(s + 1) * L], qs, "qst%d" % i, reads=[B_qs], writes=[B_QT])
                        pg = ps_f32(GB, 2 * NB).rearrange("p (t n) -> p t n", t=2, n=NB)

                        def gmm(e, qf=qf, h=h, pg=pg):
                            e.matmul(pg[:, 0, :], lhsT=qf[:, 0:P], rhs=kmean[:, h, :], start=True, stop=True)
                            return e.matmul(pg[:, 1, :], lhsT=qf[:, P:2 * P], rhs=kmean[:, h, :], start=True, stop=True)
                        pr.op("pe", gmm, reads=[B_qf, B_kmean], writes=[pbuf[GB]])
                        for qt in range(2):
                            gi = ctr["u"] % 2
                            ctr["u"] += 1
                            gs, B_gs = gsb[gi]
                            t8, B_t8 = top8[gi]
                            pr.op("dve", lambda e, gs=gs, qt=qt, pg=pg, s=s: e.tensor_tensor(
                                out=gs, in0=pg[:, qt, :], in1=gbias[:, s, :], op=ALU.add),
                                reads=[pbuf[GB], B_gbias], writes=[B_gs])
                            pr.op("dve", lambda e, gs=gs, t8=t8: e.max(out=t8, in_=gs), reads=[B_gs], writes=[B_t8])
                            pr.op("dve", lambda e, gs=gs, t8=t8, qt=qt, h=h, s=s, fdv=fdv: e.scalar_tensor_tensor(
                                out=Fall[:, 2 * s + qt, h, :], in0=gs, scalar=t8[:, 2:3], in1=fdv[:, qt, h, :],
                                op0=ALU.is_ge, op1=ALU.mult), reads=[B_gs, B_t8, B_fd], writes=[B_Fall])
            elif kind == "v":
                for s in range(NSLOT):
                    for t in range(2):
                        bank = nbank()
                        pv = ps_f32(bank)
                        col = s * SLOTW + HALO + t * P
                        mm_group(pv, [(aTo[:, k, col:col + P], w[:, k, :]) for k in range(KC)],
                                 reads=[B_aTo, B_w], writes=[pbuf[bank]])
                        i = ctr["v"] % 2
                        ctr["v"] += 1
                        vs, B_vs = vost[i]
                        pr.op("act", lambda e, vs=vs, pv=pv: e.copy(out=vs, in_=pv), reads=[pbuf[bank]], writes=[B_vs])
                        r0 = s * L + t * P
                        cv = c0 - 2048
                        pr.dma("pool", Vo_d[r0:r0 + P, cv:cv + 512], vs, "vost%d" % i, reads=[B_vs], writes=[B_Vo])
            elif kind == "uv":
                pending = (w, B_w, c0)
            else:
                wv, B_wv, cv0 = pending
                for sub in range(4):
                    cch = (cv0 - 3072) // P + sub
                    dgv, B_dg = dgb[cch % 2]
                    for jt in range(CW):
                        pr.op("dve", lambda e, dgv=dgv, cch=cch, jt=jt: e.tensor_scalar(
                            out=dgv[:, jt, :], in0=ident, scalar1=wdw[:, cch, jt:jt + 1], scalar2=None, op0=ALU.mult),
                            reads=[B_ident, B_cols], writes=[B_dg])
                    for s in range(NSLOT):
                        bv = nbank()
                        bg = nbank()
                        pval = ps_f32(bv, SLOTW)
                        pgt = ps_f32(bg, SLOTW)
                        rhs_sl = slice(s * SLOTW, (s + 1) * SLOTW)
                        mm_group(pval, [(wv[:, k, sub * P:(sub + 1) * P], aTo[:, k, rhs_sl]) for k in range(KC)],
                                 reads=[B_aTo, B_wv], writes=[pbuf[bv]])
                        mm_group(pgt, [(w[:, k, sub * P:(sub + 1) * P], aTo[:, k, rhs_sl]) for k in range(KC)],
                                 reads=[B_aTo, B_w], writes=[pbuf[bg]])
                        i = ctr["c"] % 2
                        ctr["c"] += 1
                        sg, B_sg = sig[i]
                        hs, B_hs = hst[i]
                        aD, B_aD = accD[i]
                        pr.op("act", lambda e, sg=sg, pgt=pgt, cch=cch: e.activation(
                            out=sg, in_=pgt, func=AF.Sigmoid, bias=bglu[:, 8 + cch:9 + cch], scale=1.0),
                            reads=[pbuf[bg], B_cols], writes=[B_sg])
                        pr.op("dve", lambda e, hs=hs, pval=pval, sg=sg, cch=cch: e.scalar_tensor_tensor(
                            out=hs, in0=pval, scalar=bglu[:, cch:cch + 1], in1=sg, op0=ALU.add, op1=ALU.mult),
                            reads=[pbuf[bv], B_sg, B_cols], writes=[B_hs])
                        pr.op("dve", lambda e, hs=hs, s=s: e.tensor_scalar(
                            out=hs[:, 0:HALO], in0=hs[:, 0:HALO], scalar1=hmask[:, s:s + 1], scalar2=None, op0=ALU.mult),
                            reads=[B_hs, B_hmask], writes=[B_hs])
                        def conv_unit(dgv=dgv, B_dg=B_dg, hs=hs, B_hs=B_hs, aD=aD, B_aD=B_aD, cch=cch, s=s, i=i,
                                      cb=CVB[ctr["c"] % 2]):
                            pc = ps_f32(cb, L)
                            mm_group(pc, [(dgv[:, jt, :], hs[:, 2 + jt:2 + jt + L]) for jt in range(CW)],
                                     reads=[B_dg, B_hs], writes=[pbuf[cb]])
                            pr.op("dve", lambda e: e.tensor_scalar(
                                out=aD, in0=pc, scalar1=bdw[:, cch:cch + 1], scalar2=None, op0=ALU.add),
                                reads=[pbuf[cb], B_cols], writes=[B_aD])
                            pr.dma("pool", yT_d[cch, :, s * L:(s + 1) * L], aD, "accD%d" % i, reads=[B_aD],
                                   writes=[B_yT])
                        if conv_pending:
                            conv_pending.pop(0)()
                        conv_pending.append(conv_unit)
        while conv_pending:
            conv_pending.pop(0)()
        for h in range(H):
            pr.dma("pool", F_d[h].rearrange("p (q n) -> p q n", q=16, n=NB), Fall[:, :, h, :], "fst", reads=[B_Fall],
                   writes=[B_F])
        ar.release(mB)
        if stop_after == "B":
            return _finish(nc, pr, st, ar, [B_KT, B_V, B_QT, B_KTo, B_Vo, B_yT, B_F])

        B_mixT = Buf("mixT_d")
        mL = ar.mark()
        ysl = [ar.alloc("ysl%d" % i, [CC, L], F32) for i in range(2)]
        sqs = [ar.alloc("sqs%d" % i, [CC, L], F32) for i in range(2)]
        cst = [ar.alloc("cst%d" % i, [CC, L], BF16) for i in range(2)]
        mean_t, B_mean = ar.alloc("mean_t", [L], F32)
        msq_t, B_msq = ar.alloc("msq_t", [L], F32)
        rstd_t, B_rstdL = ar.alloc("rstd_t", [L], F32)
        mr_t, B_mr = ar.alloc("mr_t", [L], F32)
        tmpL = [ar.alloc("tmpL%d" % i, [L], F32) for i in range(2)]
        yT_v = yT_d.rearrange("c p t -> p c t")
        for s in range(NSLOT):
            i = s % 2
            ys, B_ys = ysl[i]
            sq, B_sq = sqs[i]
            cs, B_cs = cst[i]
            pr.dma("sp", ys, yT_v[:, :, s * L:(s + 1) * L], "ysl%d" % i, reads=[B_yT], writes=[B_ys])
            pr.op("act", lambda e, ys=ys, sq=sq: e.activation(out=sq, in_=ys, func=AF.Square), reads=[B_ys], writes=[B_sq])
            pm = ps_f32(2, L)
            pq2 = ps_f32(3, L)
            mm_group(pm, [(ones32, ys[:, c, :]) for c in range(CC)], reads=[B_ones, B_ys], writes=[pbuf[2]])
            mm_group(pq2, [(ones32, sq[:, c, :]) for c in range(CC)], reads=[B_ones, B_sq], writes=[pbuf[3]])
            pr.op("dve", lambda e, pm=pm: e.tensor_scalar(out=mean_t, in0=pm, scalar1=1.0 / C, scalar2=None, op0=ALU.mult),
                  reads=[pbuf[2]], writes=[B_mean])
            pr.op("dve", lambda e: e.tensor_tensor(out=msq_t, in0=mean_t, in1=mean_t, op=ALU.mult),
                  reads=[B_mean], writes=[B_msq])
            pr.op("dve", lambda e, pq2=pq2: e.scalar_tensor_tensor(out=rstd_t, in0=pq2, scalar=1.0 / C, in1=msq_t,
                                                                  op0=ALU.mult, op1=ALU.subtract),
                  reads=[pbuf[3], B_msq], writes=[B_rstdL])
            pr.op("act", lambda e: e.activation(out=rstd_t, in_=rstd_t, func=AF.Sqrt, bias=LN_EPS, scale=1.0),
                  reads=[B_rstdL], writes=[B_rstdL])
            pr.op("dve", lambda e: e.reciprocal(out=rstd_t, in_=rstd_t), reads=[B_rstdL], writes=[B_rstdL])
            pr.op("dve", lambda e: e.tensor_tensor(out=mr_t, in0=mean_t, in1=rstd_t, op=ALU.mult),
                  reads=[B_mean, B_rstdL], writes=[B_mr])
            for c in range(CC):
                tl, B_tl = tmpL[c % 2]
                pr.op("dve", lambda e, tl=tl, ys=ys, c=c: e.tensor_tensor(out=tl, in0=ys[:, c, :], in1=rstd_t, op=ALU.mult),
                      reads=[B_ys, B_rstdL], writes=[B_tl])
                pr.op("dve", lambda e, tl=tl: e.tensor_tensor(out=tl, in0=tl, in1=mr_t, op=ALU.subtract),
                      reads=[B_tl, B_mr], writes=[B_tl])
                pr.op("act", lambda e, tl=tl, cs=cs, c=c: e.activation(out=cs[:, c, :], in_=tl, func=AF.Silu,
                                                                     bias=lnb[:, c:c + 1], scale=lng[:, c:c + 1]),
                      reads=[B_tl, B_cols], writes=[B_cs])
            pr.dma("pool", mixT_d[C:2 * C, s * L:(s + 1) * L].rearrange("(c p) t -> p c t", p=P), cs, "cst%d" % i,
                   reads=[B_cs], writes=[B_mixT])
        ar.release(mL)
        if stop_after == "L":
            return _finish(nc, pr, st, ar, [B_KT, B_V, B_QT, B_KTo, B_Vo, B_yT, B_F, B_mixT])

        mT = ar.mark()
        NT = S // P
        VW = DH + 2
        KTh = [ar.alloc("KTh%d" % i, [S], BF16) for i in range(2)]
        Vh = [ar.alloc("Vh%d" % i, [NT, VW], BF16) for i in range(2)]
        KToh = [ar.alloc("KToh%d" % i, [NOWN], BF16) for i in range(2)]
        Voh = [ar.alloc("Voh%d" % i, [NOWN // P, VW], BF16) for i in range(2)]
        QTh = [ar.alloc("QTh%d" % i, [NOWN], BF16) for i in range(2)]
        Fh = [ar.alloc("Fh%d" % i, [16, NB], F32) for i in range(2)]
        pTs = [ar.alloc("pT%d" % i, [2, L], BF16) for i in range(3)]
        pTo = [ar.alloc("pTo%d" % i, [3 * P], BF16) for i in range(2)]
        accs = [ar.alloc("acc%d" % i, [2, VW], F32) for i in range(2)]
        tmps = [ar.alloc("tmpT%d" % i, [2, VW], F32) for i in range(3)]
        rcs = [ar.alloc("rc%d" % i, [2], F32) for i in range(2)]
        obf = [ar.alloc("obf%d" % i, [2, DH], BF16) for i in range(2)]
        ast = [ar.alloc("ast%d" % i, [L], BF16) for i in range(2)]
        for i in range(2):
            pr.op("pool", lambda e, i=i: e.memset(Vh[i][0][:, :, DH:VW], 1.0), writes=[Vh[i][1]])
            pr.op("pool", lambda e, i=i: e.memset(Voh[i][0][:, :, DH:VW], 1.0), writes=[Voh[i][1]])
        SBK = (0, 1, 2)
        OBK = (3, 4, 5)
        TBK = 6

        def T_load(h):
            i = h % 2
            pr.dma("sp", KTh[i][0], KT_d[h], "KTh%d" % i, reads=[B_KT], writes=[KTh[i][1]])
            ch = float(np.exp(-128.0 * alibi_slopes()[h]))
            vev = Vh[i][0].rearrange("p (a two) w -> p a two w", two=2)[:, :, 0, DH:VW]
            pr.op("pool", lambda e, vev=vev, ch=ch: e.memset(vev, ch), writes=[Vh[i][1]])
            vsrc = V_d[:, h * DH:(h + 1) * DH].rearrange("(t p) d -> p t d", p=P)
            for q4 in range(4):
                pr.dma("sp", Vh[i][0][:, q4 * 16:(q4 + 1) * 16, 0:DH], vsrc[:, q4 * 16:(q4 + 1) * 16, :], "Vh%d_%d" % (i, q4),
                       reads=[B_V], writes=[Vh[i][1]])
            pr.dma("sp", KToh[i][0], KTo_d[h], "KToh%d" % i, reads=[B_KTo], writes=[KToh[i][1]])
            pr.dma("sp", Voh[i][0][:, :, 0:DH], Vo_d[:, h * DH:(h + 1) * DH].rearrange("(t p) d -> p t d", p=P),
                   "Voh%d" % i, reads=[B_Vo], writes=[Voh[i][1]])
            pr.dma("sp", QTh[i][0], QT_d[h], "QTh%d" % i, reads=[B_QT], writes=[QTh[i][1]])
            pr.dma("sp", Fh[i][0], F_d[h].rearrange("p (q n) -> p q n", q=16, n=NB), "Fh%d" % i, reads=[B_F],
                   writes=[Fh[i][1]])

        uctr = [0]

        def T_head(h):
            i = h % 2
            kth, B_kth = KTh[i]
            vh, B_vh = Vh[i]
            kto, B_kto = KToh[i]
            voh, B_voh = Voh[i]
            qth, B_qth = QTh[i]
            fh, B_fh = Fh[i]
            units = []
            for s in range(NSLOT):
                units.append((s, -1))
                for n in range(PAST[s]):
                    units.append((s, n))
            state = {}
            deferred = []

            def emit_qk(idx):
                s, n = units[idx]
                u = uctr[0]
                uctr[0] += 1
                sb = SBK[u % 3]
                qsl = slice(s * L, (s + 1) * L)
                if n >= 0:
                    sv = ps_f32(sb).rearrange("p (t q) -> p t q", t=2, q=L)

                    def qk(e, sv=sv, n=n, qsl=qsl):
                        e.matmul(sv[:, 0, :], lhsT=kth[:, (2 * n) * P:(2 * n + 1) * P], rhs=qth[:, qsl], start=True, stop=True)
                        return e.matmul(sv[:, 1, :], lhsT=kth[:, (2 * n + 1) * P:(2 * n + 2) * P], rhs=qth[:, qsl],
                                        start=True, stop=True)
                    pr.op("pe", qk, reads=[B_kth, B_qth], writes=[pbuf[sb]])
                    pt, B_pt = pTs[u % 3]
                    pr.op("act", lambda e, pt=pt, sv=sv: e.activation(
                        out=pt, in_=sv, func=AF.Exp, bias=bkt[:, h, 1:2], scale=SCALE),
                        reads=[pbuf[sb], B_bkt], writes=[B_pt])
                    state[idx] = (u, pt, B_pt)
                else:
                    sv = ps_f32(sb, 3 * P)
                    q0 = s * L

                    def qk(e, sv=sv, q0=q0):
                        e.matmul(sv[:, 0:P], lhsT=kto[:, q0:q0 + P], rhs=qth[:, q0:q0 + P], start=True, stop=True)
                        e.matmul(sv[:, P:2 * P], lhsT=kto[:, q0 + P:q0 + 2 * P], rhs=qth[:, q0 + P:q0 + 2 * P],
                                 start=True, stop=True)
                        return e.matmul(sv[:, 2 * P:3 * P], lhsT=kto[:, q0:q0 + P], rhs=qth[:, q0 + P:q0 + 2 * P],
                                        start=True, stop=True)
                    pr.op("pe", qk, reads=[B_kto, B_qth], writes=[pbuf[sb]])
                    pt, B_pt = pTo[s % 2]
                    pr.op("act", lambda e, pt=pt, sv=sv: e.activation(
                        out=pt[:, 0:2 * P], in_=sv[:, 0:2 * P], func=AF.Exp, bias=bkt[:, h, 1:2], scale=SCALE),
                        reads=[pbuf[sb], B_bkt], writes=[B_pt])
                    pr.op("act", lambda e, pt=pt, sv=sv: e.activation(
                        out=pt[:, 2 * P:3 * P], in_=sv[:, 2 * P:3 * P], func=AF.Exp, bias=bkt[:, h, 0:1], scale=SCALE),
                        reads=[pbuf[sb], B_bkt], writes=[B_pt])
                    for a in range(2):
                        pr.op("pool", lambda e, pt=pt, a=a: e.tensor_tensor(
                            out=pt[:, a * P:(a + 1) * P], in0=pt[:, a * P:(a + 1) * P], in1=tri, op=ALU.mult),
                            reads=[B_pt, B_tri], writes=[B_pt])
                    state[idx] = (u, pt, B_pt)

            def emit_pv(idx):
                s, n = units[idx]
                u, pt, B_pt = state.pop(idx)
                ob = OBK[u % 3]
                ov = ps_f32(ob, 2 * VW).rearrange("p (t d) -> p t d", t=2, d=VW)
                acc, B_acc = accs[s % 2]
                NV = DH + 1
                if n >= 0:
                    def pv(e, ov=ov, pt=pt, n=n):
                        ins = None
                        for qt in range(2):
                            for t in range(2):
                                ins = e.matmul(ov[:, qt, 0:NV], lhsT=pt[:, t, qt * P:(qt + 1) * P],
                                               rhs=vh[:, 2 * n + t, 0:NV], start=(t == 0), stop=(t == 1))
                        return ins
                    pr.op("pe", pv, reads=[B_pt, B_vh], writes=[pbuf[ob]])
                    for qt in range(2):
                        pr.op("dve", lambda e, ov=ov, acc=acc, qt=qt, n=n, s=s: e.scalar_tensor_tensor(
                            out=acc[:, qt, 0:NV], in0=ov[:, qt, 0:NV], scalar=fh[:, 2 * s + qt, n:n + 1],
                            in1=acc[:, qt, 0:NV], op0=ALU.mult, op1=ALU.add),
                            reads=[pbuf[ob], B_fh, B_acc], writes=[B_acc])
                else:
                    def pv(e, ov=ov, pt=pt, s=s):
                        e.matmul(ov[:, 0, 0:NV], lhsT=pt[:, 0:P], rhs=voh[:, 2 * s, 0:NV], start=True, stop=True)
                        e.matmul(ov[:, 1, 0:NV], lhsT=pt[:, 2 * P:3 * P], rhs=voh[:, 2 * s, 0:NV], start=True, stop=False)
                        return e.matmul(ov[:, 1, 0:NV], lhsT=pt[:, P:2 * P], rhs=voh[:, 2 * s + 1, 0:NV],
                                        start=False, stop=True)
                    pr.op("pe", pv, reads=[B_pt, B_voh], writes=[pbuf[ob]])
                    pr.op("dve", lambda e, ov=ov, acc=acc: e.tensor_scalar(
                        out=acc[:, :, 0:NV], in0=ov[:, :, 0:NV], scalar1=fown[:, h:h + 1], scalar2=None,
                        op0=ALU.mult), reads=[pbuf[ob], B_fown], writes=[B_acc])
                last = (idx + 1 == len(units)) or (units[idx + 1][0] != s)
                if last:
                    deferred.append([2, s])

            def finalize(s):
                acc, B_acc = accs[s % 2]
                rc, B_rc = rcs[s % 2]
                ob_, B_ob = obf[s % 2]
                asv, B_as = ast[s % 2]
                pr.op("dve", lambda e: e.reciprocal(out=rc, in_=acc[:, :, DH]), reads=[B_acc], writes=[B_rc])
                for qt in range(2):
                    pr.op("dve", lambda e, qt=qt: e.tensor_scalar(out=ob_[:, qt, :], in0=acc[:, qt, 0:DH],
                                                                  scalar1=rc[:, qt:qt + 1], scalar2=None, op0=ALU.mult),
                          reads=[B_acc, B_rc], writes=[B_ob])
                ptv = ps_bf16(TBK)[:, 0:L].rearrange("p (t q) -> p t q", t=2, q=P)

                def tr(e):
                    e.transpose(out=ptv[:, 0, :], in_=ob_[:, 0, :], identity=ident)
                    return e.transpose(out=ptv[:, 1, :], in_=ob_[:, 1, :], identity=ident)
                pr.op("pe", tr, reads=[B_ob, B_ident], writes=[pbuf[TBK]])
                pr.op("act", lambda e: e.copy(out=asv, in_=ps_bf16(TBK)[:, 0:L]), reads=[pbuf[TBK]], writes=[B_as])
                pr.dma("pool", mixT_d[h * DH:(h + 1) * DH, s * L:(s + 1) * L], asv, "ast%d" % (s % 2), reads=[B_as],
                       writes=[B_mixT])

            def tick():
                for dd in list(deferred):
                    dd[0] -= 1
                    if dd[0] <= 0:
                        deferred.remove(dd)
                        finalize(dd[1])

            emit_qk(0)
            emit_qk(1)
            for idx in range(len(units)):
                if idx + 2 < len(units):
                    emit_qk(idx + 2)
                emit_pv(idx)
                tick()
            for dd in list(deferred):
                finalize(dd[1])
            deferred.clear()

        T_load(0)
        for h in range(H):
            if h + 1 < H:
                T_load(h + 1)
            bg_issue(4, [B_mixT])
            T_head(h)
        bg_issue(100, [B_mixT])
        ar.release(mT)
        if stop_after == "T":
            return _finish(nc, pr, st, ar, [B_mixT])

        gb_t = []
        for gi_, nm in ((1, "gpost"), (2, "gffn"), (3, "gfpost")):
            t_, b_ = ar.alloc(nm, [D], F32)
            pr.dma("sp", t_, grow_d[gi_:gi_ + 1, :].partition_broadcast(P), "c7_%d" % gi_, writes=[b_])
            gb_t.append((t_, b_))
        (gpost_b, B_gpost), (gffn_b, B_gffn), (gfpost_b, B_gfpost) = gb_t
        mixS, B_mixS = ar.alloc("mixS", [KC, 512], BF16)
        fT, B_fT = ar.alloc("fT", [KC, 512], BF16)
        accF, _ = ar.alloc("accF", [4, D], F32)
        B_accF = [Buf("accF%d" % t) for t in range(4)]
        hTs = [ar.alloc("hT%d" % i, [4, 512], BF16) for i in range(2)]
        rts = [ar.alloc("rt%d" % i, [512], F32) for i in range(2)]
        NWS2 = 4
        wc2 = [ar.alloc("wc2_%d" % i, [KC, 512], BF16) for i in range(NWS2)]
        xsC = [ar.alloc("xsC%d" % i, [D], F32) for i in range(2)]
        abC = [ar.alloc("abC%d" % i, [D], BF16) for i in range(2)]
        w2ctr = [0]

        def load_w2(src_ap, reads):
            i = w2ctr[0] % NWS2
            w2ctr[0] += 1
            w, B_w = wc2[i]
            pr.dma("sp", w, src_ap, "wch%d" % i, reads=reads, writes=[B_w])
            return w, B_w
        B_y = [Buf("y%d" % t) for t in range(NOWN // P)]
        CB = (2, 3)
        HBK = (4, 5)
        cb_ctr = [0]
        xc_ctr = [0]

        def rstd_of(src, B_src, junk, B_junk):
            i = stat_ctr[0] % 8
            stat_ctr[0] += 1
            ssv = ss_t[:, i:i + 1]
            rsv = rs_t[:, i:i + 1]
            pr.op("act", lambda e: e.activation(out=junk, in_=src, func=AF.Square, accum_out=ssv),
                  reads=[B_src], writes=[B_ss[i], B_junk])
            pr.op("act", lambda e: e.activation(out=rsv, in_=ssv, func=AF.Sqrt, bias=RMS_EPS, scale=1.0 / D),
                  reads=[B_ss[i]], writes=[B_rs[i]])
            pr.op("dve", lambda e: e.reciprocal(out=rsv, in_=rsv), reads=[B_rs[i]], writes=[B_rs[i]])
            return rsv, B_rs[i]

        for gi in range(4):
            pr.dma("sp", mixS, mixT_d[:, gi * 512:(gi + 1) * 512].rearrange("(k p) t -> p k t", p=P), "mixS",
                   reads=[B_mixT], writes=[B_mixS])
            nxt = load_w2(w_out_bf[:, 0:512].rearrange("(k p) n -> p k n", p=P), issued(B_wout))
            for n in range(4):
                w, B_w = nxt
                if n + 1 < 4:
                    nxt = load_w2(w_out_bf[:, (n + 1) * 512:(n + 2) * 512].rearrange("(k p) n -> p k n", p=P), B_wout)
                else:
                    nxtW1 = [load_w2(w_ff1_bf[:, 0:512].rearrange("(k p) n -> p k n", p=P), B_wff1[0])]
                for t in range(4):
                    bank = CB[cb_ctr[0] % 2]
                    cb_ctr[0] += 1
                    po = ps_f32(bank)
                    mm_group(po, [(mixS[:, k, t * P:(t + 1) * P], w[:, k, :]) for k in range(KC)],
                             reads=[B_mixS, B_w], writes=[pbuf[bank]])
                    pr.op("act", lambda e, po=po, t=t, n=n: e.copy(out=accF[:, t, n * 512:(n + 1) * 512], in_=po),
                          reads=[pbuf[bank]], writes=[B_accF[t]])
            for t in range(4):
                i = xc_ctr[0] % 2
                xc_ctr[0] += 1
                xs, B_xs = xsC[i]
                ab, B_ab = abC[i]
                s_ = 2 * gi + t // 2
                r0 = s_ * SLOTW + HALO + (t % 2) * P
                pr.dma("sp", xs, xown_d[r0:r0 + P, :], "xsA%d" % i, writes=[B_xs])
                rsv, B_r = rstd_of(accF[:, t, :], B_accF[t], ab, B_ab)
                pr.op("dve", lambda e, t=t, rsv=rsv: e.scalar_tensor_tensor(
                    out=accF[:, t, :], in0=accF[:, t, :], scalar=rsv, in1=gpost_b, op0=ALU.mult, op1=ALU.mult),
                    reads=[B_accF[t], B_r, B_gpost], writes=[B_accF[t]])
                pr.op("dve", lambda e, t=t, xs=xs: e.tensor_tensor(out=xs, in0=accF[:, t, :], in1=xs, op=ALU.add),
                      reads=[B_accF[t], B_xs], writes=[B_xs])
                yt = gi * 4 + t
                pr.dma("pool", y_d[yt * P:(yt + 1) * P, :], xs, "xsSt%d" % i, reads=[B_xs], writes=[B_y[yt]])
                norm_T_tile(None, xs, B_xs, None, gffn_b, B_gffn, ab, B_ab, fT, B_fT, t * P, TB)
            NFC = DFF // 512

            def w1src(c):
                return w_ff1_bf[:, c * 512:(c + 1) * 512].rearrange("(k p) n -> p k n", p=P), B_wff1[c // 4]

            def w2src(c):
                return w_ff2_bf[c * 512:(c + 1) * 512, :].rearrange("(j p) n -> p j n", p=P), B_wff2[c // 4]
            W1 = {0: nxtW1[0]}
            W2 = {}
            W1[1] = load_w2(*w1src(1))
            W2[0] = load_w2(*w2src(0))

            def ffn_H(c):
                w, B_w = W1.pop(c)
                hT, B_hT = hTs[c % 2]
                for j in range(4):
                    bank = HBK[j % 2]
                    ph = ps_f32(bank)
                    mm_group(ph, [(w[:, k, j * P:(j + 1) * P], fT[:, k, :]) for k in range(KC)],
                             reads=[B_w, B_fT], writes=[pbuf[bank]])
                    rt, B_rt = rts[j % 2]
                    pr.op("act", lambda e, rt=rt, ph=ph: e.activation(out=rt, in_=ph, func=AF.Relu),
                          reads=[pbuf[bank]], writes=[B_rt])
                    pr.op("act", lambda e, rt=rt, hT=hT, j=j: e.activation(out=hT[:, j, :], in_=rt, func=AF.Square),
                          reads=[B_rt], writes=[B_hT])

            def ffn_O(c):
                w, B_w = W2.pop(c)
                wv_ = w.rearrange("p k n -> p (k n)").rearrange("p (j n) -> p j n", j=4, n=D)
                hT, B_hT = hTs[c % 2]
                for t in range(4):
                    for n in range(4):
                        bank = CB[cb_ctr[0] % 2]
                        cb_ctr[0] += 1
                        po = ps_f32(bank)
                        mm_group(po, [(hT[:, j, t * P:(t + 1) * P], wv_[:, j, n * 512:(n + 1) * 512]) for j in range(4)],
                                 reads=[B_hT, B_w], writes=[pbuf[bank]])
                        dst = accF[:, t, n * 512:(n + 1) * 512]
                        if c == 0:
                            pr.op("dve", lambda e, dst=dst, po=po: e.tensor_copy(out=dst, in_=po),
                                  reads=[pbuf[bank]], writes=[B_accF[t]])
                        else:
                            pr.op("dve", lambda e, dst=dst, po=po: e.tensor_tensor(out=dst, in0=po, in1=dst, op=ALU.add),
                                  reads=[pbuf[bank], B_accF[t]], writes=[B_accF[t]])
            ffn_H(0)
            for c in range(NFC):
                if c + 2 < NFC:
                    W1[c + 2] = load_w2(*w1src(c + 2))
                if c + 1 < NFC:
                    W2[c + 1] = load_w2(*w2src(c + 1))
                    ffn_H(c + 1)
                ffn_O(c)
            for t in range(4):
                i = xc_ctr[0] % 2
                xc_ctr[0] += 1
                xs, B_xs = xsC[i]
                ab, B_ab = abC[i]
                yt = gi * 4 + t
                pr.dma("sp", xs, y_d[yt * P:(yt + 1) * P, :], "xsA%d" % i, reads=[B_y[yt]], writes=[B_xs])
                rsv, B_r = rstd_of(accF[:, t, :], B_accF[t], ab, B_ab)
                pr.op("dve", lambda e, t=t, rsv=rsv: e.scalar_tensor_tensor(
                    out=accF[:, t, :], in0=accF[:, t, :], scalar=rsv, in1=gfpost_b, op0=ALU.mult, op1=ALU.mult),
                    reads=[B_accF[t], B_r, B_gfpost], writes=[B_accF[t]])
                pr.op("dve", lambda e, t=t, xs=xs: e.tensor_tensor(out=xs, in0=accF[:, t, :], in1=xs, op=ALU.add),
                      reads=[B_accF[t], B_xs], writes=[B_xs])
                o = pr.dma("pool", y_d[yt * P:(yt + 1) * P, :], xs, "xsSt%d" % i, reads=[B_xs], writes=[B_y[yt]])
                pr.must_finish(o)
        return _finish(nc, pr, st, ar, [])


def _finish(nc, pr, st, ar, bufs):
    for b in bufs:
        if b.writer is not None:
            pr.must_finish(b.writer)
    pr.emit_all(st)
    nc._mk_info = dict(n_sems=pr.n_sems, peak=ar.peak, nops=len(pr.all_ops), counts=pr.max_count)
    return nc


def alibi_slopes():
    return (2.0 ** (-8.0 * np.arange(1, H + 1) / H)).astype(np.float64)


def core_tables(j):
    blks = own_blocks(j)
    sl = alibi_slopes()
    fd = np.zeros((NSLOT, P, 2, H, NB), np.float64)
    gb = np.full((P, NSLOT, NB), -1e30, np.float32)
    hm = np.ones((P, NSLOT), np.float32)
    q = np.arange(P)
    for s, blk in enumerate(blks):
        if blk == 0:
            hm[:, s] = 0.0
        for n in range(blk):
            gb[:, s, n] = 0.0
            for qt in range(2):
                dist = 256 * (n - blk) + 255 - (128 * qt + q)
                fd[s, :, qt, :, n] = np.exp(sl[None, :] * dist[:, None])
    return (fd.reshape(NSLOT, P, 2 * H * NB).astype(np.float32), gb.reshape(P, NSLOT * NB), hm)


def const_tables():
    sl = alibi_slopes()
    p = np.arange(P)
    bkt = np.zeros((P, H, 2), np.float64)
    for t in range(2):
        bkt[:, :, t] = sl[None, :] * (t * 128 + p[:, None] - 255)
    fown = np.exp(sl[None, :] * (127 - p[:, None]))
    tri = (p[None, :] >= p[:, None]).astype(np.float32)
    cvec = np.repeat(np.exp(-128.0 * sl), DH)[None, :].repeat(P, 0)
    return (bkt.reshape(P, 2 * H).astype(np.float32), fown.astype(np.float32),
            tri.astype(ml_dtypes.bfloat16), np.eye(P, dtype=np.float32).astype(ml_dtypes.bfloat16),
            np.ascontiguousarray(cvec.astype(np.float32)))


def make_in_maps(inputs, cores=range(8)):
    x = np.asarray(inputs["x"], np.float32)
    f = lambda k: np.ascontiguousarray(np.asarray(inputs[k], np.float32)[0])
    w_in, w_out, w_ff1, w_ff2 = f("w_in"), f("w_out"), f("w_ff1"), f("w_ff2")
    grow = np.stack([f("g_mix_pre"), f("g_mix_post"), f("g_ffn_pre"), f("g_ffn_post")]).astype(np.float32)
    b_glu, w_dw, b_dw, ln_g, ln_b = f("b_glu"), f("w_dw"), f("b_dw"), f("ln_conv_g"), f("ln_conv_b")
    cols = np.concatenate([
        b_glu.reshape(16, P).T,
        w_dw.reshape(CW, CC, P).transpose(2, 1, 0).reshape(P, CC * CW),
        b_dw.reshape(CC, P).T, ln_g.reshape(CC, P).T, ln_b.reshape(CC, P).T], axis=1).astype(np.float32)
    bkt, fown, tri, ident, cvec = const_tables()
    maps = []
    for c in cores:
        bi, j = c // 4, c % 4
        fd, gb, hm = core_tables(j)
        xo = np.zeros((NSLOT, SLOTW, D), np.float32)
        for s, blk in enumerate(own_blocks(j)):
            lo = L * blk - HALO
            if lo < 0:
                xo[s, HALO:] = x[bi, 0:L]
            else:
                xo[s] = x[bi, lo:lo + SLOTW]
        maps.append(dict(xall=np.ascontiguousarray(x[bi]), xown=xo.reshape(NOWNH, D), w_in=w_in, w_out=w_out,
                         w_ff1=w_ff1, w_ff2=w_ff2, grow=grow, cols=np.ascontiguousarray(cols), fd=fd, gbias=gb,
                         hmask=hm, bkt=bkt, fown=fown, tri=tri, ident=ident, cvec=cvec))
    return maps


_NC = None


def kernel(**inputs):
    global _NC
    if _NC is None:
        _NC = build_nc()
    maps = make_in_maps(inputs)
    res = run_bass_kernel_spmd(_NC, maps, core_ids=list(range(8)))
    x = np.asarray(inputs["x"])
    out = np.zeros(x.shape, np.float32)
    for c in range(8):
        bi, j = c // 4, c % 4
        y = np.asarray(res.results[c]["y"])
        for s, blk in enumerate(own_blocks(j)):
            out[bi, L * blk:L * (blk + 1)] = y[s * L:(s + 1) * L]
    return out
```

```python
import contextlib
import numpy as np
import ml_dtypes
import concourse.bass as bass
import concourse.mybir as mybir
from concourse.bass_utils import run_bass_kernel_spmd

F32 = mybir.dt.float32
BF16 = mybir.dt.bfloat16
ALU = mybir.AluOpType
AF = mybir.ActivationFunctionType
AX = mybir.AxisListType

P = 128
D = 2048
KC = 16
S = 8192
NB = 32
L = 256
H = 8
DH = 128
C = 1024
CC = 8
DFF = 8192
INC = 5120
NSLOT = 8
HALO = 32
SLOTW = L + HALO
NOWN = NSLOT * L
NOWNH = NSLOT * SLOTW
SCALE = DH ** -0.5
RMS_EPS = 1e-6
LN_EPS = 1e-5
CW = 31

STREAMS = ("pe", "act", "dve", "pool", "sp")


class Buf:
    __slots__ = ("name", "writer", "readers", "inherit", "excl")

    def __init__(self, name, inherit=(), excl=False):
        self.name = name
        self.excl = excl
        self.writer = None
        self.readers = []
        self.inherit = list(inherit)


class Op:
    __slots__ = ("stream", "emit", "deps", "is_dma", "semkey", "signal", "name")

    def __init__(self, stream, emit, is_dma=False, semkey=None, name=""):
        self.stream = stream
        self.emit = emit
        self.deps = []
        self.is_dma = is_dma
        self.semkey = semkey
        self.signal = False
        self.name = name


class Prog:
    def __init__(self, nc):
        self.nc = nc
        self.ops = {s: [] for s in STREAMS}
        self.all_ops = []
        self.final_waits = []

    def _add(self, op, reads, writes, after=()):
        deps = []
        for b in after:
            if b.writer is not None:
                deps.append(b.writer)
        for b in reads:
            if b.writer is not None:
                deps.append(b.writer)
            elif b.inherit:
                deps.extend(b.inherit)
            if b.excl:
                deps.extend(r for r in b.readers if r.stream != op.stream)
        for b in writes:
            if b.writer is not None:
                deps.append(b.writer)
            deps.extend(b.readers)
            if b.inherit:
                deps.extend(b.inherit)
                b.inherit = []
        seen = set()
        for d in deps:
            if d is op or id(d) in seen:
                continue
            seen.add(id(d))
            op.deps.append(d)
        for b in reads:
            b.readers.append(op)
        for b in writes:
            b.writer = op
            b.readers = []
        self.ops[op.stream].append(op)
        self.all_ops.append(op)
        return op

    def op(self, stream, emit, reads=(), writes=(), name=""):
        return self._add(Op(stream, emit, name=name), reads, writes)

    def dma(self, stream, out, in_, semkey, reads=(), writes=(), name="", after=()):
        def emit(eng):
            return eng.dma_start(out=out, in_=in_)
        return self._add(Op(stream, emit, is_dma=True, semkey=semkey, name=name), reads, writes, after)

    def must_finish(self, op):
        self.final_waits.append(op)

    def emit_all(self, stack):
        nc = self.nc
        for op in self.all_ops:
            for d in op.deps:
                d.signal = True
        for op in self.final_waits:
            op.signal = True
        eng_sem = {s: stack.enter_context(nc.semaphore("done_" + s)) for s in STREAMS}
        dma_sems = {}
        dma_cnt = {}
        cnt = {s: 0 for s in STREAMS}
        comp = {}
        for op in self.all_ops:
            s = op.stream
            if op.is_dma:
                k = op.semkey
                if k not in dma_sems:
                    dma_sems[k] = stack.enter_context(nc.semaphore("dq_%d" % len(dma_sems)))
                    dma_cnt[k] = 0
                dma_cnt[k] += 16
                comp[id(op)] = (dma_sems[k], dma_cnt[k])
            elif op.signal:
                cnt[s] += 1
                comp[id(op)] = (eng_sem[s], cnt[s])
        self.n_sems = len(dma_sems) + len(STREAMS)
        self.max_count = dict(cnt)
        block = stack.enter_context(nc.Block())
        prog = self

        def run_stream(s, eng):
            waited = {}
            for op in prog.ops[s]:
                need = {}
                for d in op.deps:
                    sem, val = comp[id(d)]
                    key = id(sem)
                    if waited.get(key, 0) >= val:
                        continue
                    if key not in need or need[key][1] < val:
                        need[key] = (sem, val)
                for key, (sem, val) in need.items():
                    waited[key] = val
                    eng.wait_ge(sem, val)
                ins = op.emit(eng)
                if op.is_dma:
                    ins.then_inc(comp[id(op)][0], 16)
                elif op.signal:
                    ins.then_inc(eng_sem[s], 1)
            if s == "sp":
                for op in prog.final_waits:
                    sem, val = comp[id(op)]
                    eng.wait_ge(sem, val)

        @block.tensor
        def _(e):
            run_stream("pe", e)

        @block.scalar
        def _(e):
            run_stream("act", e)

        @block.vector
        def _(e):
            run_stream("dve", e)

        @block.gpsimd
        def _(e):
            run_stream("pool", e)

        @block.sync
        def _(e):
            run_stream("sp", e)


DT_SIZE = {F32: 4, BF16: 2}


class Arena:
    def __init__(self, nc, stack, kib):
        self.words = kib * 256
        self.t = stack.enter_context(nc.sbuf_tensor("arena", [P, self.words], F32))
        self.top = 0
        self.peak = 0
        self.retired = []
        self.live = []

    def alloc(self, name, shape, dtype):
        n = 1
        for s in shape:
            n *= s
        nbytes = (n * DT_SIZE[dtype] + 31) // 32 * 32
        lo = self.top
        hi = lo + nbytes
        assert hi <= self.words * 4, "SBUF arena overflow at %s: %d > %d" % (name, hi, self.words * 4)
        self.top = hi
        self.peak = max(self.peak, hi)
        inh = []
        keep = []
        for (l, h, ops) in self.retired:
            if l < hi and h > lo:
                inh.extend(ops)
                if l >= lo and h <= hi:
                    continue
            keep.append((l, h, ops))
        self.retired = keep
        buf = Buf(name, inherit=inh)
        v = self.t[:, lo // 4:hi // 4]
        if dtype != F32:
            v = v.bitcast(dtype)
        v = v[:, 0:n]
        if len(shape) == 2:
            v = v.rearrange("p (a b) -> p a b", a=shape[0], b=shape[1])
        elif len(shape) == 3:
            v = v.rearrange("p (a b c) -> p a b c", a=shape[0], b=shape[1], c=shape[2])
        elif len(shape) == 4:
            v = v.rearrange("p (a b c d) -> p a b c d", a=shape[0], b=shape[1], c=shape[2], d=shape[3])
        self.live.append((lo, hi, buf))
        return v, buf

    def mark(self):
        return self.top

    def release(self, mark):
        keep = []
        for (lo, hi, buf) in self.live:
            if lo >= mark:
                ops = list(buf.readers) + list(buf.inherit)
                if buf.writer is not None:
                    ops.append(buf.writer)
                if ops:
                    self.retired.append((lo, hi, ops))
            else:
                keep.append((lo, hi, buf))
        self.live = keep
        self.top = mark


def own_blocks(j):
    return [8 * (s // 2) + (j if s % 2 == 0 else 7 - j) for s in range(NSLOT)]


PAST = [8 * (s // 2) + (3 if s % 2 == 0 else 7) for s in range(NSLOT)]


def build_nc(debug=False, stop_after="all"):
    nc = bass.Bass("TRN2", target_bir_lowering=False)
    skind = "ExternalOutput" if debug else "Internal"

    def din(name, shape, dt=F32):
        return nc.dram_tensor(name, list(shape), dt, kind="ExternalInput").ap()

    def dscr(name, shape, dt):
        return nc.dram_tensor(name, list(shape), dt, kind=skind).ap()

    xall_d = din("xall", [S, D])
    xown_d = din("xown", [NOWNH, D])
    w_in_d = din("w_in", [D, INC])
    w_out_d = din("w_out", [D, D])
    w_ff1_d = din("w_ff1", [D, DFF])
    w_ff2_d = din("w_ff2", [DFF, D])
    grow_d = din("grow", [4, D])
    cols_d = din("cols", [P, 16 + CC * CW + 3 * CC])
    fd_d = din("fd", [NSLOT, P, 2 * H * NB])
    gbias_d = din("gbias", [P, NSLOT * NB])
    hmask_d = din("hmask", [P, NSLOT])
    bkt_d = din("bkt", [P, H * 2])
    fown_d = din("fown", [P, H])
    cvec_d = din("cvec", [P, H * DH])
    tri_d = din("tri", [P, P], BF16)
    ident_d = din("ident", [P, P], BF16)

    y_d = nc.dram_tensor("y", [NOWN, D], F32, kind="ExternalOutput").ap()

    w_in_bf = dscr("w_in_bf", [D, INC], BF16)
    w_out_bf = dscr("w_out_bf", [D, D], BF16)
    w_ff1_bf = dscr("w_ff1_bf", [D, DFF], BF16)
    w_ff2_bf = dscr("w_ff2_bf", [DFF, D], BF16)
    KT_d = dscr("KT", [H, DH, S], BF16)
    V_d = dscr("V", [S, H * DH], BF16)
    KTo_d = dscr("KTo", [H, DH, NOWN], BF16)
    Vo_d = dscr("Vo", [NOWN, H * DH], BF16)
    QT_d = dscr("QT", [H, DH, NOWN], BF16)
    F_d = dscr("Fsel", [H, P, 16 * NB], F32)
    yT_d = dscr("yT", [CC, P, NOWN], F32)
    mixT_d = dscr("mixT", [D, NOWN], BF16)
    kmean_dbg = dscr("kmean_dbg", [P, H * NB], F32) if debug else None

    with contextlib.ExitStack() as st:
        pr = Prog(nc)
        ar = Arena(nc, st, 205)
        psum = []
        pbuf = []
        for i in range(8):
            psum.append(st.enter_context(nc.psum_tensor("ps%d" % i, [P, 512], F32)))
            pbuf.append(Buf("ps%d" % i, excl=True))

        def ps_f32(i, n=512):
            return psum[i][:, 0:n]

        def ps_bf16(i):
            return psum[i][:, :].bitcast(BF16)

        cols, B_cols = ar.alloc("cols", [16 + CC * CW + 3 * CC], F32)
        pr.dma("sp", cols, cols_d, "c0", writes=[B_cols])
        bglu = cols[:, 0:16]
        wdw = cols[:, 16:16 + CC * CW].rearrange("p (c j) -> p c j", c=CC, j=CW)
        o0 = 16 + CC * CW
        bdw = cols[:, o0:o0 + CC]
        lng = cols[:, o0 + CC:o0 + 2 * CC]
        lnb = cols[:, o0 + 2 * CC:o0 + 3 * CC]
        ident, B_ident = ar.alloc("ident", [P], BF16)
        pr.dma("sp", ident, ident_d, "c1", writes=[B_ident])
        tri, B_tri = ar.alloc("tri", [P], BF16)
        pr.dma("sp", tri, tri_d, "c2", writes=[B_tri])
        bkt, B_bkt = ar.alloc("bkt", [H, 2], F32)
        pr.dma("sp", bkt, bkt_d.rearrange("p (h t) -> p h t", h=H, t=2), "c3", writes=[B_bkt])
        fown, B_fown = ar.alloc("fown", [H], F32)
        pr.dma("sp", fown, fown_d, "c4", writes=[B_fown])
        gbias, B_gbias = ar.alloc("gbias", [NSLOT, NB], F32)
        pr.dma("sp", gbias, gbias_d.rearrange("p (s n) -> p s n", s=NSLOT, n=NB), "c5", writes=[B_gbias])
        hmask, B_hmask = ar.alloc("hmask", [NSLOT], F32)
        pr.dma("sp", hmask, hmask_d, "c6", writes=[B_hmask])
        cvec, B_cvec = ar.alloc("cvec", [H * DH], F32)
        pr.dma("sp", cvec, cvec_d, "c8", writes=[B_cvec])
        kmean, B_kmean = ar.alloc("kmean", [H, NB], F32)
        ones32, B_ones = ar.alloc("ones32", [P], F32)
        pr.op("pool", lambda e: e.memset(ones32, 1.0), writes=[B_ones])
        ss_t, _ = ar.alloc("ss", [8], F32)
        rs_t, _ = ar.alloc("rs", [8], F32)
        B_ss = [Buf("ss%d" % i) for i in range(8)]
        B_rs = [Buf("rs%d" % i) for i in range(8)]
        stat_ctr = [0]

        def in_col_bufs(c0):
            if c0 < 1024:
                return B_wq
            if c0 < 3072:
                return B_wkv
            return B_wu

        def norm_T_tile(x_src, xs, B_xs, xs_key, g_b, B_gb, abf, B_abf, dstT, B_dstT, col0, tbanks):
            norm_tile(x_src, xs, B_xs, xs_key, g_b, B_gb, abf, B_abf)
            T_tile(abf, B_abf, dstT, B_dstT, col0, tbanks)

        def norm_tile(x_src, xs, B_xs, xs_key, g_b, B_gb, abf, B_abf):
            i = stat_ctr[0] % 8
            stat_ctr[0] += 1
            ssv = ss_t[:, i:i + 1]
            rsv = rs_t[:, i:i + 1]
            if x_src is not None:
                pr.dma("sp", xs, x_src, xs_key, writes=[B_xs])
            pr.op("act", lambda e: e.activation(out=abf, in_=xs, func=AF.Square, accum_out=ssv),
                  reads=[B_xs], writes=[B_ss[i], B_abf])
            pr.op("act", lambda e: e.activation(out=rsv, in_=ssv, func=AF.Sqrt, bias=RMS_EPS, scale=1.0 / D),
                  reads=[B_ss[i]], writes=[B_rs[i]])
            pr.op("dve", lambda e: e.reciprocal(out=rsv, in_=rsv), reads=[B_rs[i]], writes=[B_rs[i]])
            pr.op("dve", lambda e: e.scalar_tensor_tensor(out=abf, in0=xs, scalar=rsv, in1=g_b,
                                                          op0=ALU.mult, op1=ALU.mult),
                  reads=[B_xs, B_rs[i], B_gb], writes=[B_abf])

        def T_tile(abf, B_abf, dstT, B_dstT, col0, tbanks):
            for half in range(2):
                bank = tbanks[half]
                pt = ps_bf16(bank).rearrange("p (k n) -> p k n", k=8, n=P)

                def tr(e, half=half, pt=pt):
                    ins = None
                    for kk in range(8):
                        k = half * 8 + kk
                        ins = e.transpose(out=pt[:, kk, :], in_=abf[:, k * P:(k + 1) * P], identity=ident)
                    return ins
                pr.op("pe", tr, reads=[B_abf, B_ident], writes=[pbuf[bank]])
                dst = dstT[:, half * 8:(half + 1) * 8, col0:col0 + P]
                if half == 0:
                    pr.op("act", lambda e, dst=dst, pt=pt: e.copy(out=dst, in_=pt), reads=[pbuf[bank]], writes=[B_dstT])
                else:
                    pr.op("dve", lambda e, dst=dst, pt=pt: e.tensor_copy(out=dst, in_=pt), reads=[pbuf[bank]],
                          writes=[B_dstT])

        def mm_group(out_ap, pairs, reads, writes, name=""):
            def emit(e):
                ins = None
                n = len(pairs)
                for i, (l, r) in enumerate(pairs):
                    ins = e.matmul(out_ap, lhsT=l, rhs=r, start=(i == 0), stop=(i == n - 1))
                return ins
            return pr.op("pe", emit, reads=reads, writes=writes, name=name)

        mA = ar.mark()
        gpre_b, B_gpre = ar.alloc("gpre_b", [D], F32)
        pr.dma("sp", gpre_b, grow_d[0:1, :].partition_broadcast(P), "c7", writes=[B_gpre])
        wkv, _ = ar.alloc("wkv", [KC, 2048], BF16)
        B_wkvS = [Buf("wkvS%d" % i) for i in range(4)]
        wsrc = w_in_d[:, 1024:3072].rearrange("(k p) n -> p k n", p=P)
        for i in range(4):
            pr.dma("pool", wkv[:, 4 * i:4 * i + 4, :], wsrc[:, 4 * i:4 * i + 4, :], "wkvS%d" % i, writes=[B_wkvS[i]])
        bg_queue = []

        def cast_group(name, dst, src, pieces):
            bufs = []
            for i, (dsl, ssl) in enumerate(pieces):
                b = Buf("%s_%d" % (name, i))
                bg_queue.append((dst[dsl], src[ssl], "cast_" + name, b))
                bufs.append(b)
            return bufs

        def bg_issue(n, gates):
            for _ in range(n):
                if not bg_queue:
                    return
                d_, s_, k_, b_ = bg_queue.pop(0)
                pr.dma("pool", d_, s_, k_, writes=[b_], after=gates)

        def issued(bufs):
            assert all(b.writer is not None for b in bufs), "weight cast not issued before its consumer"
            return bufs

        def rows4(c0, c1, nrows=D):
            q = nrows // 4
            return [((slice(i * q, (i + 1) * q), slice(c0, c1)),) * 2 for i in range(4)]

        B_wkv = cast_group("wkv", w_in_bf, w_in_d, rows4(1024, 3072))
        B_wq = cast_group("wq", w_in_bf, w_in_d, rows4(0, 1024))
        B_wu = cast_group("wu", w_in_bf, w_in_d, rows4(3072, 5120))
        B_wout = cast_group("wout", w_out_bf, w_out_d, rows4(0, D))
        B_wff1 = []
        B_wff2 = []
        for g in range(4):
            B_wff1.append(cast_group("wff1_%d" % g, w_ff1_bf, w_ff1_d, rows4(g * 2048, (g + 1) * 2048)))
            pcs = [((slice(g * 2048 + i * 512, g * 2048 + (i + 1) * 512), slice(0, D)),) * 2 for i in range(4)]
            B_wff2.append(cast_group("wff2_%d" % g, w_ff2_bf, w_ff2_d, pcs))

        xsA = [ar.alloc("xsA%d" % i, [D], F32) for i in range(4)]
        abA = [ar.alloc("abA%d" % i, [D], BF16) for i in range(4)]
        aTA = [ar.alloc("aTA%d" % i, [KC, 512], BF16) for i in range(2)]
        ktst = [ar.alloc("ktst%d" % i, [H, 512], BF16) for i in range(2)]
        vst = [ar.alloc("vst%d" % i, [1024], BF16) for i in range(2)]
        kmsum, B_kmsum = ar.alloc("kmsum", [H, NB], F32)
        NG = S // 512
        KT_v = KT_d.rearrange("h d t -> d h t")
        TB = (0, 1)
        KB = (2, 3)
        VB = (4, 5)
        tile_ctr = [0]

        def A_norm_tile(g, t):
            r0 = g * 512 + t * P
            norm_tile(xall_d[r0:r0 + P, :], xsA[t][0], xsA[t][1], "xsA%d" % t, gpre_b, B_gpre, abA[t][0], abA[t][1])

        def A_T(g):
            aT, B_aT = aTA[g % 2]
            for t in range(4):
                T_tile(abA[t][0], abA[t][1], aT, B_aT, t * P, TB)

        def A_kv(g):
            aT, B_aT = aTA[g % 2]
            kst, B_kst = ktst[g % 2]
            for h in range(H):
                if h % 2 == 1 and g + 2 < NG:
                    A_norm_tile(g + 2, h // 2)
                bank = KB[h % 2]
                pk = ps_f32(bank)
                mm_group(pk, [(wkv[:, k, h * DH:(h + 1) * DH], aT[:, k, :]) for k in range(KC)],
                         reads=[B_aT] + B_wkvS, writes=[pbuf[bank]])
                pr.op("act", lambda e, pk=pk, h=h: e.copy(out=kst[:, h, :], in_=pk), reads=[pbuf[bank]], writes=[B_kst])
                pr.op("dve", lambda e, h=h: e.tensor_reduce(
                    out=kmsum[:, h, 2 * g:2 * g + 2], in_=kst[:, h, :].rearrange("p (a b) -> p a b", a=2, b=L),
                    axis=AX.X, op=ALU.add), reads=[B_kst], writes=[B_kmsum])
            pr.dma("pool", KT_v[:, :, g * 512:(g + 1) * 512], kst, "ktst%d" % (g % 2), reads=[B_kst], writes=[B_KT])
            for t in range(4):
                vi = (g * 4 + t) % 2
                vs, B_vs = vst[vi]
                for half in range(2):
                    bank = VB[half]
                    pv = ps_f32(bank)
                    mm_group(pv, [(aT[:, k, t * P:(t + 1) * P], wkv[:, k, 1024 + half * 512:1024 + (half + 1) * 512])
                                  for k in range(KC)], reads=[B_aT] + B_wkvS, writes=[pbuf[bank]])
                    hs_ = slice(half * 512, (half + 1) * 512)
                    if t % 2 == 0:
                        pr.op("dve", lambda e, pv=pv, vs=vs, hs_=hs_: e.tensor_tensor(
                            out=vs[:, hs_], in0=pv, in1=cvec[:, hs_], op=ALU.mult),
                            reads=[pbuf[bank], B_cvec], writes=[B_vs])
                    else:
                        pr.op("act", lambda e, pv=pv, vs=vs, hs_=hs_: e.copy(out=vs[:, hs_], in_=pv),
                              reads=[pbuf[bank]], writes=[B_vs])
                r0 = g * 512 + t * P
                pr.dma("pool", V_d[r0:r0 + P, :], vs, "vst%d" % vi, reads=[B_vs], writes=[B_V])

        B_KT = Buf("KT_d")
        B_V = Buf("V_d")
        for t in range(4):
            A_norm_tile(0, t)
        A_T(0)
        for t in range(4):
            A_norm_tile(1, t)
        for g in range(NG):
            if g + 1 < NG:
                A_T(g + 1)
            A_kv(g)
            if 2 <= g < 14:
                bg_issue(1, [ktst[g % 2][1]])
        pr.op("dve", lambda e: e.tensor_scalar(out=kmean, in0=kmsum, scalar1=1.0 / L, scalar2=None, op0=ALU.mult),
              reads=[B_kmsum], writes=[B_kmean])
        if debug:
            o = pr.dma("sp", kmean_dbg, kmean.rearrange("p h n -> p (h n)"), "dbg0", reads=[B_kmean])
            pr.must_finish(o)
        ar.release(mA)

        last_ops = []
        if stop_after == "A":
            return _finish(nc, pr, st, ar, [B_KT, B_V])

        mB = ar.mark()
        gpre_b, B_gpre = ar.alloc("gpre_b2", [D], F32)
        pr.dma("sp", gpre_b, grow_d[0:1, :].partition_broadcast(P), "c7", writes=[B_gpre])
        aTo, B_aTo = ar.alloc("aTo", [KC, NOWNH], BF16)
        Fall, B_Fall = ar.alloc("Fall", [16, H, NB], F32)
        mB1 = ar.mark()
        xsB = [ar.alloc("xsB%d" % i, [D], F32) for i in range(2)]
        abB = [ar.alloc("abB%d" % i, [D], BF16) for i in range(2)]
        for t in range(NOWNH // P):
            i = t % 2
            norm_T_tile(xown_d[t * P:(t + 1) * P, :], xsB[i][0], xsB[i][1], "xsA%d" % i, gpre_b, B_gpre,
                        abB[i][0], abB[i][1], aTo, B_aTo, t * P, TB)
        ar.release(mB1)
        NWS = 3
        wch = [ar.alloc("wch%d" % i, [KC, 512], BF16) for i in range(NWS)]
        wch_ctr = [0]

        def load_wchunk(src_ap, reads):
            i = wch_ctr[0] % NWS
            wch_ctr[0] += 1
            w, B_w = wch[i]
            pr.dma("sp", w, src_ap, "wch%d" % i, reads=reads, writes=[B_w])
            return w, B_w

        def in_chunk_src(c0):
            return w_in_bf[:, c0:c0 + 512].rearrange("(k p) n -> p k n", p=P)

        qst = [ar.alloc("qst%d" % i, [L], BF16) for i in range(2)]
        q32 = [ar.alloc("q32_%d" % i, [L], F32) for i in range(2)]
        kost = [ar.alloc("kost%d" % i, [L], BF16) for i in range(2)]
        vost = [ar.alloc("vost%d" % i, [512], BF16) for i in range(2)]
        fdt = [ar.alloc("fdt%d" % i, [2, H, NB], F32) for i in range(2)]
        gsb = [ar.alloc("gsb%d" % i, [NB], F32) for i in range(2)]
        top8 = [ar.alloc("top8_%d" % i, [8], F32) for i in range(2)]
        sig = [ar.alloc("sig%d" % i, [SLOTW], F32) for i in range(2)]
        hst = [ar.alloc("hst%d" % i, [SLOTW], BF16) for i in range(2)]
        dgb = [ar.alloc("dgb%d" % i, [CW, P], BF16) for i in range(2)]
        CVB = (6, 7)
        accD = [ar.alloc("accD%d" % i, [L], F32) for i in range(2)]
        B_QT = Buf("QT_d")
        B_KTo = Buf("KTo_d")
        B_Vo = Buf("Vo_d")
        B_yT = Buf("yT_d")
        B_F = Buf("F_d")
        PB = (2, 3, 4, 5)
        GB = 6
        pb_ctr = [0]

        def nbank():
            b = PB[pb_ctr[0] % len(PB)]
            pb_ctr[0] += 1
            return b
        ctr = {"q": 0, "k": 0, "v": 0, "u": 0, "g": 0, "c": 0}
        conv_pending = []

        chunk_list = [("q", 0), ("q", 512), ("k", 1024), ("k", 1536), ("v", 2048), ("v", 2560),
                      ("uv", 3072), ("ug", 4096), ("uv", 3584), ("ug", 4608)]
        pending = None
        nxt = load_wchunk(in_chunk_src(chunk_list[0][1]), in_col_bufs(chunk_list[0][1]))
        for ci, (kind, c0) in enumerate(chunk_list):
            w, B_w = nxt
            if ci + 1 < len(chunk_list):
                nxt = load_wchunk(in_chunk_src(chunk_list[ci + 1][1]), issued(in_col_bufs(chunk_list[ci + 1][1])))
            if ci >= 1:
                gate = {"q": B_QT, "k": B_KTo, "v": B_Vo, "uv": B_yT, "ug": B_yT}[chunk_list[ci - 1][0]]
                bg_issue(1, [gate])
            if kind in ("q", "k"):
                for s in range(NSLOT):
                    if kind == "q":
                        fi = ctr["g"] % 2
                        ctr["g"] += 1
                        fdv, B_fd = fdt[fi]
                        pr.dma("sp", fdv, fd_d[s].rearrange("p (t h n) -> p t h n", t=2, h=H, n=NB), "fdt%d" % fi,
                               writes=[B_fd])
                    for sub in range(4):
                        h = (c0 % 1024) // DH + sub
                        bank = nbank()
                        pq = ps_f32(bank, SLOTW)
                        mm_group(pq, [(w[:, k, sub * DH:(sub + 1) * DH], aTo[:, k, s * SLOTW:(s + 1) * SLOTW])
                                      for k in range(KC)], reads=[B_aTo, B_w], writes=[pbuf[bank]])
                        if kind == "k":
                            i = ctr["k"] % 2
                            ctr["k"] += 1
                            ks, B_ks = kost[i]
                            pr.op("act", lambda e, ks=ks, pq=pq: e.copy(out=ks, in_=pq[:, HALO:SLOTW]),
                                  reads=[pbuf[bank]], writes=[B_ks])
                            pr.dma("pool", KTo_d[h, :, s * L:(s + 1) * L], ks, "kost%d" % i, reads=[B_ks], writes=[B_KTo])
                            continue
                        i = ctr["q"] % 2
                        ctr["q"] += 1
                        qs, B_qs = qst[i]
                        qf, B_qf = q32[i]
                        pr.op("act", lambda e, qf=qf, pq=pq: e.copy(out=qf, in_=pq[:, HALO:SLOTW]),
                              reads=[pbuf[bank]], writes=[B_qf])
                        pr.op("dve", lambda e, qs=qs, qf=qf: e.tensor_copy(out=qs, in_=qf), reads=[B_qf], writes=[B_qs])
                        pr.dma("pool", QT_d[h, :, s * L:(s + 1) * L], qs, "qst%d" % i, reads=[B_qs], writes=[B_QT])
                        pg = ps_f32(GB, 2 * NB).rearrange("p (t n) -> p t n", t=2, n=NB)

                        def gmm(e, qf=qf, h=h, pg=pg):
                            e.matmul(pg[:, 0, :], lhsT=qf[:, 0:P], rhs=kmean[:, h, :], start=True, stop=True)
                            return e.matmul(pg[:, 1, :], lhsT=qf[:, P:2 * P], rhs=kmean[:, h, :], start=True, stop=True)
                        pr.op("pe", gmm, reads=[B_qf, B_kmean], writes=[pbuf[GB]])
                        for qt in range(2):
                            gi = ctr["u"] % 2
                            ctr["u"] += 1
                            gs, B_gs = gsb[gi]
                            t8, B_t8 = top8[gi]
                            pr.op("dve", lambda e, gs=gs, qt=qt, pg=pg, s=s: e.tensor_tensor(
                                out=gs, in0=pg[:, qt, :], in1=gbias[:, s, :], op=ALU.add),
                                reads=[pbuf[GB], B_gbias], writes=[B_gs])
                            pr.op("dve", lambda e, gs=gs, t8=t8: e.max(out=t8, in_=gs), reads=[B_gs], writes=[B_t8])
                            pr.op("dve", lambda e, gs=gs, t8=t8, qt=qt, h=h, s=s, fdv=fdv: e.scalar_tensor_tensor(
                                out=Fall[:, 2 * s + qt, h, :], in0=gs, scalar=t8[:, 2:3], in1=fdv[:, qt, h, :],
                                op0=ALU.is_ge, op1=ALU.mult), reads=[B_gs, B_t8, B_fd], writes=[B_Fall])
            elif kind == "v":
                for s in range(NSLOT):
                    for t in range(2):
                        bank = nbank()
                        pv = ps_f32(bank)
                        col = s * SLOTW + HALO + t * P
                        mm_group(pv, [(aTo[:, k, col:col + P], w[:, k, :]) for k in range(KC)],
                                 reads=[B_aTo, B_w], writes=[pbuf[bank]])
                        i = ctr["v"] % 2
                        ctr["v"] += 1
                        vs, B_vs = vost[i]
                        pr.op("act", lambda e, vs=vs, pv=pv: e.copy(out=vs, in_=pv), reads=[pbuf[bank]], writes=[B_vs])
                        r0 = s * L + t * P
                        cv = c0 - 2048
                        pr.dma("pool", Vo_d[r0:r0 + P, cv:cv + 512], vs, "vost%d" % i, reads=[B_vs], writes=[B_Vo])
            elif kind == "uv":
                pending = (w, B_w, c0)
            else:
                wv, B_wv, cv0 = pending
                for sub in range(4):
                    cch = (cv0 - 3072) // P + sub
                    dgv, B_dg = dgb[cch % 2]
                    for jt in range(CW):
                        pr.op("dve", lambda e, dgv=dgv, cch=cch, jt=jt: e.tensor_scalar(
                            out=dgv[:, jt, :], in0=ident, scalar1=wdw[:, cch, jt:jt + 1], scalar2=None, op0=ALU.mult),
                            reads=[B_ident, B_cols], writes=[B_dg])
                    for s in range(NSLOT):
                        bv = nbank()
                        bg = nbank()
                        pval = ps_f32(bv, SLOTW)
                        pgt = ps_f32(bg, SLOTW)
                        rhs_sl = slice(s * SLOTW, (s + 1) * SLOTW)
                        mm_group(pval, [(wv[:, k, sub * P:(sub + 1) * P], aTo[:, k, rhs_sl]) for k in range(KC)],
                                 reads=[B_aTo, B_wv], writes=[pbuf[bv]])
                        mm_group(pgt, [(w[:, k, sub * P:(sub + 1) * P], aTo[:, k, rhs_sl]) for k in range(KC)],
                                 reads=[B_aTo, B_w], writes=[pbuf[bg]])
                        i = ctr["c"] % 2
                        ctr["c"] += 1
                        sg, B_sg = sig[i]
                        hs, B_hs = hst[i]
                        aD, B_aD = accD[i]
                        pr.op("act", lambda e, sg=sg, pgt=pgt, cch=cch: e.activation(
                            out=sg, in_=pgt, func=AF.Sigmoid, bias=bglu[:, 8 + cch:9 + cch], scale=1.0),
                            reads=[pbuf[bg], B_cols], writes=[B_sg])
                        pr.op("dve", lambda e, hs=hs, pval=pval, sg=sg, cch=cch: e.scalar_tensor_tensor(
                            out=hs, in0=pval, scalar=bglu[:, cch:cch + 1], in1=sg, op0=ALU.add, op1=ALU.mult),
                            reads=[pbuf[bv], B_sg, B_cols], writes=[B_hs])
                        pr.op("dve", lambda e, hs=hs, s=s: e.tensor_scalar(
                            out=hs[:, 0:HALO], in0=hs[:, 0:HALO], scalar1=hmask[:, s:s + 1], scalar2=None, op0=ALU.mult),
                            reads=[B_hs, B_hmask], writes=[B_hs])
                        def conv_unit(dgv=dgv, B_dg=B_dg, hs=hs, B_hs=B_hs, aD=aD, B_aD=B_aD, cch=cch, s=s, i=i,
                                      cb=CVB[ctr["c"] % 2]):
                            pc = ps_f32(cb, L)
                            mm_group(pc, [(dgv[:, jt, :], hs[:, 2 + jt:2 + jt + L]) for jt in range(CW)],
                                     reads=[B_dg, B_hs], writes=[pbuf[cb]])
                            pr.op("dve", lambda e: e.tensor_scalar(
                                out=aD, in0=pc, scalar1=bdw[:, cch:cch + 1], scalar2=None, op0=ALU.add),
                                reads=[pbuf[cb], B_cols], writes=[B_aD])
                            pr.dma("pool", yT_d[cch, :, s * L:(s + 1) * L], aD, "accD%d" % i, reads=[B_aD],
                                   writes=[B_yT])
                        if conv_pending:
                            conv_pending.pop(0)()
                        conv_pending.append(conv_unit)
        while conv_pending:
            conv_pending.pop(0)()
        for h in range(H):
            pr.dma("pool", F_d[h].rearrange("p (q n) -> p q n", q=16, n=NB), Fall[:, :, h, :], "fst", reads=[B_Fall],
                   writes=[B_F])
        ar.release(mB)
        if stop_after == "B":
            return _finish(nc, pr, st, ar, [B_KT, B_V, B_QT, B_KTo, B_Vo, B_yT, B_F])

        B_mixT = Buf("mixT_d")
        mL = ar.mark()
        ysl = [ar.alloc("ysl%d" % i, [CC, L], F32) for i in range(2)]
        sqs = [ar.alloc("sqs%d" % i, [CC, L], F32) for i in range(2)]
        cst = [ar.alloc("cst%d" % i, [CC, L], BF16) for i in range(2)]
        mean_t, B_mean = ar.alloc("mean_t", [L], F32)
        msq_t, B_msq = ar.alloc("msq_t", [L], F32)
        rstd_t, B_rstdL = ar.alloc("rstd_t", [L], F32)
        mr_t, B_mr = ar.alloc("mr_t", [L], F32)
        tmpL = [ar.alloc("tmpL%d" % i, [L], F32) for i in range(2)]
        yT_v = yT_d.rearrange("c p t -> p c t")
        for s in range(NSLOT):
            i = s % 2
            ys, B_ys = ysl[i]
            sq, B_sq = sqs[i]
            cs, B_cs = cst[i]
            pr.dma("sp", ys, yT_v[:, :, s * L:(s + 1) * L], "ysl%d" % i, reads=[B_yT], writes=[B_ys])
            pr.op("act", lambda e, ys=ys, sq=sq: e.activation(out=sq, in_=ys, func=AF.Square), reads=[B_ys], writes=[B_sq])
            pm = ps_f32(2, L)
            pq2 = ps_f32(3, L)
            mm_group(pm, [(ones32, ys[:, c, :]) for c in range(CC)], reads=[B_ones, B_ys], writes=[pbuf[2]])
            mm_group(pq2, [(ones32, sq[:, c, :]) for c in range(CC)], reads=[B_ones, B_sq], writes=[pbuf[3]])
            pr.op("dve", lambda e, pm=pm: e.tensor_scalar(out=mean_t, in0=pm, scalar1=1.0 / C, scalar2=None, op0=ALU.mult),
                  reads=[pbuf[2]], writes=[B_mean])
            pr.op("dve", lambda e: e.tensor_tensor(out=msq_t, in0=mean_t, in1=mean_t, op=ALU.mult),
                  reads=[B_mean], writes=[B_msq])
            pr.op("dve", lambda e, pq2=pq2: e.scalar_tensor_tensor(out=rstd_t, in0=pq2, scalar=1.0 / C, in1=msq_t,
                                                                  op0=ALU.mult, op1=ALU.subtract),
                  reads=[pbuf[3], B_msq], writes=[B_rstdL])
            pr.op("act", lambda e: e.activation(out=rstd_t, in_=rstd_t, func=AF.Sqrt, bias=LN_EPS, scale=1.0),
                  reads=[B_rstdL], writes=[B_rstdL])
            pr.op("dve", lambda e: e.reciprocal(out=rstd_t, in_=rstd_t), reads=[B_rstdL], writes=[B_rstdL])
            pr.op("dve", lambda e: e.tensor_tensor(out=mr_t, in0=mean_t, in1=rstd_t, op=ALU.mult),
                  reads=[B_mean, B_rstdL], writes=[B_mr])
            for c in range(CC):
                tl, B_tl = tmpL[c % 2]
                pr.op("dve", lambda e, tl=tl, ys=ys, c=c: e.tensor_tensor(out=tl, in0=ys[:, c, :], in1=rstd_t, op=ALU.mult),
                      reads=[B_ys, B_rstdL], writes=[B_tl])
                pr.op("dve", lambda e, tl=tl: e.tensor_tensor(out=tl, in0=tl, in1=mr_t, op=ALU.subtract),
                      reads=[B_tl, B_mr], writes=[B_tl])
                pr.op("act", lambda e, tl=tl, cs=cs, c=c: e.activation(out=cs[:, c, :], in_=tl, func=AF.Silu,
                                                                     bias=lnb[:, c:c + 1], scale=lng[:, c:c + 1]),
                      reads=[B_tl, B_cols], writes=[B_cs])
            pr.dma("pool", mixT_d[C:2 * C, s * L:(s + 1) * L].rearrange("(c p) t -> p c t", p=P), cs, "cst%d" % i,
                   reads=[B_cs], writes=[B_mixT])
        ar.release(mL)
        if stop_after == "L":
            return _finish(nc, pr, st, ar, [B_KT, B_V, B_QT, B_KTo, B_Vo, B_yT, B_F, B_mixT])

        mT = ar.mark()
        NT = S // P
        VW = DH + 2
        KTh = [ar.alloc("KTh%d" % i, [S], BF16) for i in range(2)]
        Vh = [ar.alloc("Vh%d" % i, [NT, VW], BF16) for i in range(2)]
        KToh = [ar.alloc("KToh%d" % i, [NOWN], BF16) for i in range(2)]
        Voh = [ar.alloc("Voh%d" % i, [NOWN // P, VW], BF16) for i in range(2)]
        QTh = [ar.alloc("QTh%d" % i, [NOWN], BF16) for i in range(2)]
        Fh = [ar.alloc("Fh%d" % i, [16, NB], F32) for i in range(2)]
        pTs = [ar.alloc("pT%d" % i, [2, L], BF16) for i in range(3)]
        pTo = [ar.alloc("pTo%d" % i, [3 * P], BF16) for i in range(2)]
        accs = [ar.alloc("acc%d" % i, [2, VW], F32) for i in range(2)]
        tmps = [ar.alloc("tmpT%d" % i, [2, VW], F32) for i in range(3)]
        rcs = [ar.alloc("rc%d" % i, [2], F32) for i in range(2)]
        obf = [ar.alloc("obf%d" % i, [2, DH], BF16) for i in range(2)]
        ast = [ar.alloc("ast%d" % i, [L], BF16) for i in range(2)]
        for i in range(2):
            pr.op("pool", lambda e, i=i: e.memset(Vh[i][0][:, :, DH:VW], 1.0), writes=[Vh[i][1]])
            pr.op("pool", lambda e, i=i: e.memset(Voh[i][0][:, :, DH:VW], 1.0), writes=[Voh[i][1]])
        SBK = (0, 1, 2)
        OBK = (3, 4, 5)
        TBK = 6

        def T_load(h):
            i = h % 2
            pr.dma("sp", KTh[i][0], KT_d[h], "KTh%d" % i, reads=[B_KT], writes=[KTh[i][1]])
            ch = float(np.exp(-128.0 * alibi_slopes()[h]))
            vev = Vh[i][0].rearrange("p (a two) w -> p a two w", two=2)[:, :, 0, DH:VW]
            pr.op("pool", lambda e, vev=vev, ch=ch: e.memset(vev, ch), writes=[Vh[i][1]])
            vsrc = V_d[:, h * DH:(h + 1) * DH].rearrange("(t p) d -> p t d", p=P)
            for q4 in range(4):
                pr.dma("sp", Vh[i][0][:, q4 * 16:(q4 + 1) * 16, 0:DH], vsrc[:, q4 * 16:(q4 + 1) * 16, :], "Vh%d_%d" % (i, q4),
                       reads=[B_V], writes=[Vh[i][1]])
            pr.dma("sp", KToh[i][0], KTo_d[h], "KToh%d" % i, reads=[B_KTo], writes=[KToh[i][1]])
            pr.dma("sp", Voh[i][0][:, :, 0:DH], Vo_d[:, h * DH:(h + 1) * DH].rearrange("(t p) d -> p t d", p=P),
                   "Voh%d" % i, reads=[B_Vo], writes=[Voh[i][1]])
            pr.dma("sp", QTh[i][0], QT_d[h], "QTh%d" % i, reads=[B_QT], writes=[QTh[i][1]])
            pr.dma("sp", Fh[i][0], F_d[h].rearrange("p (q n) -> p q n", q=16, n=NB), "Fh%d" % i, reads=[B_F],
                   writes=[Fh[i][1]])

        uctr = [0]

        def T_head(h):
            i = h % 2
            kth, B_kth = KTh[i]
            vh, B_vh = Vh[i]
            kto, B_kto = KToh[i]
            voh, B_voh = Voh[i]
            qth, B_qth = QTh[i]
            fh, B_fh = Fh[i]
            units = []
            for s in range(NSLOT):
                units.append((s, -1))
                for n in range(PAST[s]):
                    units.append((s, n))
            state = {}
            deferred = []

            def emit_qk(idx):
                s, n = units[idx]
                u = uctr[0]
                uctr[0] += 1
                sb = SBK[u % 3]
                qsl = slice(s * L, (s + 1) * L)
                if n >= 0:
                    sv = ps_f32(sb).rearrange("p (t q) -> p t q", t=2, q=L)

                    def qk(e, sv=sv, n=n, qsl=qsl):
                        e.matmul(sv[:, 0, :], lhsT=kth[:, (2 * n) * P:(2 * n + 1) * P], rhs=qth[:, qsl], start=True, stop=True)
                        return e.matmul(sv[:, 1, :], lhsT=kth[:, (2 * n + 1) * P:(2 * n + 2) * P], rhs=qth[:, qsl],
                                        start=True, stop=True)
                    pr.op("pe", qk, reads=[B_kth, B_qth], writes=[pbuf[sb]])
                    pt, B_pt = pTs[u % 3]
                    pr.op("act", lambda e, pt=pt, sv=sv: e.activation(
                        out=pt, in_=sv, func=AF.Exp, bias=bkt[:, h, 1:2], scale=SCALE),
                        reads=[pbuf[sb], B_bkt], writes=[B_pt])
                    state[idx] = (u, pt, B_pt)
                else:
                    sv = ps_f32(sb, 3 * P)
                    q0 = s * L

                    def qk(e, sv=sv, q0=q0):
                        e.matmul(sv[:, 0:P], lhsT=kto[:, q0:q0 + P], rhs=qth[:, q0:q0 + P], start=True, stop=True)
                        e.matmul(sv[:, P:2 * P], lhsT=kto[:, q0 + P:q0 + 2 * P], rhs=qth[:, q0 + P:q0 + 2 * P],
                                 start=True, stop=True)
                        return e.matmul(sv[:, 2 * P:3 * P], lhsT=kto[:, q0:q0 + P], rhs=qth[:, q0 + P:q0 + 2 * P],
                                        start=True, stop=True)
                    pr.op("pe", qk, reads=[B_kto, B_qth], writes=[pbuf[sb]])
                    pt, B_pt = pTo[s % 2]
                    pr.op("act", lambda e, pt=pt, sv=sv: e.activation(
                        out=pt[:, 0:2 * P], in_=sv[:, 0:2 * P], func=AF.Exp, bias=bkt[:, h, 1:2], scale=SCALE),
                        reads=[pbuf[sb], B_bkt], writes=[B_pt])
                    pr.op("act", lambda e, pt=pt, sv=sv: e.activation(
                        out=pt[:, 2 * P:3 * P], in_=sv[:, 2 * P:3 * P], func=AF.Exp, bias=bkt[:, h, 0:1], scale=SCALE),
                        reads=[pbuf[sb], B_bkt], writes=[B_pt])
                    for a in range(2):
                        pr.op("pool", lambda e, pt=pt, a=a: e.tensor_tensor(
                            out=pt[:, a * P:(a + 1) * P], in0=pt[:, a * P:(a + 1) * P], in1=tri, op=ALU.mult),
                            reads=[B_pt, B_tri], writes=[B_pt])
                    state[idx] = (u, pt, B_pt)

            def emit_pv(idx):
                s, n = units[idx]
                u, pt, B_pt = state.pop(idx)
                ob = OBK[u % 3]
                ov = ps_f32(ob, 2 * VW).rearrange("p (t d) -> p t d", t=2, d=VW)
                acc, B_acc = accs[s % 2]
                NV = DH + 1
                if n >= 0:
                    def pv(e, ov=ov, pt=pt, n=n):
                        ins = None
                        for qt in range(2):
                            for t in range(2):
                                ins = e.matmul(ov[:, qt, 0:NV], lhsT=pt[:, t, qt * P:(qt + 1) * P],
                                               rhs=vh[:, 2 * n + t, 0:NV], start=(t == 0), stop=(t == 1))
                        return ins
                    pr.op("pe", pv, reads=[B_pt, B_vh], writes=[pbuf[ob]])
                    for qt in range(2):
                        pr.op("dve", lambda e, ov=ov, acc=acc, qt=qt, n=n, s=s: e.scalar_tensor_tensor(
                            out=acc[:, qt, 0:NV], in0=ov[:, qt, 0:NV], scalar=fh[:, 2 * s + qt, n:n + 1],
                            in1=acc[:, qt, 0:NV], op0=ALU.mult, op1=ALU.add),
                            reads=[pbuf[ob], B_fh, B_acc], writes=[B_acc])
                else:
                    def pv(e, ov=ov, pt=pt, s=s):
                        e.matmul(ov[:, 0, 0:NV], lhsT=pt[:, 0:P], rhs=voh[:, 2 * s, 0:NV], start=True, stop=True)
                        e.matmul(ov[:, 1, 0:NV], lhsT=pt[:, 2 * P:3 * P], rhs=voh[:, 2 * s, 0:NV], start=True, stop=False)
                        return e.matmul(ov[:, 1, 0:NV], lhsT=pt[:, P:2 * P], rhs=voh[:, 2 * s + 1, 0:NV],
                                        start=False, stop=True)
                    pr.op("pe", pv, reads=[B_pt, B_voh], writes=[pbuf[ob]])
                    pr.op("dve", lambda e, ov=ov, acc=acc: e.tensor_scalar(
                        out=acc[:, :, 0:NV], in0=ov[:, :, 0:NV], scalar1=fown[:, h:h + 1], scalar2=None,
                        op0=ALU.mult), reads=[pbuf[ob], B_fown], writes=[B_acc])
                last = (idx + 1 == len(units)) or (units[idx + 1][0] != s)
                if last:
                    deferred.append([2, s])

            def finalize(s):
                acc, B_acc = accs[s % 2]
                rc, B_rc = rcs[s % 2]
                ob_, B_ob = obf[s % 2]
                asv, B_as = ast[s % 2]
                pr.op("dve", lambda e: e.reciprocal(out=rc, in_=acc[:, :, DH]), reads=[B_acc], writes=[B_rc])
                for qt in range(2):
                    pr.op("dve", lambda e, qt=qt: e.tensor_scalar(out=ob_[:, qt, :], in0=acc[:, qt, 0:DH],
                                                                  scalar1=rc[:, qt:qt + 1], scalar2=None, op0=ALU.mult),
                          reads=[B_acc, B_rc], writes=[B_ob])
                ptv = ps_bf16(TBK)[:, 0:L].rearrange("p (t q) -> p t q", t=2, q=P)

                def tr(e):
                    e.transpose(out=ptv[:, 0, :], in_=ob_[:, 0, :], identity=ident)
                    return e.transpose(out=ptv[:, 1, :], in_=ob_[:, 1, :], identity=ident)
                pr.op("pe", tr, reads=[B_ob, B_ident], writes=[pbuf[TBK]])
                pr.op("act", lambda e: e.copy(out=asv, in_=ps_bf16(TBK)[:, 0:L]), reads=[pbuf[TBK]], writes=[B_as])
                pr.dma("pool", mixT_d[h * DH:(h + 1) * DH, s * L:(s + 1) * L], asv, "ast%d" % (s % 2), reads=[B_as],
                       writes=[B_mixT])

            def tick():
                for dd in list(deferred):
                    dd[0] -= 1
                    if dd[0] <= 0:
                        deferred.remove(dd)
                        finalize(dd[1])

            emit_qk(0)
            emit_qk(1)
            for idx in range(len(units)):
                if idx + 2 < len(units):
                    emit_qk(idx + 2)
                emit_pv(idx)
                tick()
            for dd in list(deferred):
                finalize(dd[1])
            deferred.clear()

        T_load(0)
        for h in range(H):
            if h + 1 < H:
                T_load(h + 1)
            bg_issue(4, [B_mixT])
            T_head(h)
        bg_issue(100, [B_mixT])
        ar.release(mT)
        if stop_after == "T":
            return _finish(nc, pr, st, ar, [B_mixT])

        gb_t = []
        for gi_, nm in ((1, "gpost"), (2, "gffn"), (3, "gfpost")):
            t_, b_ = ar.alloc(nm, [D], F32)
            pr.dma("sp", t_, grow_d[gi_:gi_ + 1, :].partition_broadcast(P), "c7_%d" % gi_, writes=[b_])
            gb_t.append((t_, b_))
        (gpost_b, B_gpost), (gffn_b, B_gffn), (gfpost_b, B_gfpost) = gb_t
        mixS, B_mixS = ar.alloc("mixS", [KC, 512], BF16)
        fT, B_fT = ar.alloc("fT", [KC, 512], BF16)
        accF, _ = ar.alloc("accF", [4, D], F32)
        B_accF = [Buf("accF%d" % t) for t in range(4)]
        hTs = [ar.alloc("hT%d" % i, [4, 512], BF16) for i in range(2)]
        rts = [ar.alloc("rt%d" % i, [512], F32) for i in range(2)]
        NWS2 = 4
        wc2 = [ar.alloc("wc2_%d" % i, [KC, 512], BF16) for i in range(NWS2)]
        xsC = [ar.alloc("xsC%d" % i, [D], F32) for i in range(2)]
        abC = [ar.alloc("abC%d" % i, [D], BF16) for i in range(4)]
        w2ctr = [0]

        def load_w2(src_ap, reads):
            i = w2ctr[0] % NWS2
            w2ctr[0] += 1
            w, B_w = wc2[i]
            pr.dma("sp", w, src_ap, "wch%d" % i, reads=reads, writes=[B_w])
            return w, B_w
        B_y = [Buf("y%d" % t) for t in range(NOWN // P)]
        CB = (2, 3)
        HBK = (4, 5)
        cb_ctr = [0]
        xc_ctr = [0]

        def rstd_of(src, B_src, junk, B_junk):
            i = stat_ctr[0] % 8
            stat_ctr[0] += 1
            ssv = ss_t[:, i:i + 1]
            rsv = rs_t[:, i:i + 1]
            pr.op("act", lambda e: e.activation(out=junk, in_=src, func=AF.Square, accum_out=ssv),
                  reads=[B_src], writes=[B_ss[i], B_junk])
            pr.op("act", lambda e: e.activation(out=rsv, in_=ssv, func=AF.Sqrt, bias=RMS_EPS, scale=1.0 / D),
                  reads=[B_ss[i]], writes=[B_rs[i]])
            pr.op("dve", lambda e: e.reciprocal(out=rsv, in_=rsv), reads=[B_rs[i]], writes=[B_rs[i]])
            return rsv, B_rs[i]

        def post_tile(gi, t):
            i = xc_ctr[0] % 2
            xc_ctr[0] += 1
            xs, B_xs = xsC[i]
            ab, B_ab = abC[t]
            s_ = 2 * gi + t // 2
            r0 = s_ * SLOTW + HALO + (t % 2) * P
            pr.dma("sp", xs, xown_d[r0:r0 + P, :], "xsA%d" % i, writes=[B_xs])
            rsv, B_r = rstd_of(accF[:, t, :], B_accF[t], ab, B_ab)
            pr.op("dve", lambda e, t=t, rsv=rsv: e.scalar_tensor_tensor(
                out=accF[:, t, :], in0=accF[:, t, :], scalar=rsv, in1=gpost_b, op0=ALU.mult, op1=ALU.mult),
                reads=[B_accF[t], B_r, B_gpost], writes=[B_accF[t]])
            pr.op("dve", lambda e, t=t, xs=xs: e.tensor_tensor(out=xs, in0=accF[:, t, :], in1=xs, op=ALU.add),
                  reads=[B_accF[t], B_xs], writes=[B_xs])
            yt = gi * 4 + t
            pr.dma("pool", y_d[yt * P:(yt + 1) * P, :], xs, "xsSt%d" % i, reads=[B_xs], writes=[B_y[yt]])
            ab, B_ab = abC[t]
            norm_tile(None, xs, B_xs, None, gffn_b, B_gffn, ab, B_ab)

        def final_tile(gi, t):
            i = xc_ctr[0] % 2
            xc_ctr[0] += 1
            xs, B_xs = xsC[i]
            ab, B_ab = abC[t]
            yt = gi * 4 + t
            pr.dma("sp", xs, y_d[yt * P:(yt + 1) * P, :], "xsA%d" % i, reads=[B_y[yt]], writes=[B_xs])
            rsv, B_r = rstd_of(accF[:, t, :], B_accF[t], ab, B_ab)
            pr.op("dve", lambda e, t=t, rsv=rsv: e.scalar_tensor_tensor(
                out=accF[:, t, :], in0=accF[:, t, :], scalar=rsv, in1=gfpost_b, op0=ALU.mult, op1=ALU.mult),
                reads=[B_accF[t], B_r, B_gfpost], writes=[B_accF[t]])
            pr.op("dve", lambda e, t=t, xs=xs: e.tensor_tensor(out=xs, in0=accF[:, t, :], in1=xs, op=ALU.add),
                  reads=[B_accF[t], B_xs], writes=[B_xs])
            o = pr.dma("pool", y_d[yt * P:(yt + 1) * P, :], xs, "xsSt%d" % i, reads=[B_xs], writes=[B_y[yt]])
            pr.must_finish(o)

        for gi in range(4):
            pr.dma("sp", mixS, mixT_d[:, gi * 512:(gi + 1) * 512].rearrange("(k p) t -> p k t", p=P), "mixS",
                   reads=[B_mixT], writes=[B_mixS])
            nxt = load_w2(w_out_bf[:, 0:512].rearrange("(k p) n -> p k n", p=P), issued(B_wout))
            for n in range(4):
                w, B_w = nxt
                if n + 1 < 4:
                    nxt = load_w2(w_out_bf[:, (n + 1) * 512:(n + 2) * 512].rearrange("(k p) n -> p k n", p=P), B_wout)
                else:
                    nxtW1 = [load_w2(w_ff1_bf[:, 0:512].rearrange("(k p) n -> p k n", p=P), B_wff1[0])]
                for t in range(4):
                    bank = CB[cb_ctr[0] % 2]
                    cb_ctr[0] += 1
                    po = ps_f32(bank)
                    mm_group(po, [(mixS[:, k, t * P:(t + 1) * P], w[:, k, :]) for k in range(KC)],
                             reads=[B_mixS, B_w], writes=[pbuf[bank]])
                    pr.op("act", lambda e, po=po, t=t, n=n: e.copy(out=accF[:, t, n * 512:(n + 1) * 512], in_=po),
                          reads=[pbuf[bank]], writes=[B_accF[t]])
                    if n == 3:
                        post_tile(gi, t)
            for t in range(4):
                T_tile(abC[t][0], abC[t][1], fT, B_fT, t * P, TB)
            NFC = DFF // 512

            def w1src(c):
                return w_ff1_bf[:, c * 512:(c + 1) * 512].rearrange("(k p) n -> p k n", p=P), B_wff1[c // 4]

            def w2src(c):
                return w_ff2_bf[c * 512:(c + 1) * 512, :].rearrange("(j p) n -> p j n", p=P), B_wff2[c // 4]
            W1 = {0: nxtW1[0]}
            W2 = {}
            W1[1] = load_w2(*w1src(1))
            W2[0] = load_w2(*w2src(0))

            def ffn_H(c):
                w, B_w = W1.pop(c)
                hT, B_hT = hTs[c % 2]
                for j in range(4):
                    bank = HBK[j % 2]
                    ph = ps_f32(bank)
                    mm_group(ph, [(w[:, k, j * P:(j + 1) * P], fT[:, k, :]) for k in range(KC)],
                             reads=[B_w, B_fT], writes=[pbuf[bank]])
                    rt, B_rt = rts[j % 2]
                    pr.op("act", lambda e, rt=rt, ph=ph: e.activation(out=rt, in_=ph, func=AF.Relu),
                          reads=[pbuf[bank]], writes=[B_rt])
                    pr.op("act", lambda e, rt=rt, hT=hT, j=j: e.activation(out=hT[:, j, :], in_=rt, func=AF.Square),
                          reads=[B_rt], writes=[B_hT])

            def ffn_O(c):
                w, B_w = W2.pop(c)
                wv_ = w.rearrange("p k n -> p (k n)").rearrange("p (j n) -> p j n", j=4, n=D)
                hT, B_hT = hTs[c % 2]
                for t in range(4):
                    for n in range(4):
                        bank = CB[cb_ctr[0] % 2]
                        cb_ctr[0] += 1
                        po = ps_f32(bank)
                        mm_group(po, [(hT[:, j, t * P:(t + 1) * P], wv_[:, j, n * 512:(n + 1) * 512]) for j in range(4)],
                                 reads=[B_hT, B_w], writes=[pbuf[bank]])
                        dst = accF[:, t, n * 512:(n + 1) * 512]
                        if c == 0:
                            pr.op("dve", lambda e, dst=dst, po=po: e.tensor_copy(out=dst, in_=po),
                                  reads=[pbuf[bank]], writes=[B_accF[t]])
                        else:
                            pr.op("dve", lambda e, dst=dst, po=po: e.tensor_tensor(out=dst, in0=po, in1=dst, op=ALU.add),
                                  reads=[pbuf[bank], B_accF[t]], writes=[B_accF[t]])
                    if c == NFC - 1:
                        final_tile(gi, t)
            ffn_H(0)
            for c in range(NFC):
                if c + 2 < NFC:
                    W1[c + 2] = load_w2(*w1src(c + 2))
                if c + 1 < NFC:
                    W2[c + 1] = load_w2(*w2src(c + 1))
                    ffn_H(c + 1)
                ffn_O(c)
        return _finish(nc, pr, st, ar, [])


def _finish(nc, pr, st, ar, bufs):
    for b in bufs:
        if b.writer is not None:
            pr.must_finish(b.writer)
    pr.emit_all(st)
    nc._mk_info = dict(n_sems=pr.n_sems, peak=ar.peak, nops=len(pr.all_ops), counts=pr.max_count)
    return nc


def alibi_slopes():
    return (2.0 ** (-8.0 * np.arange(1, H + 1) / H)).astype(np.float64)


def core_tables(j):
    blks = own_blocks(j)
    sl = alibi_slopes()
    fd = np.zeros((NSLOT, P, 2, H, NB), np.float64)
    gb = np.full((P, NSLOT, NB), -1e30, np.float32)
    hm = np.ones((P, NSLOT), np.float32)
    q = np.arange(P)
    for s, blk in enumerate(blks):
        if blk == 0:
            hm[:, s] = 0.0
        for n in range(blk):
            gb[:, s, n] = 0.0
            for qt in range(2):
                dist = 256 * (n - blk) + 255 - (128 * qt + q)
                fd[s, :, qt, :, n] = np.exp(sl[None, :] * dist[:, None])
    return (fd.reshape(NSLOT, P, 2 * H * NB).astype(np.float32), gb.reshape(P, NSLOT * NB), hm)


def const_tables():
    sl = alibi_slopes()
    p = np.arange(P)
    bkt = np.zeros((P, H, 2), np.float64)
    for t in range(2):
        bkt[:, :, t] = sl[None, :] * (t * 128 + p[:, None] - 255)
    fown = np.exp(sl[None, :] * (127 - p[:, None]))
    tri = (p[None, :] >= p[:, None]).astype(np.float32)
    cvec = np.repeat(np.exp(-128.0 * sl), DH)[None, :].repeat(P, 0)
    return (bkt.reshape(P, 2 * H).astype(np.float32), fown.astype(np.float32),
            tri.astype(ml_dtypes.bfloat16), np.eye(P, dtype=np.float32).astype(ml_dtypes.bfloat16),
            np.ascontiguousarray(cvec.astype(np.float32)))


def make_in_maps(inputs, cores=range(8)):
    x = np.asarray(inputs["x"], np.float32)
    f = lambda k: np.ascontiguousarray(np.asarray(inputs[k], np.float32)[0])
    w_in, w_out, w_ff1, w_ff2 = f("w_in"), f("w_out"), f("w_ff1"), f("w_ff2")
    grow = np.stack([f("g_mix_pre"), f("g_mix_post"), f("g_ffn_pre"), f("g_ffn_post")]).astype(np.float32)
    b_glu, w_dw, b_dw, ln_g, ln_b = f("b_glu"), f("w_dw"), f("b_dw"), f("ln_conv_g"), f("ln_conv_b")
    cols = np.concatenate([
        b_glu.reshape(16, P).T,
        w_dw.reshape(CW, CC, P).transpose(2, 1, 0).reshape(P, CC * CW),
        b_dw.reshape(CC, P).T, ln_g.reshape(CC, P).T, ln_b.reshape(CC, P).T], axis=1).astype(np.float32)
    bkt, fown, tri, ident, cvec = const_tables()
    maps = []
    for c in cores:
        bi, j = c // 4, c % 4
        fd, gb, hm = core_tables(j)
        xo = np.zeros((NSLOT, SLOTW, D), np.float32)
        for s, blk in enumerate(own_blocks(j)):
            lo = L * blk - HALO
            if lo < 0:
                xo[s, HALO:] = x[bi, 0:L]
            else:
                xo[s] = x[bi, lo:lo + SLOTW]
        maps.append(dict(xall=np.ascontiguousarray(x[bi]), xown=xo.reshape(NOWNH, D), w_in=w_in, w_out=w_out,
                         w_ff1=w_ff1, w_ff2=w_ff2, grow=grow, cols=np.ascontiguousarray(cols), fd=fd, gbias=gb,
                         hmask=hm, bkt=bkt, fown=fown, tri=tri, ident=ident, cvec=cvec))
    return maps


_NC = None


def kernel(**inputs):
    global _NC
    if _NC is None:
        _NC = build_nc()
    maps = make_in_maps(inputs)
    res = run_bass_kernel_spmd(_NC, maps, core_ids=list(range(8)))
    x = np.asarray(inputs["x"])
    out = np.zeros(x.shape, np.float32)
    for c in range(8):
        bi, j = c // 4, c % 4
        y = np.asarray(res.results[c]["y"])
        for s, blk in enumerate(own_blocks(j)):
            out[bi, L * blk:L * (blk + 1)] = y[s * L:(s + 1) * L]
    return out
```

```python
import contextlib
import numpy as np
import ml_dtypes
import concourse.bass as bass
import concourse.mybir as mybir
from concourse.bass_utils import run_bass_kernel_spmd

F32 = mybir.dt.float32
BF16 = mybir.dt.bfloat16
ALU = mybir.AluOpType
AF = mybir.ActivationFunctionType
AX = mybir.AxisListType

P = 128
D = 2048
KC = 16
S = 8192
NB = 32
L = 256
H = 8
DH = 128
C = 1024
CC = 8
DFF = 8192
INC = 5120
NSLOT = 8
HALO = 32
SLOTW = L + HALO
NOWN = NSLOT * L
NOWNH = NSLOT * SLOTW
SCALE = DH ** -0.5
RMS_EPS = 1e-6
LN_EPS = 1e-5
CW = 31

STREAMS = ("pe", "act", "dve", "pool", "sp")


class Buf:
    __slots__ = ("name", "writer", "readers", "inherit", "excl")

    def __init__(self, name, inherit=(), excl=False):
        self.name = name
        self.excl = excl
        self.writer = None
        self.readers = []
        self.inherit = list(inherit)


class Op:
    __slots__ = ("stream", "emit", "deps", "is_dma", "semkey", "signal", "name")

    def __init__(self, stream, emit, is_dma=False, semkey=None, name=""):
        self.stream = stream
        self.emit = emit
        self.deps = []
        self.is_dma = is_dma
        self.semkey = semkey
        self.signal = False
        self.name = name


class Prog:
    def __init__(self, nc):
        self.nc = nc
        self.ops = {s: [] for s in STREAMS}
        self.all_ops = []
        self.final_waits = []

    def _add(self, op, reads, writes, after=()):
        deps = []
        for b in after:
            if b.writer is not None:
                deps.append(b.writer)
        for b in reads:
            if b.writer is not None:
                deps.append(b.writer)
            elif b.inherit:
                deps.extend(b.inherit)
            if b.excl:
                deps.extend(r for r in b.readers if r.stream != op.stream)
        for b in writes:
            if b.writer is not None:
                deps.append(b.writer)
            deps.extend(b.readers)
            if b.inherit:
                deps.extend(b.inherit)
                b.inherit = []
        seen = set()
        for d in deps:
            if d is op or id(d) in seen:
                continue
            seen.add(id(d))
            op.deps.append(d)
        for b in reads:
            b.readers.append(op)
        for b in writes:
            b.writer = op
            b.readers = []
        self.ops[op.stream].append(op)
        self.all_ops.append(op)
        return op

    def op(self, stream, emit, reads=(), writes=(), name=""):
        return self._add(Op(stream, emit, name=name), reads, writes)

    def dma(self, stream, out, in_, semkey, reads=(), writes=(), name="", after=()):
        def emit(eng):
            return eng.dma_start(out=out, in_=in_)
        return self._add(Op(stream, emit, is_dma=True, semkey=semkey, name=name), reads, writes, after)

    def must_finish(self, op):
        self.final_waits.append(op)

    def emit_all(self, stack):
        nc = self.nc
        for op in self.all_ops:
            for d in op.deps:
                d.signal = True
        for op in self.final_waits:
            op.signal = True
        eng_sem = {s: stack.enter_context(nc.semaphore("done_" + s)) for s in STREAMS}
        dma_sems = {}
        dma_cnt = {}
        cnt = {s: 0 for s in STREAMS}
        comp = {}
        for op in self.all_ops:
            s = op.stream
            if op.is_dma:
                k = op.semkey
                if k not in dma_sems:
                    dma_sems[k] = stack.enter_context(nc.semaphore("dq_%d" % len(dma_sems)))
                    dma_cnt[k] = 0
                dma_cnt[k] += 16
                comp[id(op)] = (dma_sems[k], dma_cnt[k])
            elif op.signal:
                cnt[s] += 1
                comp[id(op)] = (eng_sem[s], cnt[s])
        self.n_sems = len(dma_sems) + len(STREAMS)
        self.max_count = dict(cnt)
        block = stack.enter_context(nc.Block())
        prog = self

        def run_stream(s, eng):
            waited = {}
            for op in prog.ops[s]:
                need = {}
                for d in op.deps:
                    sem, val = comp[id(d)]
                    key = id(sem)
                    if waited.get(key, 0) >= val:
                        continue
                    if key not in need or need[key][1] < val:
                        need[key] = (sem, val)
                for key, (sem, val) in need.items():
                    waited[key] = val
                    eng.wait_ge(sem, val)
                ins = op.emit(eng)
                if op.is_dma:
                    ins.then_inc(comp[id(op)][0], 16)
                elif op.signal:
                    ins.then_inc(eng_sem[s], 1)
            if s == "sp":
                for op in prog.final_waits:
                    sem, val = comp[id(op)]
                    eng.wait_ge(sem, val)

        @block.tensor
        def _(e):
            run_stream("pe", e)

        @block.scalar
        def _(e):
            run_stream("act", e)

        @block.vector
        def _(e):
            run_stream("dve", e)

        @block.gpsimd
        def _(e):
            run_stream("pool", e)

        @block.sync
        def _(e):
            run_stream("sp", e)


DT_SIZE = {F32: 4, BF16: 2}


class Arena:
    def __init__(self, nc, stack, kib):
        self.words = kib * 256
        self.t = stack.enter_context(nc.sbuf_tensor("arena", [P, self.words], F32))
        self.top = 0
        self.peak = 0
        self.retired = []
        self.live = []

    def alloc(self, name, shape, dtype):
        n = 1
        for s in shape:
            n *= s
        nbytes = (n * DT_SIZE[dtype] + 31) // 32 * 32
        lo = self.top
        hi = lo + nbytes
        assert hi <= self.words * 4, "SBUF arena overflow at %s: %d > %d" % (name, hi, self.words * 4)
        self.top = hi
        self.peak = max(self.peak, hi)
        inh = []
        keep = []
        for (l, h, ops) in self.retired:
            if l < hi and h > lo:
                inh.extend(ops)
                if l >= lo and h <= hi:
                    continue
            keep.append((l, h, ops))
        self.retired = keep
        buf = Buf(name, inherit=inh)
        v = self.t[:, lo // 4:hi // 4]
        if dtype != F32:
            v = v.bitcast(dtype)
        v = v[:, 0:n]
        if len(shape) == 2:
            v = v.rearrange("p (a b) -> p a b", a=shape[0], b=shape[1])
        elif len(shape) == 3:
            v = v.rearrange("p (a b c) -> p a b c", a=shape[0], b=shape[1], c=shape[2])
        elif len(shape) == 4:
            v = v.rearrange("p (a b c d) -> p a b c d", a=shape[0], b=shape[1], c=shape[2], d=shape[3])
        self.live.append((lo, hi, buf))
        return v, buf

    def mark(self):
        return self.top

    def release(self, mark):
        keep = []
        for (lo, hi, buf) in self.live:
            if lo >= mark:
                ops = list(buf.readers) + list(buf.inherit)
                if buf.writer is not None:
                    ops.append(buf.writer)
                if ops:
                    self.retired.append((lo, hi, ops))
            else:
                keep.append((lo, hi, buf))
        self.live = keep
        self.top = mark


def own_blocks(j):
    return [8 * (s // 2) + (j if s % 2 == 0 else 7 - j) for s in range(NSLOT)]


PAST = [8 * (s // 2) + (3 if s % 2 == 0 else 7) for s in range(NSLOT)]


def build_nc(debug=False, stop_after="all"):
    nc = bass.Bass("TRN2", target_bir_lowering=False)
    skind = "ExternalOutput" if debug else "Internal"

    def din(name, shape, dt=F32):
        return nc.dram_tensor(name, list(shape), dt, kind="ExternalInput").ap()

    def dscr(name, shape, dt):
        return nc.dram_tensor(name, list(shape), dt, kind=skind).ap()

    xall_d = din("xall", [S, D])
    xown_d = din("xown", [NOWNH, D])
    w_in_d = din("w_in", [D, INC])
    w_out_d = din("w_out", [D, D])
    w_ff1_d = din("w_ff1", [D, DFF])
    w_ff2_d = din("w_ff2", [DFF, D])
    grow_d = din("grow", [4, D])
    cols_d = din("cols", [P, 16 + CC * CW + 3 * CC])
    fd_d = din("fd", [NSLOT, P, 2 * H * NB])
    gbias_d = din("gbias", [P, NSLOT * NB])
    hmask_d = din("hmask", [P, NSLOT])
    bkt_d = din("bkt", [P, H * 2])
    fown_d = din("fown", [P, H])
    cvec_d = din("cvec", [P, H * DH])
    tri_d = din("tri", [P, P], BF16)
    ident_d = din("ident", [P, P], BF16)

    y_d = nc.dram_tensor("y", [NOWN, D], F32, kind="ExternalOutput").ap()

    w_in_bf = dscr("w_in_bf", [D, INC], BF16)
    w_out_bf = dscr("w_out_bf", [D, D], BF16)
    w_ff1_bf = dscr("w_ff1_bf", [D, DFF], BF16)
    w_ff2_bf = dscr("w_ff2_bf", [DFF, D], BF16)
    KT_d = dscr("KT", [H, DH, S], BF16)
    V_d = dscr("V", [S, H * DH], BF16)
    KTo_d = dscr("KTo", [H, DH, NOWN], BF16)
    Vo_d = dscr("Vo", [NOWN, H * DH], BF16)
    QT_d = dscr("QT", [H, DH, NOWN], BF16)
    F_d = dscr("Fsel", [H, P, 16 * NB], F32)
    yT_d = dscr("yT", [CC, P, NOWN], F32)
    mixT_d = dscr("mixT", [D, NOWN], BF16)
    kmean_dbg = dscr("kmean_dbg", [P, H * NB], F32) if debug else None

    with contextlib.ExitStack() as st:
        pr = Prog(nc)
        ar = Arena(nc, st, 205)
        psum = []
        pbuf = []
        for i in range(8):
            psum.append(st.enter_context(nc.psum_tensor("ps%d" % i, [P, 512], F32)))
            pbuf.append(Buf("ps%d" % i, excl=True))

        def ps_f32(i, n=512):
            return psum[i][:, 0:n]

        def ps_bf16(i):
            return psum[i][:, :].bitcast(BF16)

        cols, B_cols = ar.alloc("cols", [16 + CC * CW + 3 * CC], F32)
        pr.dma("sp", cols, cols_d, "c0", writes=[B_cols])
        bglu = cols[:, 0:16]
        wdw = cols[:, 16:16 + CC * CW].rearrange("p (c j) -> p c j", c=CC, j=CW)
        o0 = 16 + CC * CW
        bdw = cols[:, o0:o0 + CC]
        lng = cols[:, o0 + CC:o0 + 2 * CC]
        lnb = cols[:, o0 + 2 * CC:o0 + 3 * CC]
        ident, B_ident = ar.alloc("ident", [P], BF16)
        pr.dma("sp", ident, ident_d, "c1", writes=[B_ident])
        tri, B_tri = ar.alloc("tri", [P], BF16)
        pr.dma("sp", tri, tri_d, "c2", writes=[B_tri])
        bkt, B_bkt = ar.alloc("bkt", [H, 2], F32)
        pr.dma("sp", bkt, bkt_d.rearrange("p (h t) -> p h t", h=H, t=2), "c3", writes=[B_bkt])
        fown, B_fown = ar.alloc("fown", [H], F32)
        pr.dma("sp", fown, fown_d, "c4", writes=[B_fown])
        gbias, B_gbias = ar.alloc("gbias", [NSLOT, NB], F32)
        pr.dma("sp", gbias, gbias_d.rearrange("p (s n) -> p s n", s=NSLOT, n=NB), "c5", writes=[B_gbias])
        hmask, B_hmask = ar.alloc("hmask", [NSLOT], F32)
        pr.dma("sp", hmask, hmask_d, "c6", writes=[B_hmask])
        cvec, B_cvec = ar.alloc("cvec", [H * DH], F32)
        pr.dma("sp", cvec, cvec_d, "c8", writes=[B_cvec])
        kmean, B_kmean = ar.alloc("kmean", [H, NB], F32)
        ones32, B_ones = ar.alloc("ones32", [P], F32)
        pr.op("pool", lambda e: e.memset(ones32, 1.0), writes=[B_ones])
        ss_t, _ = ar.alloc("ss", [8], F32)
        rs_t, _ = ar.alloc("rs", [8], F32)
        B_ss = [Buf("ss%d" % i) for i in range(8)]
        B_rs = [Buf("rs%d" % i) for i in range(8)]
        stat_ctr = [0]

        def in_col_bufs(c0):
            if c0 < 1024:
                return B_wq
            if c0 < 3072:
                return B_wkv
            return B_wu

        def norm_T_tile(x_src, xs, B_xs, xs_key, g_b, B_gb, abf, B_abf, dstT, B_dstT, col0, tbanks):
            norm_tile(x_src, xs, B_xs, xs_key, g_b, B_gb, abf, B_abf)
            T_tile(abf, B_abf, dstT, B_dstT, col0, tbanks)

        def norm_tile(x_src, xs, B_xs, xs_key, g_b, B_gb, abf, B_abf):
            i = stat_ctr[0] % 8
            stat_ctr[0] += 1
            ssv = ss_t[:, i:i + 1]
            rsv = rs_t[:, i:i + 1]
            if x_src is not None:
                pr.dma("sp", xs, x_src, xs_key, writes=[B_xs])
            pr.op("act", lambda e: e.activation(out=abf, in_=xs, func=AF.Square, accum_out=ssv),
                  reads=[B_xs], writes=[B_ss[i], B_abf])
            pr.op("act", lambda e: e.activation(out=rsv, in_=ssv, func=AF.Sqrt, bias=RMS_EPS, scale=1.0 / D),
                  reads=[B_ss[i]], writes=[B_rs[i]])
            pr.op("dve", lambda e: e.reciprocal(out=rsv, in_=rsv), reads=[B_rs[i]], writes=[B_rs[i]])
            pr.op("dve", lambda e: e.scalar_tensor_tensor(out=abf, in0=xs, scalar=rsv, in1=g_b,
                                                          op0=ALU.mult, op1=ALU.mult),
                  reads=[B_xs, B_rs[i], B_gb], writes=[B_abf])

        def T_tile(abf, B_abf, dstT, B_dstT, col0, tbanks):
            for half in range(2):
                bank = tbanks[half]
                pt = ps_bf16(bank).rearrange("p (k n) -> p k n", k=8, n=P)

                def tr(e, half=half, pt=pt):
                    ins = None
                    for kk in range(8):
                        k = half * 8 + kk
                        ins = e.transpose(out=pt[:, kk, :], in_=abf[:, k * P:(k + 1) * P], identity=ident)
                    return ins
                pr.op("pe", tr, reads=[B_abf, B_ident], writes=[pbuf[bank]])
                dst = dstT[:, half * 8:(half + 1) * 8, col0:col0 + P]
                if half == 0:
                    pr.op("act", lambda e, dst=dst, pt=pt: e.copy(out=dst, in_=pt), reads=[pbuf[bank]], writes=[B_dstT])
                else:
                    pr.op("dve", lambda e, dst=dst, pt=pt: e.tensor_copy(out=dst, in_=pt), reads=[pbuf[bank]],
                          writes=[B_dstT])

        def mm_group(out_ap, pairs, reads, writes, name=""):
            def emit(e):
                ins = None
                n = len(pairs)
                for i, (l, r) in enumerate(pairs):
                    ins = e.matmul(out_ap, lhsT=l, rhs=r, start=(i == 0), stop=(i == n - 1))
                return ins
            return pr.op("pe", emit, reads=reads, writes=writes, name=name)

        mA = ar.mark()
        gpre_b, B_gpre = ar.alloc("gpre_b", [D], F32)
        pr.dma("sp", gpre_b, grow_d[0:1, :].partition_broadcast(P), "c7", writes=[B_gpre])
        wkv, _ = ar.alloc("wkv", [KC, 2048], BF16)
        B_wkvS = [Buf("wkvS%d" % i) for i in range(4)]
        wsrc = w_in_d[:, 1024:3072].rearrange("(k p) n -> p k n", p=P)
        for i in range(4):
            pr.dma("pool", wkv[:, 4 * i:4 * i + 4, :], wsrc[:, 4 * i:4 * i + 4, :], "wkvS%d" % i, writes=[B_wkvS[i]])
        bg_queue = []

        def cast_group(name, dst, src, pieces):
            bufs = []
            for i, (dsl, ssl) in enumerate(pieces):
                b = Buf("%s_%d" % (name, i))
                bg_queue.append((dst[dsl], src[ssl], "cast_" + name, b))
                bufs.append(b)
            return bufs

        def bg_issue(n, gates):
            for _ in range(n):
                if not bg_queue:
                    return
                d_, s_, k_, b_ = bg_queue.pop(0)
                pr.dma("pool", d_, s_, k_, writes=[b_], after=gates)

        def issued(bufs):
            assert all(b.writer is not None for b in bufs), "weight cast not issued before its consumer"
            return bufs

        def rows4(c0, c1, nrows=D):
            q = nrows // 4
            return [((slice(i * q, (i + 1) * q), slice(c0, c1)),) * 2 for i in range(4)]

        B_wkv = cast_group("wkv", w_in_bf, w_in_d, rows4(1024, 3072))
        B_wq = cast_group("wq", w_in_bf, w_in_d, rows4(0, 1024))
        B_wu = cast_group("wu", w_in_bf, w_in_d, rows4(3072, 5120))
        B_wout = cast_group("wout", w_out_bf, w_out_d, rows4(0, D))
        B_wff1 = []
        B_wff2 = []
        for g in range(4):
            B_wff1.append(cast_group("wff1_%d" % g, w_ff1_bf, w_ff1_d, rows4(g * 2048, (g + 1) * 2048)))
            pcs = [((slice(g * 2048 + i * 512, g * 2048 + (i + 1) * 512), slice(0, D)),) * 2 for i in range(4)]
            B_wff2.append(cast_group("wff2_%d" % g, w_ff2_bf, w_ff2_d, pcs))

        xsA = [ar.alloc("xsA%d" % i, [D], F32) for i in range(4)]
        abA = [ar.alloc("abA%d" % i, [D], BF16) for i in range(4)]
        aTA = [ar.alloc("aTA%d" % i, [KC, 512], BF16) for i in range(2)]
        ktst = [ar.alloc("ktst%d" % i, [H, 512], BF16) for i in range(2)]
        vst = [ar.alloc("vst%d" % i, [1024], BF16) for i in range(2)]
        kmsum, B_kmsum = ar.alloc("kmsum", [H, NB], F32)
        NG = S // 512
        KT_v = KT_d.rearrange("h d t -> d h t")
        TB = (0, 1)
        KB = (2, 3)
        VB = (4, 5)
        tile_ctr = [0]

        def A_norm_tile(g, t):
            r0 = g * 512 + t * P
            norm_tile(xall_d[r0:r0 + P, :], xsA[t][0], xsA[t][1], "xsA%d" % t, gpre_b, B_gpre, abA[t][0], abA[t][1])

        def A_T(g):
            aT, B_aT = aTA[g % 2]
            for t in range(4):
                T_tile(abA[t][0], abA[t][1], aT, B_aT, t * P, TB)

        def A_kv(g):
            aT, B_aT = aTA[g % 2]
            kst, B_kst = ktst[g % 2]
            for h in range(H):
                if h % 2 == 1 and g + 2 < NG:
                    A_norm_tile(g + 2, h // 2)
                bank = KB[h % 2]
                pk = ps_f32(bank)
                mm_group(pk, [(wkv[:, k, h * DH:(h + 1) * DH], aT[:, k, :]) for k in range(KC)],
                         reads=[B_aT] + B_wkvS, writes=[pbuf[bank]])
                pr.op("act", lambda e, pk=pk, h=h: e.copy(out=kst[:, h, :], in_=pk), reads=[pbuf[bank]], writes=[B_kst])
                pr.op("dve", lambda e, h=h: e.tensor_reduce(
                    out=kmsum[:, h, 2 * g:2 * g + 2], in_=kst[:, h, :].rearrange("p (a b) -> p a b", a=2, b=L),
                    axis=AX.X, op=ALU.add), reads=[B_kst], writes=[B_kmsum])
            pr.dma("pool", KT_v[:, :, g * 512:(g + 1) * 512], kst, "ktst%d" % (g % 2), reads=[B_kst], writes=[B_KT])
            for t in range(4):
                vi = (g * 4 + t) % 2
                vs, B_vs = vst[vi]
                for half in range(2):
                    bank = VB[half]
                    pv = ps_f32(bank)
                    mm_group(pv, [(aT[:, k, t * P:(t + 1) * P], wkv[:, k, 1024 + half * 512:1024 + (half + 1) * 512])
                                  for k in range(KC)], reads=[B_aT] + B_wkvS, writes=[pbuf[bank]])
                    hs_ = slice(half * 512, (half + 1) * 512)
                    if t % 2 == 0:
                        pr.op("dve", lambda e, pv=pv, vs=vs, hs_=hs_: e.tensor_tensor(
                            out=vs[:, hs_], in0=pv, in1=cvec[:, hs_], op=ALU.mult),
                            reads=[pbuf[bank], B_cvec], writes=[B_vs])
                    else:
                        pr.op("act", lambda e, pv=pv, vs=vs, hs_=hs_: e.copy(out=vs[:, hs_], in_=pv),
                              reads=[pbuf[bank]], writes=[B_vs])
                r0 = g * 512 + t * P
                pr.dma("pool", V_d[r0:r0 + P, :], vs, "vst%d" % vi, reads=[B_vs], writes=[B_V])

        B_KT = Buf("KT_d")
        B_V = Buf("V_d")
        for t in range(4):
            A_norm_tile(0, t)
        A_T(0)
        for t in range(4):
            A_norm_tile(1, t)
        for g in range(NG):
            if g + 1 < NG:
                A_T(g + 1)
            A_kv(g)
            if 2 <= g < 14:
                bg_issue(1, [ktst[g % 2][1]])
        pr.op("dve", lambda e: e.tensor_scalar(out=kmean, in0=kmsum, scalar1=1.0 / L, scalar2=None, op0=ALU.mult),
              reads=[B_kmsum], writes=[B_kmean])
        if debug:
            o = pr.dma("sp", kmean_dbg, kmean.rearrange("p h n -> p (h n)"), "dbg0", reads=[B_kmean])
            pr.must_finish(o)
        ar.release(mA)

        last_ops = []
        if stop_after == "A":
            return _finish(nc, pr, st, ar, [B_KT, B_V])

        mB = ar.mark()
        gpre_b, B_gpre = ar.alloc("gpre_b2", [D], F32)
        pr.dma("sp", gpre_b, grow_d[0:1, :].partition_broadcast(P), "c7", writes=[B_gpre])
        aTo, B_aTo = ar.alloc("aTo", [KC, NOWNH], BF16)
        Fall, B_Fall = ar.alloc("Fall", [16, H, NB], F32)
        mB1 = ar.mark()
        xsB = [ar.alloc("xsB%d" % i, [D], F32) for i in range(2)]
        abB = [ar.alloc("abB%d" % i, [D], BF16) for i in range(2)]
        for t in range(NOWNH // P):
            i = t % 2
            norm_T_tile(xown_d[t * P:(t + 1) * P, :], xsB[i][0], xsB[i][1], "xsA%d" % i, gpre_b, B_gpre,
                        abB[i][0], abB[i][1], aTo, B_aTo, t * P, TB)
        ar.release(mB1)
        NWS = 3
        wch = [ar.alloc("wch%d" % i, [KC, 512], BF16) for i in range(NWS)]
        wch_ctr = [0]

        def load_wchunk(src_ap, reads):
            i = wch_ctr[0] % NWS
            wch_ctr[0] += 1
            w, B_w = wch[i]
            pr.dma("sp", w, src_ap, "wch%d" % i, reads=reads, writes=[B_w])
            return w, B_w

        def in_chunk_src(c0):
            return w_in_bf[:, c0:c0 + 512].rearrange("(k p) n -> p k n", p=P)

        qst = [ar.alloc("qst%d" % i, [L], BF16) for i in range(2)]
        q32 = [ar.alloc("q32_%d" % i, [L], F32) for i in range(2)]
        kost = [ar.alloc("kost%d" % i, [L], BF16) for i in range(2)]
        vost = [ar.alloc("vost%d" % i, [512], BF16) for i in range(2)]
        fdt = [ar.alloc("fdt%d" % i, [2, H, NB], F32) for i in range(2)]
        gsb = [ar.alloc("gsb%d" % i, [NB], F32) for i in range(2)]
        top8 = [ar.alloc("top8_%d" % i, [8], F32) for i in range(2)]
        sig = [ar.alloc("sig%d" % i, [SLOTW], F32) for i in range(2)]
        hst = [ar.alloc("hst%d" % i, [SLOTW], BF16) for i in range(2)]
        dgb = [ar.alloc("dgb%d" % i, [CW, P], BF16) for i in range(2)]
        CVB = (6, 7)
        accD = [ar.alloc("accD%d" % i, [L], F32) for i in range(2)]
        B_QT = Buf("QT_d")
        B_KTo = Buf("KTo_d")
        B_Vo = Buf("Vo_d")
        B_yT = Buf("yT_d")
        B_F = Buf("F_d")
        PB = (2, 3, 4, 5)
        GB = 6
        pb_ctr = [0]

        def nbank():
            b = PB[pb_ctr[0] % len(PB)]
            pb_ctr[0] += 1
            return b
        ctr = {"q": 0, "k": 0, "v": 0, "u": 0, "g": 0, "c": 0}
        conv_pending = []

        chunk_list = [("q", 0), ("q", 512), ("k", 1024), ("k", 1536), ("v", 2048), ("v", 2560),
                      ("uv", 3072), ("ug", 4096), ("uv", 3584), ("ug", 4608)]
        pending = None
        nxt = load_wchunk(in_chunk_src(chunk_list[0][1]), in_col_bufs(chunk_list[0][1]))
        for ci, (kind, c0) in enumerate(chunk_list):
            w, B_w = nxt
            if ci + 1 < len(chunk_list):
                nxt = load_wchunk(in_chunk_src(chunk_list[ci + 1][1]), issued(in_col_bufs(chunk_list[ci + 1][1])))
            if ci >= 1:
                gate = {"q": B_QT, "k": B_KTo, "v": B_Vo, "uv": B_yT, "ug": B_yT}[chunk_list[ci - 1][0]]
                bg_issue(1, [gate])
            if kind in ("q", "k"):
                for s in range(NSLOT):
                    if kind == "q":
                        fi = ctr["g"] % 2
                        ctr["g"] += 1
                        fdv, B_fd = fdt[fi]
                        pr.dma("sp", fdv, fd_d[s].rearrange("p (t h n) -> p t h n", t=2, h=H, n=NB), "fdt%d" % fi,
                               writes=[B_fd])
                    for sub in range(4):
                        h = (c0 % 1024) // DH + sub
                        bank = nbank()
                        pq = ps_f32(bank, SLOTW)
                        mm_group(pq, [(w[:, k, sub * DH:(sub + 1) * DH], aTo[:, k, s * SLOTW:(s + 1) * SLOTW])
                                      for k in range(KC)], reads=[B_aTo, B_w], writes=[pbuf[bank]])
                        if kind == "k":
                            i = ctr["k"] % 2
                            ctr["k"] += 1
                            ks, B_ks = kost[i]
                            pr.op("act", lambda e, ks=ks, pq=pq: e.copy(out=ks, in_=pq[:, HALO:SLOTW]),
                                  reads=[pbuf[bank]], writes=[B_ks])
                            pr.dma("pool", KTo_d[h, :, s * L:(s + 1) * L], ks, "kost%d" % i, reads=[B_ks], writes=[B_KTo])
                            continue
                        i = ctr["q"] % 2
                        ctr["q"] += 1
                        qs, B_qs = qst[i]
                        qf, B_qf = q32[i]
                        pr.op("act", lambda e, qf=qf, pq=pq: e.copy(out=qf, in_=pq[:, HALO:SLOTW]),
                              reads=[pbuf[bank]], writes=[B_qf])
                        pr.op("dve", lambda e, qs=qs, qf=qf: e.tensor_copy(out=qs, in_=qf), reads=[B_qf], writes=[B_qs])
                        pr.dma("pool", QT_d[h, :, s * L:(s + 1) * L], qs, "qst%d" % i, reads=[B_qs], writes=[B_QT])
                        pg = ps_f32(GB, 2 * NB).rearrange("p (t n) -> p t n", t=2, n=NB)

                        def gmm(e, qf=qf, h=h, pg=pg):
                            e.matmul(pg[:, 0, :], lhsT=qf[:, 0:P], rhs=kmean[:, h, :], start=True, stop=True)
                            return e.matmul(pg[:, 1, :], lhsT=qf[:, P:2 * P], rhs=kmean[:, h, :], start=True, stop=True)
                        pr.op("pe", gmm, reads=[B_qf, B_kmean], writes=[pbuf[GB]])
                        for qt in range(2):
                            gi = ctr["u"] % 2
                            ctr["u"] += 1
                            gs, B_gs = gsb[gi]
                            t8, B_t8 = top8[gi]
                            pr.op("dve", lambda e, gs=gs, qt=qt, pg=pg, s=s: e.tensor_tensor(
                                out=gs, in0=pg[:, qt, :], in1=gbias[:, s, :], op=ALU.add),
                                reads=[pbuf[GB], B_gbias], writes=[B_gs])
                            pr.op("dve", lambda e, gs=gs, t8=t8: e.max(out=t8, in_=gs), reads=[B_gs], writes=[B_t8])
                            pr.op("dve", lambda e, gs=gs, t8=t8, qt=qt, h=h, s=s, fdv=fdv: e.scalar_tensor_tensor(
                                out=Fall[:, 2 * s + qt, h, :], in0=gs, scalar=t8[:, 2:3], in1=fdv[:, qt, h, :],
                                op0=ALU.is_ge, op1=ALU.mult), reads=[B_gs, B_t8, B_fd], writes=[B_Fall])
            elif kind == "v":
                for s in range(NSLOT):
                    for t in range(2):
                        bank = nbank()
                        pv = ps_f32(bank)
                        col = s * SLOTW + HALO + t * P
                        mm_group(pv, [(aTo[:, k, col:col + P], w[:, k, :]) for k in range(KC)],
                                 reads=[B_aTo, B_w], writes=[pbuf[bank]])
                        i = ctr["v"] % 2
                        ctr["v"] += 1
                        vs, B_vs = vost[i]
                        pr.op("act", lambda e, vs=vs, pv=pv: e.copy(out=vs, in_=pv), reads=[pbuf[bank]], writes=[B_vs])
                        r0 = s * L + t * P
                        cv = c0 - 2048
                        pr.dma("pool", Vo_d[r0:r0 + P, cv:cv + 512], vs, "vost%d" % i, reads=[B_vs], writes=[B_Vo])
            elif kind == "uv":
                pending = (w, B_w, c0)
            else:
                wv, B_wv, cv0 = pending
                for sub in range(4):
                    cch = (cv0 - 3072) // P + sub
                    dgv, B_dg = dgb[cch % 2]
                    for jt in range(CW):
                        pr.op("dve", lambda e, dgv=dgv, cch=cch, jt=jt: e.tensor_scalar(
                            out=dgv[:, jt, :], in0=ident, scalar1=wdw[:, cch, jt:jt + 1], scalar2=None, op0=ALU.mult),
                            reads=[B_ident, B_cols], writes=[B_dg])
                    for s in range(NSLOT):
                        bv = nbank()
                        bg = nbank()
                        pval = ps_f32(bv, SLOTW)
                        pgt = ps_f32(bg, SLOTW)
                        rhs_sl = slice(s * SLOTW, (s + 1) * SLOTW)
                        mm_group(pval, [(wv[:, k, sub * P:(sub + 1) * P], aTo[:, k, rhs_sl]) for k in range(KC)],
                                 reads=[B_aTo, B_wv], writes=[pbuf[bv]])
                        mm_group(pgt, [(w[:, k, sub * P:(sub + 1) * P], aTo[:, k, rhs_sl]) for k in range(KC)],
                                 reads=[B_aTo, B_w], writes=[pbuf[bg]])
                        i = ctr["c"] % 2
                        ctr["c"] += 1
                        sg, B_sg = sig[i]
                        hs, B_hs = hst[i]
                        aD, B_aD = accD[i]
                        pr.op("act", lambda e, sg=sg, pgt=pgt, cch=cch: e.activation(
                            out=sg, in_=pgt, func=AF.Sigmoid, bias=bglu[:, 8 + cch:9 + cch], scale=1.0),
                            reads=[pbuf[bg], B_cols], writes=[B_sg])
                        pr.op("dve", lambda e, hs=hs, pval=pval, sg=sg, cch=cch: e.scalar_tensor_tensor(
                            out=hs, in0=pval, scalar=bglu[:, cch:cch + 1], in1=sg, op0=ALU.add, op1=ALU.mult),
                            reads=[pbuf[bv], B_sg, B_cols], writes=[B_hs])
                        pr.op("dve", lambda e, hs=hs, s=s: e.tensor_scalar(
                            out=hs[:, 0:HALO], in0=hs[:, 0:HALO], scalar1=hmask[:, s:s + 1], scalar2=None, op0=ALU.mult),
                            reads=[B_hs, B_hmask], writes=[B_hs])
                        def conv_unit(dgv=dgv, B_dg=B_dg, hs=hs, B_hs=B_hs, aD=aD, B_aD=B_aD, cch=cch, s=s, i=i,
                                      cb=CVB[ctr["c"] % 2]):
                            pc = ps_f32(cb, L)
                            mm_group(pc, [(dgv[:, jt, :], hs[:, 2 + jt:2 + jt + L]) for jt in range(CW)],
                                     reads=[B_dg, B_hs], writes=[pbuf[cb]])
                            pr.op("dve", lambda e: e.tensor_scalar(
                                out=aD, in0=pc, scalar1=bdw[:, cch:cch + 1], scalar2=None, op0=ALU.add),
                                reads=[pbuf[cb], B_cols], writes=[B_aD])
                            pr.dma("pool", yT_d[cch, :, s * L:(s + 1) * L], aD, "accD%d" % i, reads=[B_aD],
                                   writes=[B_yT])
                        if conv_pending:
                            conv_pending.pop(0)()
                        conv_pending.append(conv_unit)
        while conv_pending:
            conv_pending.pop(0)()
        for h in range(H):
            pr.dma("pool", F_d[h].rearrange("p (q n) -> p q n", q=16, n=NB), Fall[:, :, h, :], "fst", reads=[B_Fall],
                   writes=[B_F])
        ar.release(mB)
        if stop_after == "B":
            return _finish(nc, pr, st, ar, [B_KT, B_V, B_QT, B_KTo, B_Vo, B_yT, B_F])

        B_mixT = Buf("mixT_d")
        mL = ar.mark()
        ysl = [ar.alloc("ysl%d" % i, [CC, L], F32) for i in range(2)]
        sqs = [ar.alloc("sqs%d" % i, [CC, L], F32) for i in range(2)]
        cst = [ar.alloc("cst%d" % i, [CC, L], BF16) for i in range(2)]
        mean_t, B_mean = ar.alloc("mean_t", [L], F32)
        msq_t, B_msq = ar.alloc("msq_t", [L], F32)
        rstd_t, B_rstdL = ar.alloc("rstd_t", [L], F32)
        mr_t, B_mr = ar.alloc("mr_t", [L], F32)
        tmpL = [ar.alloc("tmpL%d" % i, [L], F32) for i in range(2)]
        yT_v = yT_d.rearrange("c p t -> p c t")
        for s in range(NSLOT):
            i = s % 2
            ys, B_ys = ysl[i]
            sq, B_sq = sqs[i]
            cs, B_cs = cst[i]
            pr.dma("sp", ys, yT_v[:, :, s * L:(s + 1) * L], "ysl%d" % i, reads=[B_yT], writes=[B_ys])
            pr.op("act", lambda e, ys=ys, sq=sq: e.activation(out=sq, in_=ys, func=AF.Square), reads=[B_ys], writes=[B_sq])
            pm = ps_f32(2, L)
            pq2 = ps_f32(3, L)
            mm_group(pm, [(ones32, ys[:, c, :]) for c in range(CC)], reads=[B_ones, B_ys], writes=[pbuf[2]])
            mm_group(pq2, [(ones32, sq[:, c, :]) for c in range(CC)], reads=[B_ones, B_sq], writes=[pbuf[3]])
            pr.op("dve", lambda e, pm=pm: e.tensor_scalar(out=mean_t, in0=pm, scalar1=1.0 / C, scalar2=None, op0=ALU.mult),
                  reads=[pbuf[2]], writes=[B_mean])
            pr.op("dve", lambda e: e.tensor_tensor(out=msq_t, in0=mean_t, in1=mean_t, op=ALU.mult),
                  reads=[B_mean], writes=[B_msq])
            pr.op("dve", lambda e, pq2=pq2: e.scalar_tensor_tensor(out=rstd_t, in0=pq2, scalar=1.0 / C, in1=msq_t,
                                                                  op0=ALU.mult, op1=ALU.subtract),
                  reads=[pbuf[3], B_msq], writes=[B_rstdL])
            pr.op("act", lambda e: e.activation(out=rstd_t, in_=rstd_t, func=AF.Sqrt, bias=LN_EPS, scale=1.0),
                  reads=[B_rstdL], writes=[B_rstdL])
            pr.op("dve", lambda e: e.reciprocal(out=rstd_t, in_=rstd_t), reads=[B_rstdL], writes=[B_rstdL])
            pr.op("dve", lambda e: e.tensor_tensor(out=mr_t, in0=mean_t, in1=rstd_t, op=ALU.mult),
                  reads=[B_mean, B_rstdL], writes=[B_mr])
            for c in range(CC):
                tl, B_tl = tmpL[c % 2]
                pr.op("dve", lambda e, tl=tl, ys=ys, c=c: e.tensor_tensor(out=tl, in0=ys[:, c, :], in1=rstd_t, op=ALU.mult),
                      reads=[B_ys, B_rstdL], writes=[B_tl])
                pr.op("dve", lambda e, tl=tl: e.tensor_tensor(out=tl, in0=tl, in1=mr_t, op=ALU.subtract),
                      reads=[B_tl, B_mr], writes=[B_tl])
                pr.op("act", lambda e, tl=tl, cs=cs, c=c: e.activation(out=cs[:, c, :], in_=tl, func=AF.Silu,
                                                                     bias=lnb[:, c:c + 1], scale=lng[:, c:c + 1]),
                      reads=[B_tl, B_cols], writes=[B_cs])
            pr.dma("pool", mixT_d[C:2 * C, s * L:(s + 1) * L].rearrange("(c p) t -> p c t", p=P), cs, "cst%d" % i,
                   reads=[B_cs], writes=[B_mixT])
        ar.release(mL)
        if stop_after == "L":
            return _finish(nc, pr, st, ar, [B_KT, B_V, B_QT, B_KTo, B_Vo, B_yT, B_F, B_mixT])

        mT = ar.mark()
        NT = S // P
        VW = DH + 2
        KTh = [ar.alloc("KTh%d" % i, [S], BF16) for i in range(2)]
        Vh = [ar.alloc("Vh%d" % i, [NT, VW], BF16) for i in range(2)]
        KToh = [ar.alloc("KToh%d" % i, [NOWN], BF16) for i in range(2)]
        Voh = [ar.alloc("Voh%d" % i, [NOWN // P, VW], BF16) for i in range(2)]
        QTh = [ar.alloc("QTh%d" % i, [NOWN], BF16) for i in range(2)]
        Fh = [ar.alloc("Fh%d" % i, [16, NB], F32) for i in range(2)]
        pTs = [ar.alloc("pT%d" % i, [2, L], BF16) for i in range(3)]
        pTo = [ar.alloc("pTo%d" % i, [3 * P], BF16) for i in range(2)]
        accs = [ar.alloc("acc%d" % i, [2, VW], F32) for i in range(2)]
        tmps = [ar.alloc("tmpT%d" % i, [2, VW], F32) for i in range(3)]
        rcs = [ar.alloc("rc%d" % i, [2], F32) for i in range(2)]
        obf = [ar.alloc("obf%d" % i, [2, DH], BF16) for i in range(2)]
        ast = [ar.alloc("ast%d" % i, [L], BF16) for i in range(2)]
        for i in range(2):
            pr.op("pool", lambda e, i=i: e.memset(Vh[i][0][:, :, DH:VW], 1.0), writes=[Vh[i][1]])
            pr.op("pool", lambda e, i=i: e.memset(Voh[i][0][:, :, DH:VW], 1.0), writes=[Voh[i][1]])
        SBK = (0, 1, 2)
        OBK = (3, 4, 5)
        TBK = 6

        def T_load(h):
            i = h % 2
            pr.dma("sp", KTh[i][0], KT_d[h], "KTh%d" % i, reads=[B_KT], writes=[KTh[i][1]])
            ch = float(np.exp(-128.0 * alibi_slopes()[h]))
            vev = Vh[i][0].rearrange("p (a two) w -> p a two w", two=2)[:, :, 0, DH:VW]
            pr.op("pool", lambda e, vev=vev, ch=ch: e.memset(vev, ch), writes=[Vh[i][1]])
            vsrc = V_d[:, h * DH:(h + 1) * DH].rearrange("(t p) d -> p t d", p=P)
            for q4 in range(4):
                pr.dma("sp", Vh[i][0][:, q4 * 16:(q4 + 1) * 16, 0:DH], vsrc[:, q4 * 16:(q4 + 1) * 16, :], "Vh%d_%d" % (i, q4),
                       reads=[B_V], writes=[Vh[i][1]])
            pr.dma("sp", KToh[i][0], KTo_d[h], "KToh%d" % i, reads=[B_KTo], writes=[KToh[i][1]])
            pr.dma("sp", Voh[i][0][:, :, 0:DH], Vo_d[:, h * DH:(h + 1) * DH].rearrange("(t p) d -> p t d", p=P),
                   "Voh%d" % i, reads=[B_Vo], writes=[Voh[i][1]])
            pr.dma("sp", QTh[i][0], QT_d[h], "QTh%d" % i, reads=[B_QT], writes=[QTh[i][1]])
            pr.dma("sp", Fh[i][0], F_d[h].rearrange("p (q n) -> p q n", q=16, n=NB), "Fh%d" % i, reads=[B_F],
                   writes=[Fh[i][1]])

        uctr = [0]

        def T_head(h):
            i = h % 2
            kth, B_kth = KTh[i]
            vh, B_vh = Vh[i]
            kto, B_kto = KToh[i]
            voh, B_voh = Voh[i]
            qth, B_qth = QTh[i]
            fh, B_fh = Fh[i]
            units = []
            for s in range(NSLOT):
                units.append((s, -1))
                for n in range(PAST[s]):
                    units.append((s, n))
            state = {}
            deferred = []

            def emit_qk(idx):
                s, n = units[idx]
                u = uctr[0]
                uctr[0] += 1
                sb = SBK[u % 3]
                qsl = slice(s * L, (s + 1) * L)
                if n >= 0:
                    sv = ps_f32(sb).rearrange("p (t q) -> p t q", t=2, q=L)

                    def qk(e, sv=sv, n=n, qsl=qsl):
                        e.matmul(sv[:, 0, :], lhsT=kth[:, (2 * n) * P:(2 * n + 1) * P], rhs=qth[:, qsl], start=True, stop=True)
                        return e.matmul(sv[:, 1, :], lhsT=kth[:, (2 * n + 1) * P:(2 * n + 2) * P], rhs=qth[:, qsl],
                                        start=True, stop=True)
                    pr.op("pe", qk, reads=[B_kth, B_qth], writes=[pbuf[sb]])
                    pt, B_pt = pTs[u % 3]
                    pr.op("act", lambda e, pt=pt, sv=sv: e.activation(
                        out=pt, in_=sv, func=AF.Exp, bias=bkt[:, h, 1:2], scale=SCALE),
                        reads=[pbuf[sb], B_bkt], writes=[B_pt])
                    state[idx] = (u, pt, B_pt)
                else:
                    sv = ps_f32(sb, 3 * P)
                    q0 = s * L

                    def qk(e, sv=sv, q0=q0):
                        e.matmul(sv[:, 0:P], lhsT=kto[:, q0:q0 + P], rhs=qth[:, q0:q0 + P], start=True, stop=True)
                        e.matmul(sv[:, P:2 * P], lhsT=kto[:, q0 + P:q0 + 2 * P], rhs=qth[:, q0 + P:q0 + 2 * P],
                                 start=True, stop=True)
                        return e.matmul(sv[:, 2 * P:3 * P], lhsT=kto[:, q0:q0 + P], rhs=qth[:, q0 + P:q0 + 2 * P],
                                        start=True, stop=True)
                    pr.op("pe", qk, reads=[B_kto, B_qth], writes=[pbuf[sb]])
                    pt, B_pt = pTo[s % 2]
                    pr.op("act", lambda e, pt=pt, sv=sv: e.activation(
                        out=pt[:, 0:2 * P], in_=sv[:, 0:2 * P], func=AF.Exp, bias=bkt[:, h, 1:2], scale=SCALE),
                        reads=[pbuf[sb], B_bkt], writes=[B_pt])
                    pr.op("act", lambda e, pt=pt, sv=sv: e.activation(
                        out=pt[:, 2 * P:3 * P], in_=sv[:, 2 * P:3 * P], func=AF.Exp, bias=bkt[:, h, 0:1], scale=SCALE),
                        reads=[pbuf[sb], B_bkt], writes=[B_pt])
                    for a in range(2):
                        pr.op("dve", lambda e, pt=pt, a=a: e.tensor_tensor(
                            out=pt[:, a * P:(a + 1) * P], in0=pt[:, a * P:(a + 1) * P], in1=tri, op=ALU.mult),
                            reads=[B_pt, B_tri], writes=[B_pt])
                    state[idx] = (u, pt, B_pt)

            def emit_pv(idx):
                s, n = units[idx]
                u, pt, B_pt = state.pop(idx)
                ob = OBK[u % 3]
                ov = ps_f32(ob, 2 * VW).rearrange("p (t d) -> p t d", t=2, d=VW)
                acc, B_acc = accs[s % 2]
                NV = DH + 1
                if n >= 0:
                    def pv(e, ov=ov, pt=pt, n=n):
                        ins = None
                        for qt in range(2):
                            for t in range(2):
                                ins = e.matmul(ov[:, qt, 0:NV], lhsT=pt[:, t, qt * P:(qt + 1) * P],
                                               rhs=vh[:, 2 * n + t, 0:NV], start=(t == 0), stop=(t == 1))
                        return ins
                    pr.op("pe", pv, reads=[B_pt, B_vh], writes=[pbuf[ob]])
                    for qt in range(2):
                        pr.op("dve", lambda e, ov=ov, acc=acc, qt=qt, n=n, s=s: e.scalar_tensor_tensor(
                            out=acc[:, qt, 0:NV], in0=ov[:, qt, 0:NV], scalar=fh[:, 2 * s + qt, n:n + 1],
                            in1=acc[:, qt, 0:NV], op0=ALU.mult, op1=ALU.add),
                            reads=[pbuf[ob], B_fh, B_acc], writes=[B_acc])
                else:
                    def pv(e, ov=ov, pt=pt, s=s):
                        e.matmul(ov[:, 0, 0:NV], lhsT=pt[:, 0:P], rhs=voh[:, 2 * s, 0:NV], start=True, stop=True)
                        e.matmul(ov[:, 1, 0:NV], lhsT=pt[:, 2 * P:3 * P], rhs=voh[:, 2 * s, 0:NV], start=True, stop=False)
                        return e.matmul(ov[:, 1, 0:NV], lhsT=pt[:, P:2 * P], rhs=voh[:, 2 * s + 1, 0:NV],
                                        start=False, stop=True)
                    pr.op("pe", pv, reads=[B_pt, B_voh], writes=[pbuf[ob]])
                    pr.op("dve", lambda e, ov=ov, acc=acc: e.tensor_scalar(
                        out=acc[:, :, 0:NV], in0=ov[:, :, 0:NV], scalar1=fown[:, h:h + 1], scalar2=None,
                        op0=ALU.mult), reads=[pbuf[ob], B_fown], writes=[B_acc])
                last = (idx + 1 == len(units)) or (units[idx + 1][0] != s)
                if last:
                    deferred.append([2, s])

            def finalize(s):
                acc, B_acc = accs[s % 2]
                rc, B_rc = rcs[s % 2]
                ob_, B_ob = obf[s % 2]
                asv, B_as = ast[s % 2]
                pr.op("dve", lambda e: e.reciprocal(out=rc, in_=acc[:, :, DH]), reads=[B_acc], writes=[B_rc])
                for qt in range(2):
                    pr.op("dve", lambda e, qt=qt: e.tensor_scalar(out=ob_[:, qt, :], in0=acc[:, qt, 0:DH],
                                                                  scalar1=rc[:, qt:qt + 1], scalar2=None, op0=ALU.mult),
                          reads=[B_acc, B_rc], writes=[B_ob])
                ptv = ps_bf16(TBK)[:, 0:L].rearrange("p (t q) -> p t q", t=2, q=P)

                def tr(e):
                    e.transpose(out=ptv[:, 0, :], in_=ob_[:, 0, :], identity=ident)
                    return e.transpose(out=ptv[:, 1, :], in_=ob_[:, 1, :], identity=ident)
                pr.op("pe", tr, reads=[B_ob, B_ident], writes=[pbuf[TBK]])
                pr.op("act", lambda e: e.copy(out=asv, in_=ps_bf16(TBK)[:, 0:L]), reads=[pbuf[TBK]], writes=[B_as])
                pr.dma("pool", mixT_d[h * DH:(h + 1) * DH, s * L:(s + 1) * L], asv, "ast%d" % (s % 2), reads=[B_as],
                       writes=[B_mixT])

            def tick():
                for dd in list(deferred):
                    dd[0] -= 1
                    if dd[0] <= 0:
                        deferred.remove(dd)
                        finalize(dd[1])

            emit_qk(0)
            emit_qk(1)
            for idx in range(len(units)):
                if idx + 2 < len(units):
                    emit_qk(idx + 2)
                emit_pv(idx)
                tick()
            for dd in list(deferred):
                finalize(dd[1])
            deferred.clear()

        T_load(0)
        for h in range(H):
            if h + 1 < H:
                T_load(h + 1)
            bg_issue(4, [B_mixT])
            T_head(h)
        bg_issue(100, [B_mixT])
        ar.release(mT)
        if stop_after == "T":
            return _finish(nc, pr, st, ar, [B_mixT])

        gb_t = []
        for gi_, nm in ((1, "gpost"), (2, "gffn"), (3, "gfpost")):
            t_, b_ = ar.alloc(nm, [D], F32)
            pr.dma("sp", t_, grow_d[gi_:gi_ + 1, :].partition_broadcast(P), "c7_%d" % gi_, writes=[b_])
            gb_t.append((t_, b_))
        (gpost_b, B_gpost), (gffn_b, B_gffn), (gfpost_b, B_gfpost) = gb_t
        mixS, B_mixS = ar.alloc("mixS", [KC, 512], BF16)
        fT, B_fT = ar.alloc("fT", [KC, 512], BF16)
        accF, _ = ar.alloc("accF", [4, D], F32)
        B_accF = [Buf("accF%d" % t) for t in range(4)]
        hTs = [ar.alloc("hT%d" % i, [4, 512], BF16) for i in range(2)]
        rts = [ar.alloc("rt%d" % i, [512], F32) for i in range(2)]
        NWS2 = 4
        wc2 = [ar.alloc("wc2_%d" % i, [KC, 512], BF16) for i in range(NWS2)]
        xsC = [ar.alloc("xsC%d" % i, [D], F32) for i in range(2)]
        abC = [ar.alloc("abC%d" % i, [D], BF16) for i in range(4)]
        w2ctr = [0]

        def load_w2(src_ap, reads):
            i = w2ctr[0] % NWS2
            w2ctr[0] += 1
            w, B_w = wc2[i]
            pr.dma("sp", w, src_ap, "wch%d" % i, reads=reads, writes=[B_w])
            return w, B_w
        B_y = [Buf("y%d" % t) for t in range(NOWN // P)]
        CB = (2, 3)
        HBK = (4, 5)
        cb_ctr = [0]
        xc_ctr = [0]

        def rstd_of(src, B_src, junk, B_junk):
            i = stat_ctr[0] % 8
            stat_ctr[0] += 1
            ssv = ss_t[:, i:i + 1]
            rsv = rs_t[:, i:i + 1]
            pr.op("act", lambda e: e.activation(out=junk, in_=src, func=AF.Square, accum_out=ssv),
                  reads=[B_src], writes=[B_ss[i], B_junk])
            pr.op("act", lambda e: e.activation(out=rsv, in_=ssv, func=AF.Sqrt, bias=RMS_EPS, scale=1.0 / D),
                  reads=[B_ss[i]], writes=[B_rs[i]])
            pr.op("dve", lambda e: e.reciprocal(out=rsv, in_=rsv), reads=[B_rs[i]], writes=[B_rs[i]])
            return rsv, B_rs[i]

        def post_tile(gi, t):
            i = xc_ctr[0] % 2
            xc_ctr[0] += 1
            xs, B_xs = xsC[i]
            ab, B_ab = abC[t]
            s_ = 2 * gi + t // 2
            r0 = s_ * SLOTW + HALO + (t % 2) * P
            pr.dma("sp", xs, xown_d[r0:r0 + P, :], "xsA%d" % i, writes=[B_xs])
            rsv, B_r = rstd_of(accF[:, t, :], B_accF[t], ab, B_ab)
            pr.op("dve", lambda e, t=t, rsv=rsv: e.scalar_tensor_tensor(
                out=accF[:, t, :], in0=accF[:, t, :], scalar=rsv, in1=gpost_b, op0=ALU.mult, op1=ALU.mult),
                reads=[B_accF[t], B_r, B_gpost], writes=[B_accF[t]])
            pr.op("dve", lambda e, t=t, xs=xs: e.tensor_tensor(out=xs, in0=accF[:, t, :], in1=xs, op=ALU.add),
                  reads=[B_accF[t], B_xs], writes=[B_xs])
            yt = gi * 4 + t
            pr.dma("pool", y_d[yt * P:(yt + 1) * P, :], xs, "xsSt%d" % i, reads=[B_xs], writes=[B_y[yt]])
            ab, B_ab = abC[t]
            norm_tile(None, xs, B_xs, None, gffn_b, B_gffn, ab, B_ab)

        def final_tile(gi, t):
            i = xc_ctr[0] % 2
            xc_ctr[0] += 1
            xs, B_xs = xsC[i]
            ab, B_ab = abC[t]
            yt = gi * 4 + t
            pr.dma("sp", xs, y_d[yt * P:(yt + 1) * P, :], "xsA%d" % i, reads=[B_y[yt]], writes=[B_xs])
            rsv, B_r = rstd_of(accF[:, t, :], B_accF[t], ab, B_ab)
            pr.op("dve", lambda e, t=t, rsv=rsv: e.scalar_tensor_tensor(
                out=accF[:, t, :], in0=accF[:, t, :], scalar=rsv, in1=gfpost_b, op0=ALU.mult, op1=ALU.mult),
                reads=[B_accF[t], B_r, B_gfpost], writes=[B_accF[t]])
            pr.op("dve", lambda e, t=t, xs=xs: e.tensor_tensor(out=xs, in0=accF[:, t, :], in1=xs, op=ALU.add),
                  reads=[B_accF[t], B_xs], writes=[B_xs])
            o = pr.dma("pool", y_d[yt * P:(yt + 1) * P, :], xs, "xsSt%d" % i, reads=[B_xs], writes=[B_y[yt]])
            pr.must_finish(o)

        for gi in range(4):
            pr.dma("sp", mixS, mixT_d[:, gi * 512:(gi + 1) * 512].rearrange("(k p) t -> p k t", p=P), "mixS",
                   reads=[B_mixT], writes=[B_mixS])
            nxt = load_w2(w_out_bf[:, 0:512].rearrange("(k p) n -> p k n", p=P), issued(B_wout))
            for n in range(4):
                w, B_w = nxt
                if n + 1 < 4:
                    nxt = load_w2(w_out_bf[:, (n + 1) * 512:(n + 2) * 512].rearrange("(k p) n -> p k n", p=P), B_wout)
                else:
                    nxtW1 = [load_w2(w_ff1_bf[:, 0:512].rearrange("(k p) n -> p k n", p=P), B_wff1[0])]
                for t in range(4):
                    bank = CB[cb_ctr[0] % 2]
                    cb_ctr[0] += 1
                    po = ps_f32(bank)
                    mm_group(po, [(mixS[:, k, t * P:(t + 1) * P], w[:, k, :]) for k in range(KC)],
                             reads=[B_mixS, B_w], writes=[pbuf[bank]])
                    pr.op("act", lambda e, po=po, t=t, n=n: e.copy(out=accF[:, t, n * 512:(n + 1) * 512], in_=po),
                          reads=[pbuf[bank]], writes=[B_accF[t]])
                    if n == 3:
                        post_tile(gi, t)
            for t in range(4):
                T_tile(abC[t][0], abC[t][1], fT, B_fT, t * P, TB)
            NFC = DFF // 512

            def w1src(c):
                return w_ff1_bf[:, c * 512:(c + 1) * 512].rearrange("(k p) n -> p k n", p=P), B_wff1[c // 4]

            def w2src(c):
                return w_ff2_bf[c * 512:(c + 1) * 512, :].rearrange("(j p) n -> p j n", p=P), B_wff2[c // 4]
            W1 = {0: nxtW1[0]}
            W2 = {}
            W1[1] = load_w2(*w1src(1))
            W2[0] = load_w2(*w2src(0))

            def ffn_H(c):
                w, B_w = W1.pop(c)
                hT, B_hT = hTs[c % 2]
                for j in range(4):
                    bank = HBK[j % 2]
                    ph = ps_f32(bank)
                    mm_group(ph, [(w[:, k, j * P:(j + 1) * P], fT[:, k, :]) for k in range(KC)],
                             reads=[B_w, B_fT], writes=[pbuf[bank]])
                    rt, B_rt = rts[j % 2]
                    pr.op("act", lambda e, rt=rt, ph=ph: e.activation(out=rt, in_=ph, func=AF.Relu),
                          reads=[pbuf[bank]], writes=[B_rt])
                    pr.op("act", lambda e, rt=rt, hT=hT, j=j: e.activation(out=hT[:, j, :], in_=rt, func=AF.Square),
                          reads=[B_rt], writes=[B_hT])

            def ffn_O(c):
                w, B_w = W2.pop(c)
                wv_ = w.rearrange("p k n -> p (k n)").rearrange("p (j n) -> p j n", j=4, n=D)
                hT, B_hT = hTs[c % 2]
                for t in range(4):
                    for n in range(4):
                        bank = CB[cb_ctr[0] % 2]
                        cb_ctr[0] += 1
                        po = ps_f32(bank)
                        mm_group(po, [(hT[:, j, t * P:(t + 1) * P], wv_[:, j, n * 512:(n + 1) * 512]) for j in range(4)],
                                 reads=[B_hT, B_w], writes=[pbuf[bank]])
                        dst = accF[:, t, n * 512:(n + 1) * 512]
                        if c == 0:
                            pr.op("dve", lambda e, dst=dst, po=po: e.tensor_copy(out=dst, in_=po),
                                  reads=[pbuf[bank]], writes=[B_accF[t]])
                        else:
                            pr.op("dve", lambda e, dst=dst, po=po: e.tensor_tensor(out=dst, in0=po, in1=dst, op=ALU.add),
                                  reads=[pbuf[bank], B_accF[t]], writes=[B_accF[t]])
                    if c == NFC - 1:
                        final_tile(gi, t)
            ffn_H(0)
            for c in range(NFC):
                if c + 2 < NFC:
                    W1[c + 2] = load_w2(*w1src(c + 2))
                if c + 1 < NFC:
                    W2[c + 1] = load_w2(*w2src(c + 1))
                    ffn_H(c + 1)
                ffn_O(c)
        return _finish(nc, pr, st, ar, [])


def _finish(nc, pr, st, ar, bufs):
    for b in bufs:
        if b.writer is not None:
            pr.must_finish(b.writer)
    pr.emit_all(st)
    nc._mk_info = dict(n_sems=pr.n_sems, peak=ar.peak, nops=len(pr.all_ops), counts=pr.max_count)
    return nc


def alibi_slopes():
    return (2.0 ** (-8.0 * np.arange(1, H + 1) / H)).astype(np.float64)


def core_tables(j):
    blks = own_blocks(j)
    sl = alibi_slopes()
    fd = np.zeros((NSLOT, P, 2, H, NB), np.float64)
    gb = np.full((P, NSLOT, NB), -1e30, np.float32)
    hm = np.ones((P, NSLOT), np.float32)
    q = np.arange(P)
    for s, blk in enumerate(blks):
        if blk == 0:
            hm[:, s] = 0.0
        for n in range(blk):
            gb[:, s, n] = 0.0
            for qt in range(2):
                dist = 256 * (n - blk) + 255 - (128 * qt + q)
                fd[s, :, qt, :, n] = np.exp(sl[None, :] * dist[:, None])
    return (fd.reshape(NSLOT, P, 2 * H * NB).astype(np.float32), gb.reshape(P, NSLOT * NB), hm)


def const_tables():
    sl = alibi_slopes()
    p = np.arange(P)
    bkt = np.zeros((P, H, 2), np.float64)
    for t in range(2):
        bkt[:, :, t] = sl[None, :] * (t * 128 + p[:, None] - 255)
    fown = np.exp(sl[None, :] * (127 - p[:, None]))
    tri = (p[None, :] >= p[:, None]).astype(np.float32)
    cvec = np.repeat(np.exp(-128.0 * sl), DH)[None, :].repeat(P, 0)
    return (bkt.reshape(P, 2 * H).astype(np.float32), fown.astype(np.float32),
            tri.astype(ml_dtypes.bfloat16), np.eye(P, dtype=np.float32).astype(ml_dtypes.bfloat16),
            np.ascontiguousarray(cvec.astype(np.float32)))


def make_in_maps(inputs, cores=range(8)):
    x = np.asarray(inputs["x"], np.float32)
    f = lambda k: np.ascontiguousarray(np.asarray(inputs[k], np.float32)[0])
    w_in, w_out, w_ff1, w_ff2 = f("w_in"), f("w_out"), f("w_ff1"), f("w_ff2")
    grow = np.stack([f("g_mix_pre"), f("g_mix_post"), f("g_ffn_pre"), f("g_ffn_post")]).astype(np.float32)
    b_glu, w_dw, b_dw, ln_g, ln_b = f("b_glu"), f("w_dw"), f("b_dw"), f("ln_conv_g"), f("ln_conv_b")
    cols = np.concatenate([
        b_glu.reshape(16, P).T,
        w_dw.reshape(CW, CC, P).transpose(2, 1, 0).reshape(P, CC * CW),
        b_dw.reshape(CC, P).T, ln_g.reshape(CC, P).T, ln_b.reshape(CC, P).T], axis=1).astype(np.float32)
    bkt, fown, tri, ident, cvec = const_tables()
    maps = []
    for c in cores:
        bi, j = c // 4, c % 4
        fd, gb, hm = core_tables(j)
        xo = np.zeros((NSLOT, SLOTW, D), np.float32)
        for s, blk in enumerate(own_blocks(j)):
            lo = L * blk - HALO
            if lo < 0:
                xo[s, HALO:] = x[bi, 0:L]
            else:
                xo[s] = x[bi, lo:lo + SLOTW]
        maps.append(dict(xall=np.ascontiguousarray(x[bi]), xown=xo.reshape(NOWNH, D), w_in=w_in, w_out=w_out,
                         w_ff1=w_ff1, w_ff2=w_ff2, grow=grow, cols=np.ascontiguousarray(cols), fd=fd, gbias=gb,
                         hmask=hm, bkt=bkt, fown=fown, tri=tri, ident=ident, cvec=cvec))
    return maps


_NC = None


def kernel(**inputs):
    global _NC
    if _NC is None:
        _NC = build_nc()
    maps = make_in_maps(inputs)
    res = run_bass_kernel_spmd(_NC, maps, core_ids=list(range(8)))
    x = np.asarray(inputs["x"])
    out = np.zeros(x.shape, np.float32)
    for c in range(8):
        bi, j = c // 4, c % 4
        y = np.asarray(res.results[c]["y"])
        for s, blk in enumerate(own_blocks(j)):
            out[bi, L * blk:L * (blk + 1)] = y[s * L:(s + 1) * L]
    return out
```
